# Optimizing a Trainium2 kernel written in Bass

```python
import math
import jax
import jax.numpy as jnp
from jax import lax
import numpy as np


D_MODEL = 4096
BATCH = 2
SEQ = 8192
DEPTH = 2

CTX_LEN = 256
GRID_W = 64

RET_WIDTH = D_MODEL // 4
RET_HEAD_DIM = 128
RET_HEADS = RET_WIDTH // RET_HEAD_DIM
SSD_WIDTH = D_MODEL // 2
SSD_HEAD_DIM = 64
SSD_HEADS = SSD_WIDTH // SSD_HEAD_DIM
SSD_GROUPS = 8
SSD_HEADS_PER_GROUP = SSD_HEADS // SSD_GROUPS
SSD_STATE = 128
SSD_CONV = 5
SSD_CONV_DIM = SSD_WIDTH + 2 * SSD_GROUPS * SSD_STATE
DIFF_WIDTH = D_MODEL // 4
DIFF_V_DIM = 128
DIFF_HEADS = DIFF_WIDTH // DIFF_V_DIM
DIFF_QK_DIM = DIFF_V_DIM // 2
MIX_WIDTH = RET_WIDTH + SSD_WIDTH + DIFF_WIDTH

IN_SIZES = (RET_WIDTH, RET_WIDTH, RET_WIDTH, RET_WIDTH,
            SSD_WIDTH, SSD_CONV_DIM, 2 * SSD_HEADS,
            2 * DIFF_HEADS * DIFF_QK_DIM, 2 * DIFF_HEADS * DIFF_QK_DIM, DIFF_WIDTH)
IN_WIDTH = sum(IN_SIZES)
IN_SPLITS = tuple(int(s) for s in np.cumsum(IN_SIZES)[:-1])

SCAN_CHUNK = 128
Q_BLOCK = 128
ROPE_BASE = 10000.0
D_FF = 11008
N_EXPERTS = 8
TOP_K = 2
D_FF_EXPERT = D_MODEL
MOE_BLOCK = 256
NORM_EPS = 1e-6
ADA_SCALE = 0.5

kernel_name = 'hybrid_retention_ssd_diffattn_moe_dit'


def rmsnorm(x, w):
    xf = x.astype(jnp.float32)
    xf = xf * lax.rsqrt(jnp.mean(xf * xf, axis=-1, keepdims=True) + NORM_EPS)
    return xf.astype(x.dtype) * w


def modulate(h, shift, scale):
    return h * (1 + scale) + shift


def rope_2d_tables(seq, head_dim):
    rows = seq // GRID_W
    r = jnp.broadcast_to(jnp.arange(rows, dtype=jnp.float32)[:, None], (rows, GRID_W)).reshape(seq)
    col = jnp.broadcast_to(jnp.arange(GRID_W, dtype=jnp.float32)[None, :], (rows, GRID_W)).reshape(seq)
    n_freq = head_dim // 4
    inv_freq = ROPE_BASE ** (-jnp.arange(n_freq, dtype=jnp.float32) / n_freq)
    ang_r = r[:, None] * inv_freq
    ang_c = col[:, None] * inv_freq
    ang = jnp.concatenate([ang_r, ang_r, ang_c, ang_c], axis=-1)
    return jnp.cos(ang), jnp.sin(ang)


def apply_rope_2d(t, cos, sin):
    a1, a2, b1, b2 = jnp.split(t, 4, axis=-1)
    rot = jnp.concatenate([-a2, a1, -b2, b1], axis=-1)
    return t * cos[:, None, :].astype(t.dtype) + rot * sin[:, None, :].astype(t.dtype)


def chunked_linear_scan(q, k, v, log_a, s0):
    bsz, seq, g, n = q.shape
    r, p = v.shape[-2:]
    nc = seq // SCAN_CHUNK

    def chunks(t):
        t = t.astype(jnp.float32)
        return jnp.moveaxis(t.reshape((bsz, nc, SCAN_CHUNK) + t.shape[2:]), 1, 0)

    lower = jnp.tril(jnp.ones((SCAN_CHUNK, SCAN_CHUNK), dtype=bool))[None, :, :, None, None]

    def step(s, inp):
        qc, kc, vc, ac = inp
        cs = jnp.cumsum(ac, axis=1)
        seg = cs[:, :, None] - cs[:, None, :]
        decay = jnp.exp(jnp.where(lower, seg, -jnp.inf))
        scores = jnp.einsum('bign,bjgn->bijg', qc, kc)
        y = jnp.einsum('bijg,bijgr,bjgrp->bigrp', scores, decay, vc)
        y = y + jnp.einsum('bign,bgrnp->bigrp', qc, s) * jnp.exp(cs)[..., None]
        tail = jnp.exp(cs[:, -1:] - cs)
        s = jnp.exp(cs[:, -1])[..., None, None] * s + jnp.einsum('bjgn,bjgr,bjgrp->bgrnp', kc, tail, vc)
        return s, y

    s, y = lax.scan(step, s0.astype(jnp.float32), (chunks(q), chunks(k), chunks(v), chunks(log_a)))
    y = jnp.moveaxis(y, 0, 1).reshape(bsz, seq, g, r, p)
    return y, s


def bidir_scan(q, k, v_fb, a_fb, qc, kc, vc_fb, ac_fb):
    flip = lambda t: jnp.flip(t, axis=1)
    bsz, _, g, n = q.shape
    r, p = v_fb[0].shape[-2:]
    s0 = jnp.zeros((bsz, g, r, n, p), jnp.float32)
    yc_f, s_f = chunked_linear_scan(qc, kc, vc_fb[0], ac_fb[0], s0)
    yc_b, s_b = chunked_linear_scan(flip(qc), flip(kc), flip(vc_fb[1]), flip(ac_fb[1]), s0)
    y_f, _ = chunked_linear_scan(q, k, v_fb[0], a_fb[0], s_f)
    y_b, _ = chunked_linear_scan(flip(q), flip(k), flip(v_fb[1]), flip(a_fb[1]), s_b)
    return y_f + flip(y_b), yc_f + flip(yc_b)


def depthwise_conv_centred(x, w, b):
    kw, ch = w.shape
    y = lax.conv_general_dilated(x, w[:, None, :].astype(x.dtype), window_strides=(1,),
                                 padding=[(kw // 2, kw // 2)],
                                 dimension_numbers=('NWC', 'WIO', 'NWC'),
                                 feature_group_count=ch)
    return y + b


def retention_group(proj, proj_c, rope, decay_logit, norm_w, need_ctx_out):
    q, k, v, g = proj
    qc, kc, vc, gc = proj_c
    cos, sin = rope
    bsz, seq, _ = q.shape
    n_ctx = qc.shape[1]
    heads = lambda t: t.reshape(t.shape[0], t.shape[1], RET_HEADS, RET_HEAD_DIM)
    k_scale = RET_HEAD_DIM ** -0.5
    q = apply_rope_2d(heads(q), cos, sin)
    k = apply_rope_2d(heads(k), cos, sin) * k_scale
    qc = heads(qc)
    kc = heads(kc) * k_scale
    v = heads(v)[:, :, :, None, :]
    vc = heads(vc)[:, :, :, None, :]
    log_gamma = jax.nn.log_sigmoid(decay_logit.astype(jnp.float32))

    def decay(n, d):
        return jnp.broadcast_to(log_gamma[d][None, None, :, None], (bsz, n, RET_HEADS, 1))

    y, yc = bidir_scan(q, k, (v, v), (decay(seq, 0), decay(seq, 1)),
                       qc, kc, (vc, vc), (decay(n_ctx, 0), decay(n_ctx, 1)))

    def finish(y, gate):
        y = y[..., 0, :]
        y = y * lax.rsqrt(jnp.mean(y * y, axis=-1, keepdims=True) + NORM_EPS)
        y = y.reshape(y.shape[0], y.shape[1], RET_WIDTH).astype(gate.dtype) * norm_w
        return jax.nn.silu(gate) * y

    return finish(y, g), (finish(yc, gc) if need_ctx_out else None)


def ssd_group(proj, proj_c, conv_w, conv_b, dt_bias, a_log, d_skip, norm_w, need_ctx_out):
    a_neg = -jnp.exp(a_log.astype(jnp.float32)).reshape(2, SSD_GROUPS, SSD_HEADS_PER_GROUP)
    dt_b = dt_bias.astype(jnp.float32).reshape(2, SSD_GROUPS, SSD_HEADS_PER_GROUP)

    def prepare(xbc, dt):
        bsz, n, _ = xbc.shape
        xbc = jax.nn.silu(depthwise_conv_centred(xbc, conv_w, conv_b))
        xs, bm, cm = jnp.split(xbc, [SSD_WIDTH, SSD_WIDTH + SSD_GROUPS * SSD_STATE], axis=-1)
        xs = xs.reshape(bsz, n, SSD_GROUPS, SSD_HEADS_PER_GROUP, SSD_HEAD_DIM)
        bm = bm.reshape(bsz, n, SSD_GROUPS, SSD_STATE)
        cm = cm.reshape(bsz, n, SSD_GROUPS, SSD_STATE)
        dt = jax.nn.softplus(dt.astype(jnp.float32).reshape(bsz, n, 2, SSD_GROUPS, SSD_HEADS_PER_GROUP) + dt_b)
        xf = xs.astype(jnp.float32)
        v_fb = (xf * dt[:, :, 0, :, :, None], xf * dt[:, :, 1, :, :, None])
        a_fb = (dt[:, :, 0] * a_neg[0], dt[:, :, 1] * a_neg[1])
        return xf, bm, cm, v_fb, a_fb

    z, xbc, dt = proj
    zc, xbcc, dtc = proj_c
    xs, bm, cm, v_fb, a_fb = prepare(xbc, dt)
    xsc, bmc, cmc, vc_fb, ac_fb = prepare(xbcc, dtc)
    y, yc = bidir_scan(cm, bm, v_fb, a_fb, cmc, bmc, vc_fb, ac_fb)
    d = d_skip.astype(jnp.float32).reshape(SSD_GROUPS, SSD_HEADS_PER_GROUP, 1)

    def finish(y, xs, z):
        bsz, n = z.shape[:2]
        y = (y + d * xs).reshape(bsz, n, SSD_WIDTH) * jax.nn.silu(z.astype(jnp.float32))
        y = y.reshape(bsz, n, SSD_GROUPS, SSD_WIDTH // SSD_GROUPS)
        y = y * lax.rsqrt(jnp.mean(y * y, axis=-1, keepdims=True) + NORM_EPS)
        return y.reshape(bsz, n, SSD_WIDTH).astype(z.dtype) * norm_w

    return finish(y, xs, z), (finish(yc, xsc, zc) if need_ctx_out else None)


def diff_attention_group(proj, proj_c, rope, lam_vec, norm_w, lambda_init, need_ctx_out):
    q, k, v = proj
    qc, kc, vc = proj_c
    cos, sin = rope
    bsz, seq, _ = q.shape
    pair = lambda t: t.reshape(t.shape[0], t.shape[1], DIFF_HEADS, 2, DIFF_QK_DIM)
    heads = lambda t: t.reshape(t.shape[0], t.shape[1], DIFF_HEADS, DIFF_V_DIM)
    q, k, qc, kc = pair(q), pair(k), pair(qc), pair(kc)
    q1 = apply_rope_2d(q[:, :, :, 0], cos, sin)
    q2 = apply_rope_2d(q[:, :, :, 1], cos, sin)
    k_all1 = jnp.concatenate([apply_rope_2d(k[:, :, :, 0], cos, sin), kc[:, :, :, 0]], axis=1)
    k_all2 = jnp.concatenate([apply_rope_2d(k[:, :, :, 1], cos, sin), kc[:, :, :, 1]], axis=1)
    vc_h = heads(vc)
    v_all = jnp.concatenate([heads(v), vc_h], axis=1)
    lv = lam_vec.astype(jnp.float32)
    lam = jnp.exp(jnp.sum(lv[0] * lv[1])) - jnp.exp(jnp.sum(lv[2] * lv[3])) + lambda_init
    scale = DIFF_QK_DIM ** -0.5

    def attend(qa, qb, ka, kb, vv):
        s1 = jnp.einsum('bqhd,bkhd->bhqk', qa, ka, preferred_element_type=jnp.float32) * scale
        s2 = jnp.einsum('bqhd,bkhd->bhqk', qb, kb, preferred_element_type=jnp.float32) * scale
        w = jax.nn.softmax(s1, axis=-1) - lam * jax.nn.softmax(s2, axis=-1)
        return jnp.einsum('bhqk,bkhd->bqhd', w.astype(vv.dtype), vv)

    def finish(o):
        of = o.astype(jnp.float32)
        of = of * lax.rsqrt(jnp.mean(of * of, axis=-1, keepdims=True) + NORM_EPS)
        out = of.astype(o.dtype) * norm_w * (1.0 - lambda_init)
        return out.reshape(o.shape[0], o.shape[1], DIFF_WIDTH)

    nb = seq // Q_BLOCK
    blocks = lambda t: jnp.moveaxis(t.reshape(bsz, nb, Q_BLOCK, DIFF_HEADS, DIFF_QK_DIM), 1, 0)
    o = lax.map(lambda qq: attend(qq[0], qq[1], k_all1, k_all2, v_all), (blocks(q1), blocks(q2)))
    o = jnp.moveaxis(o, 0, 1).reshape(bsz, seq, DIFF_HEADS, DIFF_V_DIM)
    if need_ctx_out:
        out_c = finish(attend(qc[:, :, :, 0], qc[:, :, :, 1], kc[:, :, :, 0], kc[:, :, :, 1], vc_h))
    else:
        out_c = None
    return finish(o), out_c


def hybrid_mixer(h, hc, w_in, w_out, ret_decay_logit, ret_norm_w, ssd_conv_w, ssd_conv_b,
                 ssd_dt_bias, ssd_a_log, ssd_d, ssd_norm_w, diff_lambda, diff_norm_w,
                 rope_ret, rope_diff, lambda_init, need_ctx_out):
    parts = jnp.split(h @ w_in, IN_SPLITS, axis=-1)
    parts_c = jnp.split(hc @ w_in, IN_SPLITS, axis=-1)
    r_l, r_c = retention_group(parts[0:4], parts_c[0:4], rope_ret, ret_decay_logit, ret_norm_w, need_ctx_out)
    s_l, s_c = ssd_group(parts[4:7], parts_c[4:7], ssd_conv_w, ssd_conv_b, ssd_dt_bias, ssd_a_log,
                         ssd_d, ssd_norm_w, need_ctx_out)
    d_l, d_c = diff_attention_group(parts[7:10], parts_c[7:10], rope_diff, diff_lambda, diff_norm_w,
                                    lambda_init, need_ctx_out)
    y = jnp.concatenate([r_l, s_l, d_l], axis=-1) @ w_out
    yc = jnp.concatenate([r_c, s_c, d_c], axis=-1) @ w_out if need_ctx_out else None
    return y, yc


def swiglu(h, w_gate, w_up, w_down):
    return (jax.nn.silu(h @ w_gate) * (h @ w_up)) @ w_down


def moe_swiglu(h, router_w, w_gate, w_up, w_down):
    n_tok, d = h.shape
    n_exp = router_w.shape[-1]
    n_assign = n_tok * TOP_K
    logits = jnp.matmul(h, router_w, preferred_element_type=jnp.float32)
    top_logits, top_idx = lax.top_k(logits, TOP_K)
    gates = jax.nn.softmax(top_logits, axis=-1)
    expert = top_idx.reshape(n_assign)
    order = jnp.argsort(expert)
    expert_s = expert[order]
    token_s = order // TOP_K
    gate_s = gates.reshape(n_assign)[order]
    counts = jnp.bincount(expert, length=n_exp)
    starts = jnp.cumsum(counts) - counts
    padded = (counts + MOE_BLOCK - 1) // MOE_BLOCK * MOE_BLOCK
    pad_ends = jnp.cumsum(padded)
    pad_starts = pad_ends - padded
    dest = pad_starts[expert_s] + jnp.arange(n_assign) - starts[expert_s]
    n_blocks = -(-n_assign // MOE_BLOCK) + n_exp
    rows = jnp.zeros((n_blocks * MOE_BLOCK, d), h.dtype).at[dest].set(h[token_s])
    block_expert = jnp.minimum(jnp.searchsorted(pad_ends, jnp.arange(n_blocks) * MOE_BLOCK, side='right'), n_exp - 1)

    def expert_block(args):
        xb, e = args
        return swiglu(xb, w_gate[e], w_up[e], w_down[e])

    y_rows = lax.map(expert_block, (rows.reshape(n_blocks, MOE_BLOCK, d), block_expert))
    y_rows = y_rows.reshape(n_blocks * MOE_BLOCK, d)
    return jnp.zeros((n_tok, d), h.dtype).at[token_s].add(gate_s[:, None].astype(h.dtype) * y_rows[dest])


def setup_inputs(seed: int = 0) -> dict:
    key = jax.random.key(seed)
    ks = jax.random.split(key, 32)
    f32 = jnp.float32
    nrm = lambda k, shape, s: jax.random.normal(k, shape, f32) * s
    n_dense = (DEPTH + 1) // 2
    n_moe = DEPTH // 2
    x = nrm(ks[0], (BATCH, SEQ, D_MODEL), 1.0)
    c = nrm(ks[1], (BATCH, D_MODEL), 1.0)
    ctx = nrm(ks[2], (BATCH, CTX_LEN, D_MODEL), 1.0)
    c_ctx = nrm(ks[3], (D_MODEL,), 1.0)
    ada_w = nrm(ks[4], (DEPTH, D_MODEL, 6 * D_MODEL), ADA_SCALE * D_MODEL ** -0.5)
    ada_b = nrm(ks[5], (DEPTH, 6 * D_MODEL), 0.02)
    norm_mix_w = 1.0 + nrm(ks[6], (DEPTH, D_MODEL), 0.02)
    norm_ffn_w = 1.0 + nrm(ks[7], (DEPTH, D_MODEL), 0.02)
    w_in = nrm(ks[8], (DEPTH, D_MODEL, IN_WIDTH), D_MODEL ** -0.5)
    w_out = nrm(ks[9], (DEPTH, MIX_WIDTH, D_MODEL), MIX_WIDTH ** -0.5)
    base_logit = jnp.log(2.0 ** (5.0 + jnp.arange(RET_HEADS, dtype=f32)) - 1.0)
    ret_decay_logit = base_logit + nrm(ks[10], (DEPTH, 2, RET_HEADS), 0.1)
    ret_norm_w = 1.0 + nrm(ks[11], (DEPTH, RET_WIDTH), 0.02)
    ssd_conv_w = nrm(ks[12], (DEPTH, SSD_CONV, SSD_CONV_DIM), SSD_CONV ** -0.5)
    ssd_conv_b = nrm(ks[13], (DEPTH, SSD_CONV_DIM), 0.02)
    u = jax.random.uniform(ks[14], (DEPTH, 2, SSD_HEADS), f32)
    dt0 = jnp.exp(u * (math.log(0.1) - math.log(0.001)) + math.log(0.001))
    ssd_dt_bias = dt0 + jnp.log(-jnp.expm1(-dt0))
    ssd_a_log = jnp.log(jax.random.uniform(ks[15], (DEPTH, 2, SSD_HEADS), f32, 1.0, 16.0))
    ssd_d = 1.0 + nrm(ks[16], (DEPTH, SSD_HEADS), 0.02)
    ssd_norm_w = 1.0 + nrm(ks[17], (DEPTH, SSD_WIDTH), 0.02)
    diff_lambda = nrm(ks[18], (DEPTH, 4, DIFF_QK_DIM), 0.1)
    diff_norm_w = 1.0 + nrm(ks[19], (DEPTH, DIFF_V_DIM), 0.02)
    dense_w_gate = nrm(ks[20], (n_dense, D_MODEL, D_FF), D_MODEL ** -0.5)
    dense_w_up = nrm(ks[21], (n_dense, D_MODEL, D_FF), D_MODEL ** -0.5)
    dense_w_down = nrm(ks[22], (n_dense, D_FF, D_MODEL), D_FF ** -0.5)
    moe_router = nrm(ks[23], (n_moe, D_MODEL, N_EXPERTS), D_MODEL ** -0.5)
    moe_w_gate = nrm(ks[24], (n_moe, N_EXPERTS, D_MODEL, D_FF_EXPERT), D_MODEL ** -0.5)
    moe_w_up = nrm(ks[25], (n_moe, N_EXPERTS, D_MODEL, D_FF_EXPERT), D_MODEL ** -0.5)
    moe_w_down = nrm(ks[26], (n_moe, N_EXPERTS, D_FF_EXPERT, D_MODEL), D_FF_EXPERT ** -0.5)
    final_norm_w = 1.0 + nrm(ks[27], (D_MODEL,), 0.02)
    return {'x': x, 'c': c, 'ctx': ctx, 'c_ctx': c_ctx, 'ada_w': ada_w, 'ada_b': ada_b,
            'norm_mix_w': norm_mix_w, 'norm_ffn_w': norm_ffn_w, 'w_in': w_in, 'w_out': w_out,
            'ret_decay_logit': ret_decay_logit, 'ret_norm_w': ret_norm_w,
            'ssd_conv_w': ssd_conv_w, 'ssd_conv_b': ssd_conv_b, 'ssd_dt_bias': ssd_dt_bias,
            'ssd_a_log': ssd_a_log, 'ssd_d': ssd_d, 'ssd_norm_w': ssd_norm_w,
            'diff_lambda': diff_lambda, 'diff_norm_w': diff_norm_w,
            'dense_w_gate': dense_w_gate, 'dense_w_up': dense_w_up, 'dense_w_down': dense_w_down,
            'moe_router': moe_router, 'moe_w_gate': moe_w_gate, 'moe_w_up': moe_w_up,
            'moe_w_down': moe_w_down, 'final_norm_w': final_norm_w}


def reference(x, c, ctx, c_ctx, ada_w, ada_b, norm_mix_w, norm_ffn_w, w_in, w_out,
              ret_decay_logit, ret_norm_w, ssd_conv_w, ssd_conv_b, ssd_dt_bias, ssd_a_log,
              ssd_d, ssd_norm_w, diff_lambda, diff_norm_w, dense_w_gate, dense_w_up,
              dense_w_down, moe_router, moe_w_gate, moe_w_up, moe_w_down, final_norm_w):
    seq = x.shape[1]
    n_ctx = ctx.shape[1]
    rope_ret = rope_2d_tables(seq, RET_HEAD_DIM)
    rope_diff = rope_2d_tables(seq, DIFF_QK_DIM)
    xc = ctx
    for layer in range(DEPTH):
        last = layer == DEPTH - 1
        lambda_init = 0.8 - 0.6 * math.exp(-0.3 * layer)
        mod = jax.nn.silu(c) @ ada_w[layer] + ada_b[layer]
        mod_c = jax.nn.silu(c_ctx) @ ada_w[layer] + ada_b[layer]
        shift_m, scale_m, gate_m, shift_f, scale_f, gate_f = jnp.split(mod[:, None, :], 6, axis=-1)
        shift_mc, scale_mc, gate_mc, shift_fc, scale_fc, gate_fc = jnp.split(mod_c, 6)
        h = modulate(rmsnorm(x, norm_mix_w[layer]), shift_m, scale_m)
        hc = modulate(rmsnorm(xc, norm_mix_w[layer]), shift_mc, scale_mc)
        y, yc = hybrid_mixer(h, hc, w_in[layer], w_out[layer], ret_decay_logit[layer], ret_norm_w[layer],
                             ssd_conv_w[layer], ssd_conv_b[layer], ssd_dt_bias[layer], ssd_a_log[layer],
                             ssd_d[layer], ssd_norm_w[layer], diff_lambda[layer], diff_norm_w[layer],
                             rope_ret, rope_diff, lambda_init, not last)
        x = x + gate_m * y
        h = modulate(rmsnorm(x, norm_ffn_w[layer]), shift_f, scale_f)
        if not last:
            xc = xc + gate_mc * yc
            hc = modulate(rmsnorm(xc, norm_ffn_w[layer]), shift_fc, scale_fc)
            h = jnp.concatenate([hc, h], axis=1)
        if layer % 2 == 0:
            i = layer // 2
            f = swiglu(h, dense_w_gate[i], dense_w_up[i], dense_w_down[i])
        else:
            i = layer // 2
            f = moe_swiglu(h.reshape(-1, D_MODEL), moe_router[i], moe_w_gate[i], moe_w_up[i],
                           moe_w_down[i]).reshape(h.shape)
        if not last:
            xc = xc + gate_fc * f[:, :n_ctx]
        x = x + gate_f * f[:, f.shape[1] - seq:]
    return rmsnorm(x, final_norm_w)
```

```python
import contextlib
import math
import numpy as np
import concourse.bass as bass
import concourse.mybir as mybir
from concourse.bass_utils import run_bass_kernel_spmd

F32 = mybir.dt.float32
BF16 = mybir.dt.bfloat16
AF = mybir.ActivationFunctionType
ALU = mybir.AluOpType
AX = mybir.AxisListType
EPS = 1e-6


class Buf:
    __slots__ = ("ap", "name", "w", "r")

    def __init__(self, ap=None, name=""):
        self.ap = ap
        self.name = name
        self.w = None
        self.r = {}


class Sched:
    ENG = ("pe", "act", "dve", "pool", "sp")

    def __init__(self, nc, same_engine_sync=True, ring=12):
        self.nc = nc
        self.engs = {"pe": nc.tensor, "act": nc.scalar, "dve": nc.vector, "pool": nc.gpsimd, "sp": nc.sync}
        self.semh = {}
        self.cnt = {}
        self.seen = {e: {} for e in self.ENG}
        self.same = same_engine_sync
        for e in self.ENG:
            self.semh[e] = nc.alloc_semaphore(name=f"s_{e}")
            self.cnt[e] = 0
        self.ring = ring
        self.dma_i = {}
        self.dma_val = {}
        for q in ("sp", "pool"):
            self.dma_i[q] = 0
            for s in range(ring):
                k = (q, s)
                self.semh[k] = nc.alloc_semaphore(name=f"d_{q}{s}")
                self.dma_val[k] = 0
        self.n_ins = 0
        self.n_wait = 0
        self.semh["cc"] = nc.alloc_semaphore(name="s_cc")
        self.cc_val = 0

    def cc_allgather(self, in_ap, out_ap, groups, reads=(), writes=()):
        self._deps("pool", reads, writes)
        ins = self.engs["pool"].collective_compute("AllGather", ALU.bypass, replica_groups=groups, ins=[in_ap], outs=[out_ap])
        self.cc_val += 1
        ins.then_inc(self.semh["cc"], 1)
        self.n_ins += 1
        self._record(("cc", self.cc_val), reads, writes)
        return ins

    def idma(self, out_ap, in_ap, idx_ap, reads=(), writes=()):
        q = "pool"
        self._deps(q, reads, writes)
        slot = self.dma_i[q] % self.ring
        self.dma_i[q] += 1
        k = (q, slot)
        prev = self.dma_val[k]
        if prev > 0:
            self._wait(q, (k, prev))
        self.dma_val[k] = prev + 16
        ins = self.engs[q].indirect_dma_start(out=out_ap, out_offset=None, in_=in_ap, in_offset=bass.IndirectOffsetOnAxis(ap=idx_ap, axis=0))
        ins.then_inc(self.semh[k], 16)
        self.n_ins += 1
        self._record((k, prev + 16), reads, writes)
        return ins

    def _wait(self, eng, ev):
        if ev is None:
            return
        key, val = ev
        if key == eng and (eng == "pe" or not self.same):
            return
        if self.seen[eng].get(key, 0) >= val:
            return
        self.engs[eng].wait_ge(self.semh[key], val)
        self.seen[eng][key] = val
        self.n_wait += 1

    def _deps(self, eng, reads, writes):
        for b in reads:
            self._wait(eng, b.w)
        for b in writes:
            self._wait(eng, b.w)
            for k, v in list(b.r.items()):
                self._wait(eng, (k, v))

    def _record(self, ev, reads, writes):
        k, v = ev
        for b in reads:
            if b.r.get(k, 0) < v:
                b.r[k] = v
        for b in writes:
            b.w = ev
            b.r = {}

    def op(self, eng, fn, reads=(), writes=(), signal=True):
        self._deps(eng, reads, writes)
        ins = fn(self.engs[eng])
        self.n_ins += 1
        if signal:
            self.cnt[eng] += 1
            ins.then_inc(self.semh[eng], 1)
            ev = (eng, self.cnt[eng])
        else:
            ev = (eng, self.cnt[eng] + 1)
        self._record(ev, reads, writes)
        return ins

    def dma(self, q, out_ap, in_ap, reads=(), writes=()):
        self._deps(q, reads, writes)
        slot = self.dma_i[q] % self.ring
        self.dma_i[q] += 1
        k = (q, slot)
        prev = self.dma_val[k]
        if prev > 0:
            self._wait(q, (k, prev))
        self.dma_val[k] = prev + 16
        ins = self.engs[q].dma_start(out=out_ap, in_=in_ap)
        ins.then_inc(self.semh[k], 16)
        self.n_ins += 1
        self._record((k, prev + 16), reads, writes)
        return ins

    def barrier(self):
        evs = [(e, self.cnt[e]) for e in self.ENG if self.cnt[e] > 0]
        evs += [(k, v) for k, v in self.dma_val.items() if v > 0]
        if self.cc_val > 0:
            evs.append(("cc", self.cc_val))
        for e in self.ENG:
            for ev in evs:
                if ev[0] == e and e == "pe":
                    continue
                key, val = ev
                if self.seen[e].get(key, 0) >= val:
                    continue
                self.engs[e].wait_ge(self.semh[key], val)
                self.seen[e][key] = val

    def act(self, out, in_, func, reads, writes, **kw):
        return self.op("act", lambda e: e.activation(out=out, in_=in_, func=func, **kw), reads, writes)

    def tt(self, eng, out, in0, in1, op, reads, writes):
        return self.op(eng, lambda e: e.tensor_tensor(out=out, in0=in0, in1=in1, op=op), reads, writes)

    def ts(self, eng, out, in0, s1, s2, op0, op1, reads, writes):
        if op1 is None:
            return self.op(eng, lambda e: e.tensor_scalar(out=out, in0=in0, scalar1=s1, scalar2=None, op0=op0), reads, writes)
        return self.op(eng, lambda e: e.tensor_scalar(out=out, in0=in0, scalar1=s1, scalar2=s2, op0=op0, op1=op1), reads, writes)

    def stt(self, out, in0, scalar, in1, op0, op1, reads, writes):
        return self.op("dve", lambda e: e.scalar_tensor_tensor(out=out, in0=in0, scalar=scalar, in1=in1, op0=op0, op1=op1), reads, writes)

    def mm(self, out, lhsT, rhs, reads, writes, start=True, stop=True, signal=True):
        return self.op("pe", lambda e: e.matmul(out, lhsT, rhs, start=start, stop=stop), reads, writes, signal=signal)

    def tr(self, out, in_, ident, reads, writes):
        return self.op("pe", lambda e: e.transpose(out, in_, ident), reads, writes)

    def copy(self, eng, out, in_, reads, writes):
        if eng == "act":
            return self.act(out, in_, AF.Copy, reads, writes)
        return self.op(eng, lambda e: e.tensor_copy(out=out, in_=in_), reads, writes)


class Cfg:
    def __init__(s, D, SEQ, CTX, NQ, DFF, DFE, NEXP=8, B=2):
        s.D, s.SEQ, s.CTX, s.NQ, s.DFF, s.DFE, s.NEXP, s.B = D, SEQ, CTX, NQ, DFF, DFE, NEXP, B
        s.KD = D // 128
        s.T = CTX + SEQ
        s.NCH = s.T // 128
        s.NCC = CTX // 128
        s.MIX = 1024 * NQ
        s.PC = 3344
        assert CTX % 128 == 0 and CTX <= 512 and SEQ % 512 == 0
        s.tiles = [(0, CTX)] + [(CTX + 512 * i, 512) for i in range(SEQ // 512)]
        s.GRID_W = 64


FULL = Cfg(4096, 8192, 256, 4, 11008, 4096)


def rope_tables(cfg, dh):
    seq = cfg.SEQ
    rows = seq // cfg.GRID_W
    r = np.repeat(np.arange(rows, dtype=np.float32), cfg.GRID_W)
    col = np.tile(np.arange(cfg.GRID_W, dtype=np.float32), rows)
    nf = dh // 4
    inv = (10000.0 ** (-np.arange(nf, dtype=np.float32) / nf)).astype(np.float32)
    ar = r[:, None] * inv
    ac = col[:, None] * inv
    ang = np.concatenate([ar, ar, ac, ac], axis=-1)
    cos = np.cos(ang).astype(np.float32)
    sin = np.sin(ang).astype(np.float32)
    cosT = np.concatenate([np.ones((dh, cfg.CTX), np.float32), cos.T], axis=1)
    sinT = np.concatenate([np.zeros((dh, cfg.CTX), np.float32), sin.T], axis=1)
    reps = 128 // dh
    return np.ascontiguousarray(np.tile(cosT, (reps, 1))), np.ascontiguousarray(np.tile(sinT, (reps, 1)))


def rot_matrix(dh):
    R = np.zeros((128, 128), np.float32)
    q = dh // 4
    for base in range(0, 128, dh):
        for m in range(dh):
            quarter = m // q
            if quarter % 2 == 0:
                R[base + m + q, base + m] = -1.0
            else:
                R[base + m - q, base + m] = 1.0
    return R


def const_pack():
    j = np.arange(128)[:, None].astype(np.float32)
    i = np.arange(128)[None, :].astype(np.float32)
    mats = {}
    mats["ident"] = np.eye(128, dtype=np.float32)
    mats["ones"] = np.ones((128, 128), np.float32)
    mats["dpos"] = np.maximum(i - j, 0.0)
    mats["ufs"] = (i > j).astype(np.float32)
    mats["dneg"] = np.maximum(j - i, 0.0)
    mats["ubs"] = (j > i).astype(np.float32)
    mats["i2"] = 2.0 * np.eye(128, dtype=np.float32)
    mats["uf"] = (j <= i).astype(np.float32)
    mats["ub"] = (j >= i).astype(np.float32)
    mats["sl"] = (j > i).astype(np.float32)
    mats["su"] = (j < i).astype(np.float32)
    mats["rowip1"] = np.broadcast_to(i + 1.0, (128, 128)).copy()
    mats["row128mi"] = np.broadcast_to(128.0 - i, (128, 128)).copy()
    mats["rot64"] = rot_matrix(64)
    mats["rot128"] = rot_matrix(128)
    cols = np.zeros((128, 128), np.float32)
    cols[:, 0] = 127.0 - np.arange(128)
    cols[:, 1] = np.arange(128)
    mats["cols"] = cols
    names = list(mats.keys())
    arr = np.stack([mats[n] for n in names], axis=1)
    return names, np.ascontiguousarray(arr)


CNAMES, CPACK = const_pack()
CI = {n: k for k, n in enumerate(CNAMES)}


def emit_k1(nc, S, ps, cfg, last, lam_init, I, pf, load_x, store_mix, d_mix):
    D, KD, T, NCH, NCC, CTX = cfg.D, cfg.KD, cfg.T, cfg.NCH, cfg.NCC, cfg.CTX
    tiles = cfg.tiles
    w_in, ada_w, ada_b, ccol, nmw, consts = I["w_in"], I["ada_w"], I["ada_b"], I["ccol"], I["nmw"], I["consts"]
    cosd, sind, cosr, sinr = I["cosd"], I["sind"], I["cosr"], I["sinr"]
    rdl, rnw, cw, cb, dtb, alog, dsk, snw, dlam, dnw = (I[k] for k in ("rdl", "rnw", "cw", "cb", "dtb", "alog", "dsk", "snw", "dlam", "dnw"))
    hT, projT, convT = I["hT"], I["projT"], I["convT"]
    d_hT = Buf(None, "d_hT"); d_proj = Buf(None, "d_proj"); d_conv = Buf(None, "d_conv")

    with contextlib.ExitStack() as es0:
        def SB(es, n, sh, dt=F32):
            return Buf(es.enter_context(nc.sbuf_tensor(pf + n, sh, dt)), n)
        C = SB(es0, "C", [128, len(CNAMES), 128])
        Cb = SB(es0, "Cb", [128, 2, 128], BF16)
        S.dma("sp", C.ap[:], consts, writes=[C])
        S.copy("dve", Cb.ap[:, 0, :], C.ap[:, CI["ones"], :], [C], [Cb])
        S.copy("dve", Cb.ap[:, 1, :], C.ap[:, CI["ident"], :], [C], [Cb])
        cm = lambda n: C.ap[:, CI[n], :]
        ones_bf = Cb.ap[:, 0, :]
        AB = SB(es0, "AB", [128, KD, 4])

        with contextlib.ExitStack() as es:
            sc = SB(es, "sc", [128, KD, 2]); adb = SB(es, "adb", [128, 2 * KD]); nm = SB(es, "nm", [128, KD])
            modc = SB(es, "modc", [128, 2 * KD, 2])
            wa = [SB(es, f"wa{i}", [128, KD, 512]) for i in range(2)]
            S.dma("sp", sc.ap[:], ccol, writes=[sc]); S.dma("sp", adb.ap[:], ada_b, writes=[adb]); S.dma("sp", nm.ap[:], nmw, writes=[nm])
            S.act(sc.ap[:], sc.ap[:], AF.Silu, [sc], [sc])
            nfc = 2 * KD
            pm = ps[0]
            for fg in range(0, nfc, 4):
                w = wa[(fg // 4) % 2]
                for kg in range(0, KD, 8):
                    k2 = min(KD, kg + 8)
                    S.dma("sp", w.ap[:, kg:k2, :], ada_w[kg * 128:k2 * 128, fg * 128:(fg + 4) * 128].rearrange("(k p) c -> p k c", p=128), writes=[w])
                for f in range(fg, fg + 4):
                    for k in range(KD):
                        S.mm(pm.ap[:, 2 * f:2 * f + 2], w.ap[:, k, (f - fg) * 128:(f - fg + 1) * 128], sc.ap[:, k, :], [w, sc], [pm],
                             start=(k == 0), stop=(k == KD - 1), signal=(k == KD - 1))
            for j in range(2):
                S.tt("dve", modc.ap[:, :, j], pm.ap[:, 0:2 * nfc].rearrange("p (f j) -> p f j", j=2)[:, :, j], adb.ap[:], ALU.add, [pm, adb], [modc])
            for j in range(2):
                S.stt(AB.ap[:, :, j], modc.ap[:, KD:2 * KD, j], 1.0, nm.ap[:], ALU.add, ALU.mult, [modc, nm], [AB])
                S.copy("dve", AB.ap[:, :, 2 + j], modc.ap[:, 0:KD, j], [modc], [AB])
        S.barrier()

        with contextlib.ExitStack() as es:
            xt = SB(es, "xt", [128, KD, 512]); ht = SB(es, "ht", [128, KD, 512], BF16)
            sq = [SB(es, f"sq{i}", [128, 512], BF16) for i in range(2)]
            tmp = [SB(es, f"tmp{i}", [128, 512]) for i in range(2)]
            rs = SB(es, "rs", [128, 512])
            for (t0, n) in tiles:
                j = 1 if t0 < CTX else 0
                load_x(S, xt, t0, n)
                for k in range(KD):
                    s_ = sq[k % 2]
                    S.act(s_.ap[:, :n], xt.ap[:, k, :n], AF.Square, [xt], [s_])
                    S.mm(ps[0].ap[:, :n], ones_bf, s_.ap[:, :n], [s_, Cb], [ps[0]], start=(k == 0), stop=(k == KD - 1))
                S.act(rs.ap[:, :n], ps[0].ap[:, :n], AF.Ln, [ps[0]], [rs], scale=1.0 / D, bias=EPS)
                S.act(rs.ap[:, :n], rs.ap[:, :n], AF.Exp, [rs], [rs], scale=-0.5)
                for k in range(KD):
                    t_ = tmp[k % 2]
                    S.tt("dve", t_.ap[:, :n], xt.ap[:, k, :n], rs.ap[:, :n], ALU.mult, [xt, rs], [t_])
                    S.act(ht.ap[:, k, :n], t_.ap[:, :n], AF.Identity, [t_, AB], [ht], scale=AB.ap[:, k, j:j + 1], bias=AB.ap[:, k, 2 + j:3 + j])
                for kg in range(0, KD, 8):
                    k2 = min(KD, kg + 8)
                    S.dma("sp", hT[kg * 128:k2 * 128, t0:t0 + n].rearrange("(k p) t -> p k t", p=128), ht.ap[:, kg:k2, :n], reads=[ht], writes=[d_hT])
        S.barrier()

        groups = [(0, 1024), (1024, 1552), (2576, 768)]
        with contextlib.ExitStack() as es:
            wg = SB(es, "wg", [128, KD, 1552], BF16)
            hb = [SB(es, f"hb{i}", [128, KD, 512], BF16) for i in range(2)]
            stg = [SB(es, f"stg{i}", [128, 512]) for i in range(4)]
            si = 0; pi = 0; ti = 0
            for (c0, ncols) in groups:
                for kg in range(0, KD, 8):
                    k2 = min(KD, kg + 8)
                    S.dma("pool", wg.ap[:, kg:k2, :ncols], w_in[kg * 128:k2 * 128, c0:c0 + ncols].rearrange("(k p) c -> p k c", p=128), writes=[wg])
                for (t0, n) in tiles:
                    h = hb[ti % 2]; ti += 1
                    for kg in range(0, KD, 8):
                        k2 = min(KD, kg + 8)
                        S.dma("sp", h.ap[:, kg:k2, :n], hT[kg * 128:k2 * 128, t0:t0 + n].rearrange("(k p) t -> p k t", p=128), reads=[d_hT], writes=[h])
                    cs = 0
                    while cs < ncols:
                        m = min(128, ncols - cs)
                        p = ps[pi % 4]; pi += 1
                        for k in range(KD):
                            S.mm(p.ap[:m, :n], wg.ap[:, k, cs:cs + m], h.ap[:, k, :n], [wg, h], [p], start=(k == 0), stop=(k == KD - 1), signal=(k == KD - 1))
                        st = stg[si % 4]; si += 1
                        S.copy("act" if si % 2 else "dve", st.ap[:m, :n], p.ap[:m, :n], [p], [st])
                        S.dma("sp", projT[c0 + cs:c0 + cs + m, t0:t0 + n], st.ap[:m, :n], reads=[st], writes=[d_proj])
                        cs += m
        S.barrier()

        base_d = 2576
        with contextlib.ExitStack() as es:
            QT = SB(es, "QT", [128, T], BF16); KT = SB(es, "KT", [128, T], BF16); Vtm = SB(es, "Vtm", [128, NCH, 128], BF16)
            ld = [SB(es, f"ld{i}", [128, 512]) for i in range(2)]
            cs_t = SB(es, "cs_t", [128, 512]); sn_t = SB(es, "sn_t", [128, 512])
            t1 = SB(es, "t1", [128, 512]); t2 = SB(es, "t2", [128, 512]); sqb = SB(es, "sqb", [128, 512], BF16)
            P = [SB(es, f"P{i}", [128, 512], BF16) for i in range(4)]
            mx = SB(es, "mx", [128, 8]); negc = SB(es, "negc", [128, 2]); lamt = SB(es, "lamt", [128, 8]); lv = SB(es, "lv", [128, 4, 64])
            nwd = SB(es, "nwd", [128, 1]); rl = [SB(es, f"rl{i}", [128, 512]) for i in range(2)]
            ob = SB(es, "ob", [128, 512]); rsd = SB(es, "rsd", [128, 512]); oo = SB(es, "oo", [128, 512])
            S.dma("sp", lv.ap[:], dlam, writes=[lv]); S.dma("sp", nwd.ap[:], dnw, writes=[nwd])
            S.tt("dve", lv.ap[:, 0, :], lv.ap[:, 0, :], lv.ap[:, 1, :], ALU.mult, [lv], [lv])
            S.tt("dve", lv.ap[:, 2, :], lv.ap[:, 2, :], lv.ap[:, 3, :], ALU.mult, [lv], [lv])
            S.op("dve", lambda e: e.tensor_reduce(out=lamt.ap[:, 0:1], in_=lv.ap[:, 0, :], axis=AX.X, op=ALU.add), [lv], [lamt])
            S.op("dve", lambda e: e.tensor_reduce(out=lamt.ap[:, 1:2], in_=lv.ap[:, 2, :], axis=AX.X, op=ALU.add), [lv], [lamt])
            S.act(lamt.ap[:, 0:2], lamt.ap[:, 0:2], AF.Exp, [lamt], [lamt])
            S.tt("dve", lamt.ap[:, 2:3], lamt.ap[:, 1:2], lamt.ap[:, 0:1], ALU.subtract, [lamt], [lamt])
            S.ts("dve", lamt.ap[:, 3:4], lamt.ap[:, 2:3], -lam_init, None, ALU.add, None, [lamt], [lamt])
            S.ts("dve", nwd.ap[:], nwd.ap[:], 1.0 - lam_init, None, ALU.mult, None, [nwd], [nwd])
            neglam = lamt.ap[:, 3:4]
            for hd in range(2):
                rq = base_d + hd * 128; rk = base_d + 256 + hd * 128; rv = base_d + 512 + hd * 128
                S.op("dve", lambda e: e.memset(mx.ap[:], 0.0), [], [mx])
                li = 0
                for (t0, n) in tiles:
                    S.dma("sp", cs_t.ap[:, :n], cosd[:, t0:t0 + n], writes=[cs_t]); S.dma("sp", sn_t.ap[:, :n], sind[:, t0:t0 + n], writes=[sn_t])
                    for which, row, dst in ((0, rq, QT), (1, rk, KT)):
                        l = ld[li % 2]; li += 1
                        S.dma("sp", l.ap[:, :n], projT[row:row + 128, t0:t0 + n], reads=[d_proj], writes=[l])
                        S.act(sqb.ap[:, :n], l.ap[:, :n], AF.Square, [l], [sqb])
                        for m in range(2):
                            S.mm(ps[m].ap[:, :n], ones_bf[m * 64:(m + 1) * 64, :], sqb.ap[m * 64:(m + 1) * 64, :n], [sqb, Cb], [ps[m]])
                            S.op("dve", lambda e: e.tensor_reduce(out=mx.ap[:, 4 + m:5 + m], in_=ps[m].ap[:, :n], axis=AX.X, op=ALU.max), [ps[m]], [mx])
                            c_ = which * 2 + m
                            S.tt("dve", mx.ap[:, c_:c_ + 1], mx.ap[:, c_:c_ + 1], mx.ap[:, 4 + m:5 + m], ALU.max, [mx], [mx])
                        S.mm(ps[2].ap[:, :n], cm("rot64"), l.ap[:, :n], [C, l], [ps[2]])
                        S.tt("dve", t1.ap[:, :n], l.ap[:, :n], cs_t.ap[:, :n], ALU.mult, [l, cs_t], [t1])
                        S.tt("dve", t2.ap[:, :n], ps[2].ap[:, :n], sn_t.ap[:, :n], ALU.mult, [ps[2], sn_t], [t2])
                        S.tt("pool", dst.ap[:, t0:t0 + n], t1.ap[:, :n], t2.ap[:, :n], ALU.add, [t1, t2], [dst])
                    l = ld[li % 2]; li += 1
                    S.dma("sp", l.ap[:, :n], projT[rv:rv + 128, t0:t0 + n], reads=[d_proj], writes=[l])
                    for cc in range(n // 128):
                        S.tr(ps[3].ap[:, 0:128], l.ap[:, cc * 128:(cc + 1) * 128], cm("ident"), [l, C], [ps[3]])
                        S.copy("act", Vtm.ap[:, t0 // 128 + cc, :], ps[3].ap[:, 0:128], [ps[3]], [Vtm])
                S.tt("dve", mx.ap[:, 6:8], mx.ap[:, 0:2], mx.ap[:, 2:4], ALU.mult, [mx], [mx])
                S.act(mx.ap[:, 6:8], mx.ap[:, 6:8], AF.Ln, [mx], [mx])
                S.act(mx.ap[:, 6:8], mx.ap[:, 6:8], AF.Exp, [mx], [mx], scale=0.5)
                S.ts("dve", negc.ap[:], mx.ap[:, 6:8], -0.125, None, ALU.mult, None, [mx], [negc])
                qtiles = [(t0, n, list(range(NCH))) for (t0, n) in tiles if t0 >= CTX]
                if not last:
                    qtiles = [(0, CTX, list(range(NCC)))] + qtiles
                pj = 0
                for (t0, n, kcs) in qtiles:
                    for i, kc in enumerate(kcs):
                        for m in range(2):
                            pS = ps[pj % 4]; Pm = P[pj % 4]; pj += 1
                            S.mm(pS.ap[:, :n], KT.ap[m * 64:(m + 1) * 64, kc * 128:(kc + 1) * 128], QT.ap[m * 64:(m + 1) * 64, t0:t0 + n], [KT, QT], [pS])
                            S.act(Pm.ap[:, :n], pS.ap[:, :n], AF.Exp, [pS, negc], [Pm], scale=0.125, bias=negc.ap[:, m:m + 1])
                            S.mm(ps[4 + m].ap[:, :n], Vtm.ap[:, kc, :], Pm.ap[:, :n], [Vtm, Pm], [ps[4 + m]], start=(i == 0), stop=(i == len(kcs) - 1), signal=False)
                            S.mm(ps[6 + m].ap[:, :n], ones_bf, Pm.ap[:, :n], [Cb, Pm], [ps[6 + m]], start=(i == 0), stop=(i == len(kcs) - 1))
                    for m in range(2):
                        S.op("dve", lambda e: e.reciprocal(out=rl[m].ap[:, :n], in_=ps[6 + m].ap[:, :n]), [ps[6 + m]], [rl[m]])
                    S.tt("dve", t1.ap[:, :n], ps[4].ap[:, :n], rl[0].ap[:, :n], ALU.mult, [ps[4], rl[0]], [t1])
                    S.tt("dve", t2.ap[:, :n], ps[5].ap[:, :n], rl[1].ap[:, :n], ALU.mult, [ps[5], rl[1]], [t2])
                    S.stt(ob.ap[:, :n], t2.ap[:, :n], neglam, t1.ap[:, :n], ALU.mult, ALU.add, [t1, t2, lamt], [ob])
                    S.act(sqb.ap[:, :n], ob.ap[:, :n], AF.Square, [ob], [sqb])
                    S.mm(ps[0].ap[:, :n], ones_bf, sqb.ap[:, :n], [sqb, Cb], [ps[0]])
                    S.act(rsd.ap[:, :n], ps[0].ap[:, :n], AF.Ln, [ps[0]], [rsd], scale=1.0 / 128, bias=EPS)
                    S.act(rsd.ap[:, :n], rsd.ap[:, :n], AF.Exp, [rsd], [rsd], scale=-0.5)
                    S.stt(oo.ap[:, :n], ob.ap[:, :n], nwd.ap[:, 0:1], rsd.ap[:, :n], ALU.mult, ALU.mult, [ob, nwd, rsd], [oo])
                    store_mix(S, 768 + hd * 128, 128, t0, n, oo.ap[:, :n], oo, d_mix)
        S.barrier()

        ksc = 128.0 ** -0.5
        with contextlib.ExitStack() as es:
            QT = SB(es, "rQT", [128, T], BF16); KT = SB(es, "rKT", [128, T], BF16)
            Vtm = SB(es, "rVtm", [128, NCH, 128], BF16); Kf = SB(es, "Kf", [128, NCH, 128], BF16); Kb = SB(es, "Kb", [128, NCH, 128], BF16)
            Sf = SB(es, "Sf", [128, NCH, 128], BF16); Sbk = SB(es, "Sbk", [128, NCH, 128], BF16)
            ld = [SB(es, f"rld{i}", [128, 512]) for i in range(2)]
            cs_t = SB(es, "rcs", [128, 512]); sn_t = SB(es, "rsn", [128, 512])
            t1 = SB(es, "rt1", [128, 512]); t2 = SB(es, "rt2", [128, 512]); kro = SB(es, "kro", [128, 512])
            lg = SB(es, "lg", [128, 4]); M = SB(es, "M", [128, 128]); M2 = SB(es, "M2", [128, 128]); wfb = SB(es, "wfb", [128, 2])
            G = SB(es, "G", [128, 2, 128]); gT = SB(es, "gT", [128, 2]); nwr = SB(es, "nwr", [128, 2])
            St = SB(es, "St", [128, 128]); AT = SB(es, "AT", [128, 128], BF16)
            Qf = SB(es, "Qf", [128, 128], BF16); Qb = SB(es, "Qb", [128, 128], BF16)
            yt = SB(es, "yt", [128, 512]); sqb = SB(es, "rsqb", [128, 512], BF16); rsd = SB(es, "rrsd", [128, 512])
            gl = SB(es, "gl", [128, 512]); oo = SB(es, "roo", [128, 512])
            S.dma("sp", lg.ap[:], rdl, writes=[lg]); S.dma("sp", nwr.ap[:], rnw, writes=[nwr])
            S.act(lg.ap[:], lg.ap[:], AF.Exp, [lg], [lg], scale=-1.0)
            S.act(lg.ap[:], lg.ap[:], AF.Ln, [lg], [lg], bias=1.0)
            S.ts("dve", lg.ap[:], lg.ap[:], -1.0, None, ALU.mult, None, [lg], [lg])
            for hr in range(2):
                lgf = lg.ap[:, hr:hr + 1]; lgb = lg.ap[:, 2 + hr:3 + hr]
                rq = hr * 128; rk = 256 + hr * 128; rv = 512 + hr * 128; rg = 768 + hr * 128
                S.act(M.ap[:], cm("dpos"), AF.Exp, [C, lg], [M], scale=lgf)
                S.tt("dve", M.ap[:], M.ap[:], cm("ufs"), ALU.mult, [M, C], [M])
                S.act(M2.ap[:], cm("dneg"), AF.Exp, [C, lg], [M2], scale=lgb)
                S.tt("dve", M2.ap[:], M2.ap[:], cm("ubs"), ALU.mult, [M2, C], [M2])
                S.tt("dve", M.ap[:], M.ap[:], M2.ap[:], ALU.add, [M, M2], [M])
                S.tt("dve", M.ap[:], M.ap[:], cm("i2"), ALU.add, [M, C], [M])
                S.ts("dve", M.ap[:], M.ap[:], ksc, None, ALU.mult, None, [M], [M])
                S.act(wfb.ap[:, 0:1], C.ap[:, CI["cols"], 0:1], AF.Exp, [C, lg], [wfb], scale=lgf)
                S.act(wfb.ap[:, 1:2], C.ap[:, CI["cols"], 1:2], AF.Exp, [C, lg], [wfb], scale=lgb)
                S.ts("dve", wfb.ap[:], wfb.ap[:], ksc, None, ALU.mult, None, [wfb], [wfb])
                S.act(G.ap[:, 0, :], cm("rowip1"), AF.Exp, [C, lg], [G], scale=lgf)
                S.act(G.ap[:, 1, :], cm("row128mi"), AF.Exp, [C, lg], [G], scale=lgb)
                S.act(gT.ap[:, 0:1], lgf, AF.Exp, [lg], [gT], scale=128.0)
                S.act(gT.ap[:, 1:2], lgb, AF.Exp, [lg], [gT], scale=128.0)
                li = 0
                for (t0, n) in tiles:
                    S.dma("sp", cs_t.ap[:, :n], cosr[:, t0:t0 + n], writes=[cs_t]); S.dma("sp", sn_t.ap[:, :n], sinr[:, t0:t0 + n], writes=[sn_t])
                    for which, row in ((0, rq), (1, rk)):
                        l = ld[li % 2]; li += 1
                        S.dma("sp", l.ap[:, :n], projT[row:row + 128, t0:t0 + n], reads=[d_proj], writes=[l])
                        S.mm(ps[2].ap[:, :n], cm("rot128"), l.ap[:, :n], [C, l], [ps[2]])
                        S.tt("dve", t1.ap[:, :n], l.ap[:, :n], cs_t.ap[:, :n], ALU.mult, [l, cs_t], [t1])
                        S.tt("dve", t2.ap[:, :n], ps[2].ap[:, :n], sn_t.ap[:, :n], ALU.mult, [ps[2], sn_t], [t2])
                        if which == 0:
                            S.tt("pool", QT.ap[:, t0:t0 + n], t1.ap[:, :n], t2.ap[:, :n], ALU.add, [t1, t2], [QT])
                        else:
                            S.tt("pool", kro.ap[:, :n], t1.ap[:, :n], t2.ap[:, :n], ALU.add, [t1, t2], [kro])
                            S.copy("act", KT.ap[:, t0:t0 + n], kro.ap[:, :n], [kro], [KT])
                            for cc in range(n // 128):
                                c = t0 // 128 + cc
                                S.tr(ps[3].ap[:, 0:128], kro.ap[:, cc * 128:(cc + 1) * 128], cm("ident"), [kro, C], [ps[3]])
                                S.act(Kf.ap[:, c, :], ps[3].ap[:, 0:128], AF.Copy, [ps[3], wfb], [Kf], scale=wfb.ap[:, 0:1])
                                S.act(Kb.ap[:, c, :], ps[3].ap[:, 0:128], AF.Copy, [ps[3], wfb], [Kb], scale=wfb.ap[:, 1:2])
                    l = ld[li % 2]; li += 1
                    S.dma("sp", l.ap[:, :n], projT[rv:rv + 128, t0:t0 + n], reads=[d_proj], writes=[l])
                    for cc in range(n // 128):
                        S.tr(ps[1].ap[:, 0:128], l.ap[:, cc * 128:(cc + 1) * 128], cm("ident"), [l, C], [ps[1]])
                        S.copy("act", Vtm.ap[:, t0 // 128 + cc, :], ps[1].ap[:, 0:128], [ps[1]], [Vtm])
                fwd = list(range(NCH))
                bwd = list(range(NCC - 1, -1, -1)) + list(range(NCH - 1, NCC - 1, -1))
                for order, Kx, Sx, gcol in ((fwd, Kf, Sf, 0), (bwd, Kb, Sbk, 1)):
                    S.op("dve", lambda e: e.memset(St.ap[:], 0.0), [], [St])
                    for idx, c in enumerate(order):
                        S.copy("act", Sx.ap[:, c, :], St.ap[:], [St], [Sx])
                        if idx < len(order) - 1:
                            S.mm(ps[0].ap[:, 0:128], Kx.ap[:, c, :], Vtm.ap[:, c, :], [Kx, Vtm], [ps[0]])
                            S.stt(St.ap[:], St.ap[:], gT.ap[:, gcol:gcol + 1], ps[0].ap[:, 0:128], ALU.mult, ALU.add, [St, gT, ps[0]], [St])
                for (t0, n) in tiles:
                    if last and t0 < CTX:
                        continue
                    for cc in range(n // 128):
                        c = t0 // 128 + cc
                        sl = slice(c * 128, (c + 1) * 128)
                        S.mm(ps[4].ap[:, 0:128], KT.ap[:, sl], QT.ap[:, sl], [KT, QT], [ps[4]])
                        S.tt("dve", AT.ap[:], ps[4].ap[:, 0:128], M.ap[:], ALU.mult, [ps[4], M], [AT])
                        S.tt("pool", Qf.ap[:], QT.ap[:, sl], G.ap[:, 0, :], ALU.mult, [QT, G], [Qf])
                        S.tt("pool", Qb.ap[:], QT.ap[:, sl], G.ap[:, 1, :], ALU.mult, [QT, G], [Qb])
                        S.mm(ps[5].ap[:, 0:128], Vtm.ap[:, c, :], AT.ap[:], [Vtm, AT], [ps[5]], start=True, stop=False)
                        S.mm(ps[5].ap[:, 0:128], Sf.ap[:, c, :], Qf.ap[:], [Sf, Qf], [ps[5]], start=False, stop=False)
                        S.mm(ps[5].ap[:, 0:128], Sbk.ap[:, c, :], Qb.ap[:], [Sbk, Qb], [ps[5]], start=False, stop=True)
                        S.copy("act", yt.ap[:, cc * 128:(cc + 1) * 128], ps[5].ap[:, 0:128], [ps[5]], [yt])
                    S.act(sqb.ap[:, :n], yt.ap[:, :n], AF.Square, [yt], [sqb])
                    S.mm(ps[6].ap[:, :n], ones_bf, sqb.ap[:, :n], [sqb, Cb], [ps[6]])
                    S.act(rsd.ap[:, :n], ps[6].ap[:, :n], AF.Ln, [ps[6]], [rsd], scale=1.0 / 128, bias=EPS)
                    S.act(rsd.ap[:, :n], rsd.ap[:, :n], AF.Exp, [rsd], [rsd], scale=-0.5)
                    S.dma("sp", gl.ap[:, :n], projT[rg:rg + 128, t0:t0 + n], reads=[d_proj], writes=[gl])
                    S.act(gl.ap[:, :n], gl.ap[:, :n], AF.Silu, [gl], [gl])
                    S.stt(oo.ap[:, :n], yt.ap[:, :n], nwr.ap[:, hr:hr + 1], rsd.ap[:, :n], ALU.mult, ALU.mult, [yt, nwr, rsd], [oo])
                    S.tt("dve", oo.ap[:, :n], oo.ap[:, :n], gl.ap[:, :n], ALU.mult, [oo, gl], [oo])
                    store_mix(S, hr * 128, 128, t0, n, oo.ap[:, :n], oo, d_mix)
        S.barrier()

        base_s = 1024
        segs = [(0, CTX), (CTX, T)]
        with contextlib.ExitStack() as es:
            cwt = SB(es, "cwt", [128, 8, 5]); cbt = SB(es, "cbt", [128, 8])
            S.dma("sp", cwt.ap[:], cw, writes=[cwt]); S.dma("sp", cbt.ap[:], cb, writes=[cbt])
            with contextlib.ExitStack() as es2:
                xr = SB(es2, "xr", [128, T]); yc = SB(es2, "yc", [128, T])
                for ch in range(8):
                    r0 = base_s + 512 + ch * 128
                    S.dma("sp", xr.ap[:], projT[r0:r0 + 128, :], reads=[d_proj], writes=[xr])
                    for (a, b) in segs:
                        S.ts("dve", yc.ap[:, a:b], xr.ap[:, a:b], cwt.ap[:, ch, 2:3], cbt.ap[:, ch:ch + 1], ALU.mult, ALU.add, [xr, cwt, cbt], [yc])
                        for k in (0, 1, 3, 4):
                            off = k - 2
                            lo = max(a, a - off); hi = min(b, b - off)
                            S.stt(yc.ap[:, lo:hi], xr.ap[:, lo + off:hi + off], cwt.ap[:, ch, k:k + 1], yc.ap[:, lo:hi], ALU.mult, ALU.add, [xr, cwt, yc], [yc])
                    S.act(yc.ap[:], yc.ap[:], AF.Silu, [yc], [yc])
                    S.dma("sp", convT[ch * 128:(ch + 1) * 128, :], yc.ap[:], reads=[yc], writes=[d_conv])
            S.barrier()
            dt_tm = SB(es, "dt_tm", [128, NCH, 16]); a_tm = SB(es, "a_tm", [128, NCH, 16]); aneg = SB(es, "aneg", [128, 16])
            dsk_t = SB(es, "dsk_t", [128, 8]); snw_t = SB(es, "snw_t", [64, 8])
            S.dma("sp", aneg.ap[:], alog, writes=[aneg]); S.dma("sp", dsk_t.ap[:], dsk, writes=[dsk_t]); S.dma("sp", snw_t.ap[:], snw, writes=[snw_t])
            S.act(aneg.ap[:], aneg.ap[:], AF.Exp, [aneg], [aneg])
            S.ts("dve", aneg.ap[:], aneg.ap[:], -1.0, None, ALU.mult, None, [aneg], [aneg])
            with contextlib.ExitStack() as es2:
                dx = SB(es2, "dx", [16, T]); da = SB(es2, "da", [16, T]); dtb_t = SB(es2, "dtb_t", [16, 1])
                S.dma("sp", dx.ap[:], projT[base_s + 1536:base_s + 1552, :], reads=[d_proj], writes=[dx])
                S.dma("sp", dtb_t.ap[:], dtb, writes=[dtb_t])
                S.ts("dve", dx.ap[:], dx.ap[:], dtb_t.ap[:, 0:1], None, ALU.add, None, [dx, dtb_t], [dx])
                S.act(da.ap[:], dx.ap[:], AF.Abs, [dx], [da])
                S.act(da.ap[:], da.ap[:], AF.Exp, [da], [da], scale=-1.0)
                S.act(da.ap[:], da.ap[:], AF.Ln, [da], [da], bias=1.0)
                S.ts("dve", dx.ap[:], dx.ap[:], 0.0, None, ALU.max, None, [dx], [dx])
                S.tt("dve", dx.ap[:], dx.ap[:], da.ap[:], ALU.add, [dx, da], [dx])
                for c in range(NCH):
                    S.tr(ps[0].ap[:, 0:16], dx.ap[:, c * 128:(c + 1) * 128], C.ap[0:16, CI["ident"], 0:16], [dx, C], [ps[0]])
                    S.copy("act", dt_tm.ap[:, c, :], ps[0].ap[:, 0:16], [ps[0]], [dt_tm])
                    S.tt("dve", a_tm.ap[:, c, :], dt_tm.ap[:, c, :], aneg.ap[:], ALU.mult, [dt_tm, aneg], [a_tm])
            S.barrier()
            BT = SB(es, "BT", [128, T], BF16); CT = SB(es, "CT", [128, T], BF16)
            Btm = SB(es, "Btm", [128, NCH, 128], BF16); xsb = SB(es, "xsb", [128, NCH, 256], BF16)
            Sf = SB(es, "sSf", [128, NCH, 256], BF16); Sbk = SB(es, "sSb", [128, NCH, 256], BF16)
            wfb = SB(es, "swfb", [128, NCH, 8]); etot = SB(es, "etot", [128, NCH, 8]); ew = SB(es, "ew", [128, 16])
            ld = [SB(es, f"sld{i}", [128, 512]) for i in range(2)]
            St = SB(es, "sSt", [128, 256]); vp = SB(es, "vp", [128, 256], BF16)
            scs = SB(es, "scs", [128, 128]); rf = SB(es, "rf", [128, 128]); rb = SB(es, "rb", [128, 128]); E = SB(es, "E", [128, 512])
            u1 = SB(es, "u1", [128, 128]); u2 = SB(es, "u2", [128, 128]); AT = SB(es, "sAT", [128, 128], BF16)
            Qf = SB(es, "sQf", [128, 128], BF16); Qb = SB(es, "sQb", [128, 128], BF16)
            xs_t = SB(es, "xs_t", [64, 512]); z_t = SB(es, "z_t", [64, 512]); yd = [SB(es, f"yd{h}", [64, 512]) for h in range(4)]
            sq4 = SB(es, "sq4", [64, 512], BF16); rsd = SB(es, "srsd", [64, 512]); oo = SB(es, "soo", [64, 512])
            for gs in range(2):
                fc = gs * 4; bc = 8 + gs * 4
                li = 0
                for (t0, n) in tiles:
                    for which, dst in ((0, BT), (1, CT)):
                        l = ld[li % 2]; li += 1
                        r0 = 512 + which * 256 + gs * 128
                        S.dma("sp", l.ap[:, :n], convT[r0:r0 + 128, t0:t0 + n], reads=[d_conv], writes=[l])
                        S.copy("act", dst.ap[:, t0:t0 + n], l.ap[:, :n], [l], [dst])
                        if which == 0:
                            for cc in range(n // 128):
                                S.tr(ps[0].ap[:, 0:128], l.ap[:, cc * 128:(cc + 1) * 128], cm("ident"), [l, C], [ps[0]])
                                S.copy("act", Btm.ap[:, t0 // 128 + cc, :], ps[0].ap[:, 0:128], [ps[0]], [Btm])
                    for half in range(2):
                        l = ld[li % 2]; li += 1
                        r0 = gs * 256 + half * 128
                        S.dma("sp", l.ap[:, :n], convT[r0:r0 + 128, t0:t0 + n], reads=[d_conv], writes=[l])
                        for cc in range(n // 128):
                            S.tr(ps[1].ap[:, 0:128], l.ap[:, cc * 128:(cc + 1) * 128], cm("ident"), [l, C], [ps[1]])
                            S.copy("act", xsb.ap[:, t0 // 128 + cc, half * 128:(half + 1) * 128], ps[1].ap[:, 0:128], [ps[1]], [xsb])
                for c in range(NCH):
                    S.mm(ps[2].ap[:, 0:4], cm("sl"), a_tm.ap[:, c, fc:fc + 4], [C, a_tm], [ps[2]])
                    S.mm(ps[2].ap[:, 4:8], cm("su"), a_tm.ap[:, c, bc:bc + 4], [C, a_tm], [ps[2]])
                    S.mm(ps[2].ap[:, 8:12], cm("ones"), a_tm.ap[:, c, fc:fc + 4], [C, a_tm], [ps[2]])
                    S.mm(ps[2].ap[:, 12:16], cm("ones"), a_tm.ap[:, c, bc:bc + 4], [C, a_tm], [ps[2]])
                    S.act(ew.ap[:], ps[2].ap[:, 0:16], AF.Exp, [ps[2]], [ew])
                    S.tt("dve", wfb.ap[:, c, 0:4], ew.ap[:, 0:4], dt_tm.ap[:, c, fc:fc + 4], ALU.mult, [ew, dt_tm], [wfb])
                    S.tt("dve", wfb.ap[:, c, 4:8], ew.ap[:, 4:8], dt_tm.ap[:, c, bc:bc + 4], ALU.mult, [ew, dt_tm], [wfb])
                    S.copy("dve", etot.ap[:, c, :], ew.ap[:, 8:16], [ew], [etot])
                fwd = list(range(NCH))
                bwd = list(range(NCC - 1, -1, -1)) + list(range(NCH - 1, NCC - 1, -1))
                for order, Sx, o4 in ((fwd, Sf, 0), (bwd, Sbk, 4)):
                    S.op("dve", lambda e: e.memset(St.ap[:], 0.0), [], [St])
                    for idx, c in enumerate(order):
                        S.copy("act", Sx.ap[:, c, :], St.ap[:], [St], [Sx])
                        if idx < len(order) - 1:
                            for h in range(4):
                                S.ts("pool", vp.ap[:, h * 64:(h + 1) * 64], xsb.ap[:, c, h * 64:(h + 1) * 64], wfb.ap[:, c, o4 + h:o4 + h + 1], None, ALU.mult, None, [xsb, wfb], [vp])
                            S.mm(ps[3].ap[:, 0:256], Btm.ap[:, c, :], vp.ap[:], [Btm, vp], [ps[3]])
                            for h in range(4):
                                S.stt(St.ap[:, h * 64:(h + 1) * 64], St.ap[:, h * 64:(h + 1) * 64], etot.ap[:, c, o4 + h:o4 + h + 1], ps[3].ap[:, h * 64:(h + 1) * 64],
                                      ALU.mult, ALU.add, [St, etot, ps[3]], [St])
                for (t0, n) in tiles:
                    if last and t0 < CTX:
                        continue
                    for cc in range(n // 128):
                        c = t0 // 128 + cc
                        sl = slice(c * 128, (c + 1) * 128)
                        S.mm(ps[0].ap[:, 0:128], BT.ap[:, sl], CT.ap[:, sl], [BT, CT], [ps[0]])
                        S.copy("act", scs.ap[:], ps[0].ap[:, 0:128], [ps[0]], [scs])
                        for h in range(4):
                            S.ts("dve", rf.ap[:], cm("uf"), a_tm.ap[:, c, fc + h:fc + h + 1], None, ALU.mult, None, [C, a_tm], [rf])
                            S.ts("dve", rb.ap[:], cm("ub"), a_tm.ap[:, c, bc + h:bc + h + 1], None, ALU.mult, None, [C, a_tm], [rb])
                            S.mm(ps[1].ap[:, 0:128], cm("sl"), rf.ap[:], [C, rf], [ps[1]])
                            S.mm(ps[1].ap[:, 128:256], cm("su"), rb.ap[:], [C, rb], [ps[1]])
                            S.mm(ps[1].ap[:, 256:384], cm("ones"), rf.ap[:], [C, rf], [ps[1]])
                            S.mm(ps[1].ap[:, 384:512], cm("ones"), rb.ap[:], [C, rb], [ps[1]])
                            S.act(E.ap[:], ps[1].ap[:], AF.Exp, [ps[1]], [E])
                            S.stt(u1.ap[:], E.ap[:, 0:128], dt_tm.ap[:, c, fc + h:fc + h + 1], cm("uf"), ALU.mult, ALU.mult, [E, dt_tm, C], [u1])
                            S.stt(u2.ap[:], E.ap[:, 128:256], dt_tm.ap[:, c, bc + h:bc + h + 1], cm("ub"), ALU.mult, ALU.mult, [E, dt_tm, C], [u2])
                            S.tt("pool", u1.ap[:], u1.ap[:], u2.ap[:], ALU.add, [u1, u2], [u1])
                            S.tt("pool", AT.ap[:], u1.ap[:], scs.ap[:], ALU.mult, [u1, scs], [AT])
                            S.tt("pool", Qf.ap[:], CT.ap[:, sl], E.ap[:, 256:384], ALU.mult, [CT, E], [Qf])
                            S.tt("pool", Qb.ap[:], CT.ap[:, sl], E.ap[:, 384:512], ALU.mult, [CT, E], [Qb])
                            py = ps[4 + h]
                            S.mm(py.ap[0:64, cc * 128:(cc + 1) * 128], xsb.ap[:, c, h * 64:(h + 1) * 64], AT.ap[:], [xsb, AT], [py], start=True, stop=False)
                            S.mm(py.ap[0:64, cc * 128:(cc + 1) * 128], Sf.ap[:, c, h * 64:(h + 1) * 64], Qf.ap[:], [Sf, Qf], [py], start=False, stop=False)
                            S.mm(py.ap[0:64, cc * 128:(cc + 1) * 128], Sbk.ap[:, c, h * 64:(h + 1) * 64], Qb.ap[:], [Sbk, Qb], [py], start=False, stop=True)
                    for h in range(4):
                        rx = gs * 256 + h * 64
                        S.dma("sp", xs_t.ap[:, :n], convT[rx:rx + 64, t0:t0 + n], reads=[d_conv], writes=[xs_t])
                        S.dma("sp", z_t.ap[:, :n], projT[base_s + rx:base_s + rx + 64, t0:t0 + n], reads=[d_proj], writes=[z_t])
                        S.stt(yd[h].ap[:, :n], xs_t.ap[:, :n], dsk_t.ap[0:64, gs * 4 + h:gs * 4 + h + 1], ps[4 + h].ap[0:64, :n], ALU.mult, ALU.add, [xs_t, dsk_t, ps[4 + h]], [yd[h]])
                        S.act(z_t.ap[:, :n], z_t.ap[:, :n], AF.Silu, [z_t], [z_t])
                        S.tt("dve", yd[h].ap[:, :n], yd[h].ap[:, :n], z_t.ap[:, :n], ALU.mult, [yd[h], z_t], [yd[h]])
                        S.act(sq4.ap[:, :n], yd[h].ap[:, :n], AF.Square, [yd[h]], [sq4])
                        S.mm(ps[3].ap[0:64, :n], ones_bf[0:64, 0:64], sq4.ap[:, :n], [Cb, sq4], [ps[3]], start=(h == 0), stop=(h == 3))
                    S.act(rsd.ap[:, :n], ps[3].ap[0:64, :n], AF.Ln, [ps[3]], [rsd], scale=1.0 / 256, bias=EPS)
                    S.act(rsd.ap[:, :n], rsd.ap[:, :n], AF.Exp, [rsd], [rsd], scale=-0.5)
                    for h in range(4):
                        S.stt(oo.ap[:, :n], yd[h].ap[:, :n], snw_t.ap[:, gs * 4 + h:gs * 4 + h + 1], rsd.ap[:, :n], ALU.mult, ALU.mult, [yd[h], snw_t, rsd], [oo])
                        r0 = 256 + gs * 256 + h * 64
                        store_mix(S, r0, 64, t0, n, oo.ap[:, :n], oo, d_mix)
        S.barrier()


K1_SMALL = (("rdl", [128, 4]), ("rnw", [128, 2]), ("cw", [128, 8, 5]), ("cb", [128, 8]), ("dtb", [16, 1]), ("alog", [128, 16]),
            ("dsk", [128, 8]), ("snw", [64, 8]), ("dlam", [128, 4, 64]), ("dnw", [128, 1]))


def build_k1(cfg, last, lam_init):
    D, KD, T = cfg.D, cfg.KD, cfg.T
    nc = bass.Bass("TRN2", target_bir_lowering=False)
    dt_in = lambda n, sh: nc.dram_tensor(n, sh, F32, kind="ExternalInput").ap()
    xT = dt_in("xT", [D, T])
    I = {"w_in": dt_in("w_in", [D, cfg.PC]), "ada_w": dt_in("ada_w", [D, 2 * D]), "ada_b": dt_in("ada_b", [128, 2 * KD]),
         "ccol": dt_in("ccol", [128, KD, 2]), "nmw": dt_in("nmw", [128, KD]), "consts": dt_in("consts", [128, len(CNAMES), 128])}
    for n_ in ("cosd", "sind", "cosr", "sinr"):
        I[n_] = dt_in(n_, [128, T])
    for n_, sh in K1_SMALL:
        I[n_] = dt_in(n_, sh)
    mixT = nc.dram_tensor("mixT", [1024, T], F32, kind="ExternalOutput").ap()
    I["hT"] = nc.dram_tensor("hT", [D, T], BF16, kind="Internal").ap()
    I["projT"] = nc.dram_tensor("projT", [cfg.PC, T], F32, kind="Internal").ap()
    I["convT"] = nc.dram_tensor("convT", [1024, T], F32, kind="Internal").ap()

    def load_x(S, xt, t0, n):
        for kg in range(0, KD, 8):
            k2 = min(KD, kg + 8)
            S.dma("sp", xt.ap[:, kg:k2, :n], xT[kg * 128:k2 * 128, t0:t0 + n].rearrange("(k p) t -> p k t", p=128), writes=[xt])

    def store_mix(S, r0, nr, t0, n, src, sbuf, d_mix):
        S.dma("sp", mixT[r0:r0 + nr, t0:t0 + n], src, reads=[sbuf], writes=[d_mix])

    with contextlib.ExitStack() as es:
        S = Sched(nc)
        ps = [Buf(es.enter_context(nc.psum_tensor(f"ps{i}", [128, 512], F32)), f"ps{i}") for i in range(8)]
        emit_k1(nc, S, ps, cfg, last, lam_init, I, "", load_x, store_mix, Buf(None, "d_mix"))
    return nc


def col_layout(v):
    v = np.asarray(v, np.float32)
    return np.ascontiguousarray(v.reshape(-1, 128).T)


def rep128(v):
    v = np.asarray(v, np.float32)
    return np.ascontiguousarray(np.broadcast_to(v[None], (128,) + v.shape))


def k1_cols(cfg, q):
    NQ = cfg.NQ
    RW, SW, DW, G, SH = 256 * NQ, 512 * NQ, 256 * NQ, 2 * NQ, 8 * NQ
    sizes = (RW, RW, RW, RW, SW, SW + 2 * G * 128, 2 * SH, DW, DW, DW)
    o = np.concatenate([[0], np.cumsum(sizes)]).astype(int)
    heads = (2 * q, 2 * q + 1)
    cols = []
    for part in range(4):
        for h in heads:
            cols += list(range(o[part] + h * 128, o[part] + (h + 1) * 128))
    for g in heads:
        cols += list(range(o[4] + g * 256, o[4] + (g + 1) * 256))
    conv_ch = []
    for g in heads:
        conv_ch += list(range(g * 256, (g + 1) * 256))
    for g in heads:
        conv_ch += list(range(SW + g * 128, SW + (g + 1) * 128))
    for g in heads:
        conv_ch += list(range(SW + G * 128 + g * 128, SW + G * 128 + (g + 1) * 128))
    cols += [o[5] + c for c in conv_ch]
    dt_idx = [d * SH + g * 4 + r for d in range(2) for g in heads for r in range(4)]
    cols += [o[6] + i for i in dt_idx]
    for part in (7, 8, 9):
        for h in heads:
            cols += list(range(o[part] + h * 128, o[part] + (h + 1) * 128))
    assert len(cols) == cfg.PC
    return np.array(cols), np.array(conv_ch), dt_idx


def k1_inputs(cfg, inp, layer, b, q, xT_b, tabs):
    D, KD = cfg.D, cfg.KD
    cols, conv_ch, dt_idx = k1_cols(cfg, q)
    heads = [2 * q, 2 * q + 1]
    m = {}
    m["xT"] = xT_b
    m["w_in"] = np.ascontiguousarray(inp["w_in"][layer][:, cols])
    m["ada_w"] = np.ascontiguousarray(inp["ada_w"][layer][:, 0:2 * D])
    m["ada_b"] = col_layout(inp["ada_b"][layer][0:2 * D])
    m["ccol"] = np.ascontiguousarray(np.stack([col_layout(inp["c"][b]), col_layout(inp["c_ctx"])], axis=-1))
    m["nmw"] = col_layout(inp["norm_mix_w"][layer])
    m["consts"] = CPACK
    m["cosd"], m["sind"], m["cosr"], m["sinr"] = tabs
    rd = inp["ret_decay_logit"][layer]
    m["rdl"] = rep128(np.array([rd[0, heads[0]], rd[0, heads[1]], rd[1, heads[0]], rd[1, heads[1]]], np.float32))
    m["rnw"] = np.ascontiguousarray(inp["ret_norm_w"][layer].reshape(-1, 128)[heads].T)
    cwf = inp["ssd_conv_w"][layer][:, conv_ch]
    m["cw"] = np.ascontiguousarray(cwf.reshape(5, 8, 128).transpose(2, 1, 0))
    m["cb"] = np.ascontiguousarray(inp["ssd_conv_b"][layer][conv_ch].reshape(8, 128).T)
    m["dtb"] = np.ascontiguousarray(inp["ssd_dt_bias"][layer].reshape(-1)[dt_idx].reshape(16, 1))
    m["alog"] = rep128(inp["ssd_a_log"][layer].reshape(-1)[dt_idx])
    hidx = [g * 4 + r for g in heads for r in range(4)]
    m["dsk"] = rep128(inp["ssd_d"][layer][hidx])
    nw = inp["ssd_norm_w"][layer].reshape(-1, 4, 64)[heads]
    m["snw"] = np.ascontiguousarray(nw.reshape(8, 64).T)
    m["dlam"] = rep128(inp["diff_lambda"][layer])
    m["dnw"] = np.ascontiguousarray(inp["diff_norm_w"][layer].reshape(128, 1))
    return {k: np.ascontiguousarray(v, dtype=np.float32) for k, v in m.items()}


def assemble_mix(cfg, per_q):
    NQ = cfg.NQ
    RW, SW = 256 * NQ, 512 * NQ
    out = np.empty((cfg.MIX, per_q[0].shape[1]), np.float32)
    for q, loc in enumerate(per_q):
        out[q * 256:(q + 1) * 256] = loc[0:256]
        out[RW + q * 512:RW + (q + 1) * 512] = loc[256:768]
        out[RW + SW + q * 256:RW + SW + (q + 1) * 256] = loc[768:1024]
    return out


def k2_tiles(cfg, ntq, last):
    tl_c = 0 if last else cfg.CTX // ntq
    tl_l = cfg.SEQ // ntq
    tiles = []
    if tl_c:
        tiles.append((0, tl_c, 1))
    t = tl_c
    while t < tl_c + tl_l:
        n = min(512, tl_c + tl_l - t)
        tiles.append((t, n, 0))
        t += n
    return tiles, tl_c + tl_l


def emit_k2(nc, S, ps, cfg, ntq, last, moe, I, pf, x_src, load_mix, out_ap, d_out):
    D, KD, MIX = cfg.D, cfg.KD, cfg.MIX
    KM = MIX // 128
    tiles, TL = k2_tiles(cfg, ntq, last)
    NF = (cfg.DFE if moe else cfg.DFF) // 128
    NE = cfg.NEXP if moe else 1
    FS = 22 if not moe else 16
    splits = []
    f = 0
    while f < NF:
        nf = min(FS, NF - f)
        splits.append((f, nf)); f += nf
    w_out, ada_w, ada_b, ccol, nfw, consts = I["w_out"], I["ada_w"], I["ada_b"], I["ccol"], I["nfw"], I["consts"]
    wg_d, wu_d, wd_d = I["wg"], I["wu"], I["wd"]
    if moe:
        rw, selc = I["rw"], I["selc"]
    if last:
        fnw = I["fnw"]
    xT = x_src
    xout = out_ap
    WBE = 32 * 256

    with contextlib.ExitStack() as es0:
        def SB(es, n, sh, dt=F32):
            return Buf(es.enter_context(nc.sbuf_tensor(pf + n, sh, dt)), n)
        C = SB(es0, "C", [128, 2, 128]); Cb = SB(es0, "Cb", [128, 2, 128], BF16)
        S.dma("sp", C.ap[:], consts[:, 0:2, :], writes=[C])
        S.copy("dve", Cb.ap[:, 0, :], C.ap[:, CI["ones"], :], [C], [Cb])
        S.copy("dve", Cb.ap[:, 1, :], C.ap[:, CI["ident"], :], [C], [Cb])
        cm = lambda n: C.ap[:, CI[n], :]
        ones_bf = Cb.ap[:, 0, :]
        MV = SB(es0, "MV", [128, KD, 8])
        with contextlib.ExitStack() as es:
            sc = SB(es, "sc", [128, KD, 2]); adb = SB(es, "adb", [128, 4 * KD]); nm = SB(es, "nm", [128, KD])
            modc = SB(es, "modc", [128, 4 * KD, 2])
            wa = [SB(es, f"wa{i}", [128, KD, 512]) for i in range(2)]
            S.dma("sp", sc.ap[:], ccol, writes=[sc]); S.dma("sp", adb.ap[:], ada_b, writes=[adb]); S.dma("sp", nm.ap[:], nfw, writes=[nm])
            S.act(sc.ap[:], sc.ap[:], AF.Silu, [sc], [sc])
            nfc = 4 * KD
            pm = ps[0]
            for fg in range(0, nfc, 4):
                w = wa[(fg // 4) % 2]
                for kg in range(0, KD, 8):
                    k2 = min(KD, kg + 8)
                    S.dma("sp", w.ap[:, kg:k2, :], ada_w[kg * 128:k2 * 128, fg * 128:(fg + 4) * 128].rearrange("(k p) c -> p k c", p=128), writes=[w])
                for f in range(fg, fg + 4):
                    for k in range(KD):
                        S.mm(pm.ap[:, 2 * f:2 * f + 2], w.ap[:, k, (f - fg) * 128:(f - fg + 1) * 128], sc.ap[:, k, :], [w, sc], [pm],
                             start=(k == 0), stop=(k == KD - 1), signal=(k == KD - 1))
            for j in range(2):
                S.tt("dve", modc.ap[:, :, j], pm.ap[:, 0:2 * nfc].rearrange("p (f j) -> p f j", j=2)[:, :, j], adb.ap[:], ALU.add, [pm, adb], [modc])
            for j in range(2):
                S.copy("dve", MV.ap[:, :, 0 + j], modc.ap[:, 0:KD, j], [modc], [MV])
                S.stt(MV.ap[:, :, 2 + j], modc.ap[:, 2 * KD:3 * KD, j], 1.0, nm.ap[:], ALU.add, ALU.mult, [modc, nm], [MV])
                S.copy("dve", MV.ap[:, :, 4 + j], modc.ap[:, KD:2 * KD, j], [modc], [MV])
                S.copy("dve", MV.ap[:, :, 6 + j], modc.ap[:, 3 * KD:4 * KD, j], [modc], [MV])
        S.barrier()

        with contextlib.ExitStack() as es:
            x1 = SB(es, "x1", [128, KD, 512]); mh = SB(es, "mh", [128, max(KD, KM), 512], BF16)
            aT = SB(es, "aT", [128, FS, 512], BF16)
            wbuf = [SB(es, f"wbuf{i}", [128, WBE], BF16) for i in range(4)]
            sq = [SB(es, f"sq{i}", [128, 512], BF16) for i in range(2)]
            tmp = [SB(es, f"tmp{i}", [128, 512]) for i in range(1 if moe else 2)]
            sg = [SB(es, f"sg{i}", [128, 512]) for i in range(2)]
            rs = SB(es, "rs", [128, 512])
            if moe:
                rwt = SB(es, "rwt", [128, KD, 8]); sel = SB(es, "sel", [8, 8, 128]); lgt = SB(es, "lgt", [128, 8]); m8 = SB(es, "m8", [128, 8])
                mk = SB(es, "mk", [128, 16]); gg = SB(es, "gg", [128, 4]); Gm = SB(es, "Gm", [128, 8]); GT = SB(es, "GT", [8, 512])
                gb = SB(es, "gb", [128, 8, 512], BF16); hf = [SB(es, "hf0", [128, 512])]
                S.dma("sp", rwt.ap[:], rw, writes=[rwt]); S.dma("sp", sel.ap[:], selc, writes=[sel])
            if last:
                fn = SB(es, "fn", [128, KD]); S.dma("sp", fn.ap[:], fnw, writes=[fn])
            wi = [0]

            def wload(src, nk, ncol):
                b = wbuf[wi[0] % 4]; wi[0] += 1
                view = b.ap[:, 0:nk * ncol].rearrange("p (k c) -> p k c", c=ncol)
                S.dma("pool", view, src.rearrange("(k p) c -> p k c", p=128), writes=[b])
                return b, view

            pi = [0, 0, 0]
            for (t0, n, kind) in tiles:
                for kg in range(0, KD, 8):
                    k2 = min(KD, kg + 8)
                    S.dma("sp", x1.ap[:, kg:k2, :n], xT[kg * 128:k2 * 128, t0:t0 + n].rearrange("(k p) t -> p k t", p=128), writes=[x1])
                load_mix(S, mh, t0, n, kind)
                for db in range(0, KD, 2):
                    ncb = min(2, KD - db)
                    b, v = wload(w_out[:, db * 128:(db + ncb) * 128], KM, ncb * 128)
                    for j in range(ncb):
                        dc = db + j
                        p = ps[pi[0] % 2]; pi[0] += 1
                        for k in range(KM):
                            S.mm(p.ap[:, :n], v[:, k, j * 128:(j + 1) * 128], mh.ap[:, k, :n], [b, mh], [p], start=(k == 0), stop=(k == KM - 1), signal=(k == KM - 1))
                        S.stt(x1.ap[:, dc, :n], p.ap[:, :n], MV.ap[:, dc, kind:kind + 1], x1.ap[:, dc, :n], ALU.mult, ALU.add, [p, MV, x1], [x1])
                for k in range(KD):
                    s_ = sq[k % 2]
                    S.act(s_.ap[:, :n], x1.ap[:, k, :n], AF.Square, [x1], [s_])
                    S.mm(ps[6].ap[:, :n], ones_bf, s_.ap[:, :n], [s_, Cb], [ps[6]], start=(k == 0), stop=(k == KD - 1))
                S.act(rs.ap[:, :n], ps[6].ap[:, :n], AF.Ln, [ps[6]], [rs], scale=1.0 / D, bias=EPS)
                S.act(rs.ap[:, :n], rs.ap[:, :n], AF.Exp, [rs], [rs], scale=-0.5)
                nsub = (n + 127) // 128
                for k in range(KD):
                    t_ = tmp[k % len(tmp)]
                    S.tt("dve", t_.ap[:, :n], x1.ap[:, k, :n], rs.ap[:, :n], ALU.mult, [x1, rs], [t_])
                    S.act(mh.ap[:, k, :n], t_.ap[:, :n], AF.Identity, [t_, MV], [mh], scale=MV.ap[:, k, 2 + kind:3 + kind], bias=MV.ap[:, k, 4 + kind:5 + kind])
                    if moe:
                        h_ = hf[0]
                        S.act(h_.ap[:, :n], t_.ap[:, :n], AF.Identity, [t_, MV], [h_], scale=MV.ap[:, k, 2 + kind:3 + kind], bias=MV.ap[:, k, 4 + kind:5 + kind])
                        for sub in range(nsub):
                            m_ = min(128, n - sub * 128)
                            S.mm(ps[2 + sub].ap[:m_, 0:8], h_.ap[:, sub * 128:sub * 128 + m_], rwt.ap[:, k, :], [h_, rwt], [ps[2 + sub]], start=(k == 0), stop=(k == KD - 1))
                if moe:
                    for sub in range(nsub):
                        m_ = min(128, n - sub * 128)
                        S.copy("dve", lgt.ap[:m_, :], ps[2 + sub].ap[:m_, 0:8], [ps[2 + sub]], [lgt])
                        S.op("dve", lambda e: e.max(out=m8.ap[:m_, :], in_=lgt.ap[:m_, :]), [lgt], [m8])
                        S.ts("dve", mk.ap[:m_, 0:8], lgt.ap[:m_, :], m8.ap[:m_, 0:1], None, ALU.is_equal, None, [lgt, m8], [mk])
                        S.ts("dve", mk.ap[:m_, 8:16], lgt.ap[:m_, :], m8.ap[:m_, 1:2], None, ALU.is_equal, None, [lgt, m8], [mk])
                        S.tt("dve", gg.ap[:m_, 0:1], m8.ap[:m_, 1:2], m8.ap[:m_, 0:1], ALU.subtract, [m8], [gg])
                        S.act(gg.ap[:m_, 1:2], gg.ap[:m_, 0:1], AF.Exp, [gg], [gg])
                        S.ts("dve", gg.ap[:m_, 2:3], gg.ap[:m_, 1:2], 1.0, None, ALU.add, None, [gg], [gg])
                        S.op("dve", lambda e: e.reciprocal(out=gg.ap[:m_, 2:3], in_=gg.ap[:m_, 2:3]), [gg], [gg])
                        S.tt("dve", gg.ap[:m_, 3:4], gg.ap[:m_, 1:2], gg.ap[:m_, 2:3], ALU.mult, [gg], [gg])
                        S.ts("dve", Gm.ap[:m_, :], mk.ap[:m_, 0:8], gg.ap[:m_, 2:3], None, ALU.mult, None, [mk, gg], [Gm])
                        S.stt(Gm.ap[:m_, :], mk.ap[:m_, 8:16], gg.ap[:m_, 3:4], Gm.ap[:m_, :], ALU.mult, ALU.add, [mk, gg, Gm], [Gm])
                        S.tr(ps[6].ap[0:8, 0:m_], Gm.ap[:m_, :], C.ap[:m_, CI["ident"], 0:m_], [Gm, C], [ps[6]])
                        S.copy("dve", GT.ap[:, sub * 128:sub * 128 + m_], ps[6].ap[0:8, 0:m_], [ps[6]], [GT])
                    for e_ in range(NE):
                        S.mm(ps[7].ap[:, :n], sel.ap[:, e_, :], GT.ap[:, :n], [sel, GT], [ps[7]])
                        S.copy("act", gb.ap[:, e_, :n], ps[7].ap[:, :n], [ps[7]], [gb])
                for e_ in range(NE):
                    wg_e = wg_d[e_] if moe else wg_d
                    wu_e = wu_d[e_] if moe else wu_d
                    wd_e = wd_d[e_] if moe else wd_d
                    for (f0, nf) in splits:
                        for fb in range(f0, f0 + nf, 2):
                            ncb = min(2, f0 + nf - fb)
                            bg, vg = wload(wg_e[:, fb * 128:(fb + ncb) * 128], KD, ncb * 128)
                            bu, vu = wload(wu_e[:, fb * 128:(fb + ncb) * 128], KD, ncb * 128)
                            for j in range(ncb):
                                fc = fb + j
                                pg = ps[2 + pi[1] % 2]; pu = ps[4 + pi[1] % 2]; s_ = sg[pi[1] % 2]; pi[1] += 1
                                for k in range(KD):
                                    S.mm(pg.ap[:, :n], vg[:, k, j * 128:(j + 1) * 128], mh.ap[:, k, :n], [bg, mh], [pg], start=(k == 0), stop=(k == KD - 1), signal=(k == KD - 1))
                                for k in range(KD):
                                    S.mm(pu.ap[:, :n], vu[:, k, j * 128:(j + 1) * 128], mh.ap[:, k, :n], [bu, mh], [pu], start=(k == 0), stop=(k == KD - 1), signal=(k == KD - 1))
                                S.act(s_.ap[:, :n], pg.ap[:, :n], AF.Silu, [pg], [s_])
                                if moe:
                                    S.tt("dve", s_.ap[:, :n], s_.ap[:, :n], pu.ap[:, :n], ALU.mult, [s_, pu], [s_])
                                    S.tt("pool", aT.ap[:, fc - f0, :n], s_.ap[:, :n], gb.ap[:, e_, :n], ALU.mult, [s_, gb], [aT])
                                else:
                                    S.tt("dve", aT.ap[:, fc - f0, :n], s_.ap[:, :n], pu.ap[:, :n], ALU.mult, [s_, pu], [aT])
                        for db in range(0, KD, 2):
                            ncb = min(2, KD - db)
                            b, v = wload(wd_e[f0 * 128:(f0 + nf) * 128, db * 128:(db + ncb) * 128], nf, ncb * 128)
                            for j in range(ncb):
                                dc = db + j
                                p = ps[pi[0] % 2]; pi[0] += 1
                                for k in range(nf):
                                    S.mm(p.ap[:, :n], v[:, k, j * 128:(j + 1) * 128], aT.ap[:, k, :n], [b, aT], [p], start=(k == 0), stop=(k == nf - 1), signal=(k == nf - 1))
                                S.stt(x1.ap[:, dc, :n], p.ap[:, :n], MV.ap[:, dc, 6 + kind:7 + kind], x1.ap[:, dc, :n], ALU.mult, ALU.add, [p, MV, x1], [x1])
                if last:
                    for k in range(KD):
                        s_ = sq[k % 2]
                        S.act(s_.ap[:, :n], x1.ap[:, k, :n], AF.Square, [x1], [s_])
                        S.mm(ps[6].ap[:, :n], ones_bf, s_.ap[:, :n], [s_, Cb], [ps[6]], start=(k == 0), stop=(k == KD - 1))
                    S.act(rs.ap[:, :n], ps[6].ap[:, :n], AF.Ln, [ps[6]], [rs], scale=1.0 / D, bias=EPS)
                    S.act(rs.ap[:, :n], rs.ap[:, :n], AF.Exp, [rs], [rs], scale=-0.5)
                    for k in range(KD):
                        S.stt(x1.ap[:, k, :n], x1.ap[:, k, :n], fn.ap[:, k:k + 1], rs.ap[:, :n], ALU.mult, ALU.mult, [x1, fn, rs], [x1])
                for kg in range(0, KD, 8):
                    k2 = min(KD, kg + 8)
                    S.dma("sp", xout[kg * 128:k2 * 128, t0:t0 + n].rearrange("(k p) t -> p k t", p=128), x1.ap[:, kg:k2, :n], reads=[x1], writes=[d_out])
        S.barrier()


def build_k2(cfg, ntq, last, moe):
    D, KD, MIX = cfg.D, cfg.KD, cfg.MIX
    KM = MIX // 128
    tiles, TL = k2_tiles(cfg, ntq, last)
    NF = (cfg.DFE if moe else cfg.DFF) // 128
    NE = cfg.NEXP
    nc = bass.Bass("TRN2", target_bir_lowering=False)
    dt_in = lambda n, sh: nc.dram_tensor(n, sh, F32, kind="ExternalInput").ap()
    xT = dt_in("xT", [D, TL]); mixT = dt_in("mixT", [MIX, TL])
    I = {"w_out": dt_in("w_out", [MIX, D]), "ada_w": dt_in("ada_w", [D, 4 * D]), "ada_b": dt_in("ada_b", [128, 4 * KD]),
         "ccol": dt_in("ccol", [128, KD, 2]), "nfw": dt_in("nfw", [128, KD]), "consts": dt_in("consts", [128, len(CNAMES), 128])}
    if moe:
        I["wg"] = dt_in("wg", [NE, D, NF * 128]); I["wu"] = dt_in("wu", [NE, D, NF * 128]); I["wd"] = dt_in("wd", [NE, NF * 128, D])
        I["rw"] = dt_in("rw", [128, KD, 8]); I["selc"] = dt_in("selc", [8, 8, 128])
    else:
        I["wg"] = dt_in("wg", [D, NF * 128]); I["wu"] = dt_in("wu", [D, NF * 128]); I["wd"] = dt_in("wd", [NF * 128, D])
    if last:
        I["fnw"] = dt_in("fnw", [128, KD])
    xout = nc.dram_tensor("xout", [D, TL], F32, kind="ExternalOutput").ap()

    def load_mix(S, mh, t0, n, kind):
        for kg in range(0, KM, 8):
            k2 = min(KM, kg + 8)
            S.dma("pool", mh.ap[:, kg:k2, :n], mixT[kg * 128:k2 * 128, t0:t0 + n].rearrange("(k p) t -> p k t", p=128), writes=[mh])

    with contextlib.ExitStack() as es:
        S = Sched(nc)
        ps = [Buf(es.enter_context(nc.psum_tensor(f"ps{i}", [128, 512], F32)), f"ps{i}") for i in range(8)]
        emit_k2(nc, S, ps, cfg, ntq, last, moe, I, "", xT, load_mix, xout, Buf(None, "d_out"))
    return nc


SELC = np.zeros((8, 8, 128), np.float32)
for _e in range(8):
    SELC[_e, _e, :] = 1.0


def k2_inputs(cfg, inp, layer, b, xT_loc, mixT_loc, last, moe):
    D = cfg.D
    m = {"xT": xT_loc, "mixT": mixT_loc, "w_out": inp["w_out"][layer],
         "ada_w": np.ascontiguousarray(inp["ada_w"][layer][:, 2 * D:6 * D]),
         "ada_b": col_layout(inp["ada_b"][layer][2 * D:6 * D]),
         "ccol": np.ascontiguousarray(np.stack([col_layout(inp["c"][b]), col_layout(inp["c_ctx"])], axis=-1)),
         "nfw": col_layout(inp["norm_ffn_w"][layer]), "consts": CPACK}
    i = layer // 2
    if moe:
        m["wg"] = inp["moe_w_gate"][i]; m["wu"] = inp["moe_w_up"][i]; m["wd"] = inp["moe_w_down"][i]
        m["rw"] = np.ascontiguousarray(inp["moe_router"][i].reshape(cfg.KD, 128, 8).transpose(1, 0, 2))
        m["selc"] = SELC
    else:
        m["wg"] = inp["dense_w_gate"][i]; m["wu"] = inp["dense_w_up"][i]; m["wd"] = inp["dense_w_down"][i]
    if last:
        m["fnw"] = col_layout(inp["final_norm_w"])
    return {k: np.ascontiguousarray(v, dtype=np.float32) for k, v in m.items()}


def tq_index(cfg, ntq, tq, last):
    lat = cfg.CTX + np.arange(tq * (cfg.SEQ // ntq), (tq + 1) * (cfg.SEQ // ntq))
    if last:
        return lat
    c = np.arange(tq * (cfg.CTX // ntq), (tq + 1) * (cfg.CTX // ntq))
    return np.concatenate([c, lat])


def run_pipeline(cfg, inp, ntq, depth=2, verbose=False):
    B, NQ = cfg.B, cfg.NQ
    tabs = rope_tables(cfg, 64) + rope_tables(cfg, 128)
    xT = [np.ascontiguousarray(np.concatenate([inp["ctx"][b], inp["x"][b]], axis=0).T) for b in range(B)]
    out = None
    for layer in range(depth):
        last = layer == depth - 1
        moe = layer % 2 == 1
        lam_init = 0.8 - 0.6 * math.exp(-0.3 * layer)
        nc1 = build_k1(cfg, last, lam_init)
        maps = [k1_inputs(cfg, inp, layer, b, q, xT[b], tabs) for b in range(B) for q in range(NQ)]
        res = run_bass_kernel_spmd(nc1, maps, core_ids=list(range(len(maps))))
        mix = [assemble_mix(cfg, [res.results[b * NQ + q]["mixT"] for q in range(NQ)]) for b in range(B)]
        del maps, res
        nc2 = build_k2(cfg, ntq, last, moe)
        maps = []
        for b in range(B):
            for tq in range(ntq):
                idx = tq_index(cfg, ntq, tq, last)
                maps.append(k2_inputs(cfg, inp, layer, b, xT[b][:, idx], mix[b][:, idx], last, moe))
        res = run_bass_kernel_spmd(nc2, maps, core_ids=list(range(len(maps))))
        if last:
            out = np.empty((B, cfg.SEQ, cfg.D), np.float32)
            for b in range(B):
                for tq in range(ntq):
                    idx = tq_index(cfg, ntq, tq, True) - cfg.CTX
                    out[b, idx, :] = res.results[b * ntq + tq]["xout"].T
        else:
            for b in range(B):
                for tq in range(ntq):
                    idx = tq_index(cfg, ntq, tq, False)
                    xT[b][:, idx] = res.results[b * ntq + tq]["xout"]
        del maps, res
    return out, xT


def mix_perm(cfg):
    NQ = cfg.NQ
    RW, SW = 256 * NQ, 512 * NQ
    idx = []
    for q in range(NQ):
        idx += list(range(q * 256, (q + 1) * 256))
        idx += list(range(RW + q * 512, RW + (q + 1) * 512))
        idx += list(range(RW + SW + q * 256, RW + SW + (q + 1) * 256))
    return np.array(idx)


def build_fused(cfg, stop=99):
    D, KD, T, NQ, CTX, SEQ, MIX = cfg.D, cfg.KD, cfg.T, cfg.NQ, cfg.CTX, cfg.SEQ, cfg.MIX
    KM = MIX // 128
    TLc, TLl = CTX // NQ, SEQ // NQ
    TL0 = TLc + TLl
    NTJ = TLl // 512
    NE = cfg.NEXP
    groups = [[b * NQ + q for q in range(NQ)] for b in range(cfg.B)]
    nc = bass.Bass("TRN2", target_bir_lowering=False)
    dt_in = lambda n, sh, dt=F32: nc.dram_tensor(n, sh, dt, kind="ExternalInput").ap()
    dt_sc = lambda n, sh, dt=F32: nc.dram_tensor(n, sh, dt, kind="Internal").ap()
    xTb = dt_in("xTb", [D, T]); xTl = dt_in("xTl", [D, TL0])
    sh = {"ccol": dt_in("ccol", [128, KD, 2]), "consts": dt_in("consts", [128, len(CNAMES), 128]), "selc": dt_in("selc", [8, 8, 128])}
    for n_ in ("cosd", "sind", "cosr", "sinr"):
        sh[n_] = dt_in(n_, [128, T])
    L = []
    for l in range(2):
        d = {"w_in": dt_in(f"w_in{l}", [D, cfg.PC]), "ada_w": dt_in(f"ada_w{l}", [D, 6 * D]), "ada_b": dt_in(f"ada_b{l}", [128, 6 * KD]),
             "nmw": dt_in(f"nmw{l}", [128, KD]), "nfw": dt_in(f"nfw{l}", [128, KD]), "w_out": dt_in(f"w_out{l}", [MIX, D]),
             "midx": dt_in(f"midx{l}", [128, KM, NTJ + 1], mybir.dt.int32)}
        for n_, shp in K1_SMALL:
            d[n_] = dt_in(f"{n_}{l}", shp)
        L.append(d)
    NFd = cfg.DFF // 128; NFe = cfg.DFE // 128
    L[0]["wg"] = dt_in("wg0", [D, NFd * 128]); L[0]["wu"] = dt_in("wu0", [D, NFd * 128]); L[0]["wd"] = dt_in("wd0", [NFd * 128, D])
    L[1]["wg"] = dt_in("wg1", [NE, D, NFe * 128]); L[1]["wu"] = dt_in("wu1", [NE, D, NFe * 128]); L[1]["wd"] = dt_in("wd1", [NE, NFe * 128, D])
    L[1]["rw"] = dt_in("rw1", [128, KD, 8]); L[1]["fnw"] = dt_in("fnw", [128, KD])
    xout = nc.dram_tensor("xout", [D, TLl], F32, kind="ExternalOutput").ap()
    hT = dt_sc("hT", [D, T], BF16); projT = dt_sc("projT", [cfg.PC, T]); convT = dt_sc("convT", [1024, T])
    mixl = [dt_sc(f"mixl{l}", [NQ * NTJ * 1024, 512], BF16) for l in range(2)]
    mixlg = [dt_sc(f"mixlg{l}", [NQ * NQ * NTJ * 1024, 512], BF16) for l in range(2)]
    mixc = dt_sc("mixc", [NQ * 1024, TLc], BF16); mixcg = dt_sc("mixcg", [NQ * NQ * 1024, TLc], BF16)
    x1loc = dt_sc("x1loc", [D, TL0]); xg = dt_sc("xg", [NQ * D, TL0])
    d_xg = Buf(None, "d_xg"); d_x1 = Buf(None, "d_x1")
    xg5 = xg.rearrange("(k h q r) t -> h q r k t", h=2, q=NQ, r=64)

    with contextlib.ExitStack() as es:
        S = Sched(nc)
        ps = [Buf(es.enter_context(nc.psum_tensor(f"ps{i}", [128, 512], F32)), f"ps{i}") for i in range(8)]
        midx_sb = [Buf(es.enter_context(nc.sbuf_tensor(f"midx_sb{l}", [128, KM, NTJ + 1], mybir.dt.int32)), f"midx{l}") for l in range(2)]
        for l in range(2):
            S.dma("sp", midx_sb[l].ap[:], L[l]["midx"], writes=[midx_sb[l]])
        for l in range(2):
            last = l == 1
            lam_init = 0.8 - 0.6 * math.exp(-0.3 * l)
            I1 = dict(sh); I1.update(L[l]); I1["ada_w"] = L[l]["ada_w"][:, 0:2 * D]; I1["ada_b"] = L[l]["ada_b"][:, 0:2 * KD]
            I1["hT"], I1["projT"], I1["convT"] = hT, projT, convT
            d_mix = Buf(None, f"d_mix{l}"); d_mixg = Buf(None, f"d_mixg{l}")

            def load_x(S_, xt, t0, n, l=l):
                if l == 0:
                    for kg in range(0, KD, 8):
                        k2 = min(KD, kg + 8)
                        S_.dma("sp", xt.ap[:, kg:k2, :n], xTb[kg * 128:k2 * 128, t0:t0 + n].rearrange("(k p) t -> p k t", p=128), writes=[xt])
                elif t0 < CTX:
                    for tq in range(NQ):
                        for kg in range(0, KD, 8):
                            k2 = min(KD, kg + 8)
                            for h in range(2):
                                S_.dma("sp", xt.ap[h * 64:(h + 1) * 64, kg:k2, tq * TLc:(tq + 1) * TLc], xg5[h, tq, :, kg:k2, 0:TLc], reads=[d_xg], writes=[xt])
                else:
                    lt = t0 - CTX
                    tq = lt // TLl; off = TLc + lt % TLl
                    for kg in range(0, KD, 8):
                        k2 = min(KD, kg + 8)
                        for h in range(2):
                            S_.dma("sp", xt.ap[h * 64:(h + 1) * 64, kg:k2, :n], xg5[h, tq, :, kg:k2, off:off + n], reads=[d_xg], writes=[xt])

            def store_mix(S_, r0, nr, t0, n, src, sbuf, dm, l=l):
                if t0 < CTX:
                    for tq in range(NQ):
                        S_.dma("pool", mixc[tq * 1024 + r0:tq * 1024 + r0 + nr, :], src[:, tq * TLc:(tq + 1) * TLc], reads=[sbuf], writes=[dm])
                else:
                    lt = t0 - CTX
                    tq = lt // TLl; j = (lt % TLl) // 512
                    base = (tq * NTJ + j) * 1024 + r0
                    S_.dma("pool", mixl[l][base:base + nr, :], src, reads=[sbuf], writes=[dm])

            emit_k1(nc, S, ps, cfg, last, lam_init, I1, f"a{l}_", load_x, store_mix, d_mix)
            if stop <= 4 * l + 0:
                break
            for c in range(NQ * NTJ):
                S.cc_allgather(mixl[l][c * 1024:(c + 1) * 1024, :], mixlg[l][c * NQ * 1024:(c + 1) * NQ * 1024, :], groups, reads=[d_mix], writes=[d_mixg])
            if not last:
                S.cc_allgather(mixc, mixcg, groups, reads=[d_mix], writes=[d_mixg])
            S.barrier()
            if stop <= 4 * l + 1:
                break
            I2 = dict(sh); I2.update(L[l]); I2["ada_w"] = L[l]["ada_w"][:, 2 * D:6 * D]; I2["ada_b"] = L[l]["ada_b"][:, 2 * KD:6 * KD]
            tl_c = 0 if last else TLc

            def load_mix(S_, mh, t0, n, kind, l=l, tl_c=tl_c):
                if kind == 1:
                    src = mixcg; col = NTJ
                else:
                    src = mixlg[l]; col = (t0 - tl_c) // 512
                for km in range(KM):
                    S_.idma(mh.ap[:, km, :n], src, midx_sb[l].ap[:, km, col:col + 1], reads=[d_mixg, midx_sb[l]], writes=[mh])

            if not last:
                emit_k2(nc, S, ps, cfg, NQ, False, False, I2, f"b{l}_", xTl, load_mix, x1loc, d_x1)
                if stop <= 4 * l + 2:
                    break
                for c in range(D // 64):
                    S.cc_allgather(x1loc[c * 64:(c + 1) * 64, :], xg[c * NQ * 64:(c + 1) * NQ * 64, :], groups, reads=[d_x1], writes=[d_xg])
                S.barrier()
            else:
                d_out = Buf(None, "d_out")
                emit_k2(nc, S, ps, cfg, NQ, True, True, I2, f"b{l}_", x1loc[:, TLc:TL0], load_mix, xout, d_out)
    return nc


def fused_inputs(cfg, inp, b, q, shared):
    D, KD, NQ, CTX, SEQ, MIX = cfg.D, cfg.KD, cfg.NQ, cfg.CTX, cfg.SEQ, cfg.MIX
    KM = MIX // 128
    TLc, TLl = CTX // NQ, SEQ // NQ
    NTJ = TLl // 512
    xTb = shared["xT"][b]
    m = {"xTb": xTb, "xTl": np.ascontiguousarray(xTb[:, tq_index(cfg, NQ, q, False)])}
    m["ccol"] = np.ascontiguousarray(np.stack([col_layout(inp["c"][b]), col_layout(inp["c_ctx"])], axis=-1))
    m["consts"] = CPACK; m["selc"] = SELC
    m["cosd"], m["sind"], m["cosr"], m["sinr"] = shared["tabs"]
    for l in range(2):
        k1 = k1_inputs(cfg, inp, l, b, q, xTb, shared["tabs"])
        m[f"w_in{l}"] = k1["w_in"]
        for n_, _ in K1_SMALL:
            m[f"{n_}{l}"] = k1[n_]
        m[f"ada_w{l}"] = inp["ada_w"][l]
        m[f"ada_b{l}"] = col_layout(inp["ada_b"][l])
        m[f"nmw{l}"] = col_layout(inp["norm_mix_w"][l]); m[f"nfw{l}"] = col_layout(inp["norm_ffn_w"][l])
        m[f"w_out{l}"] = shared["w_out"][l]
        ntj1 = NTJ + 1
        idx = np.zeros((128, KM, ntj1), np.int64)
        p = np.arange(128)
        for km in range(KM):
            qq = km // 8; rr = (km % 8) * 128 + p
            for j in range(NTJ):
                idx[:, km, j] = (q * NTJ + j) * (NQ * 1024) + qq * 1024 + rr
            idx[:, km, NTJ] = qq * (NQ * 1024) + q * 1024 + rr
        m[f"midx{l}"] = idx.astype(np.int32)
    m["wg0"] = inp["dense_w_gate"][0]; m["wu0"] = inp["dense_w_up"][0]; m["wd0"] = inp["dense_w_down"][0]
    m["wg1"] = inp["moe_w_gate"][0]; m["wu1"] = inp["moe_w_up"][0]; m["wd1"] = inp["moe_w_down"][0]
    m["rw1"] = np.ascontiguousarray(inp["moe_router"][0].reshape(KD, 128, 8).transpose(1, 0, 2))
    m["fnw"] = col_layout(inp["final_norm_w"])
    out = {}
    for k, v in m.items():
        out[k] = np.ascontiguousarray(v, dtype=(np.int32 if k.startswith("midx") else np.float32))
    return out


def run_fused(cfg, inp, stop=99):
    B, NQ = cfg.B, cfg.NQ
    TLl = cfg.SEQ // NQ
    perm = mix_perm(cfg)
    shared = {"tabs": rope_tables(cfg, 64) + rope_tables(cfg, 128),
              "xT": [np.ascontiguousarray(np.concatenate([inp["ctx"][b], inp["x"][b]], axis=0).T) for b in range(B)],
              "w_out": [np.ascontiguousarray(inp["w_out"][l][perm]) for l in range(2)]}
    nc = build_fused(cfg, stop)
    maps = [fused_inputs(cfg, inp, b, q, shared) for b in range(B) for q in range(NQ)]
    res = run_bass_kernel_spmd(nc, maps, core_ids=list(range(len(maps))))
    out = np.empty((B, cfg.SEQ, cfg.D), np.float32)
    for b in range(B):
        for q in range(NQ):
            out[b, q * TLl:(q + 1) * TLl, :] = res.results[b * NQ + q]["xout"].T
    return out


def kernel(**inputs):
    inp = {k: np.asarray(v) for k, v in inputs.items()}
    return run_fused(FULL, inp)
```

```python
import contextlib
import math
import numpy as np
import concourse.bass as bass
import concourse.mybir as mybir
from concourse.bass_utils import run_bass_kernel_spmd

F32 = mybir.dt.float32
BF16 = mybir.dt.bfloat16
AF = mybir.ActivationFunctionType
ALU = mybir.AluOpType
AX = mybir.AxisListType
EPS = 1e-6
SAME_SYNC = True


class Buf:
    __slots__ = ("ap", "name", "w", "r")

    def __init__(self, ap=None, name=""):
        self.ap = ap
        self.name = name
        self.w = None
        self.r = {}


class Sched:
    ENG = ("pe", "act", "dve", "pool", "sp")

    def __init__(self, nc, same_engine_sync=SAME_SYNC, ring=12):
        self.nc = nc
        self.engs = {"pe": nc.tensor, "act": nc.scalar, "dve": nc.vector, "pool": nc.gpsimd, "sp": nc.sync}
        self.semh = {}
        self.cnt = {}
        self.seen = {e: {} for e in self.ENG}
        self.same = same_engine_sync
        for e in self.ENG:
            self.semh[e] = nc.alloc_semaphore(name=f"s_{e}")
            self.cnt[e] = 0
        self.ring = ring
        self.dma_i = {}
        self.dma_val = {}
        for q in ("sp", "pool"):
            self.dma_i[q] = 0
            for s in range(ring):
                k = (q, s)
                self.semh[k] = nc.alloc_semaphore(name=f"d_{q}{s}")
                self.dma_val[k] = 0
        self.n_ins = 0
        self.n_wait = 0
        self.semh["cc"] = nc.alloc_semaphore(name="s_cc")
        self.cc_val = 0

    def cc_allgather(self, in_ap, out_ap, groups, reads=(), writes=()):
        self._deps("pool", reads, writes)
        ins = self.engs["pool"].collective_compute("AllGather", ALU.bypass, replica_groups=groups, ins=[in_ap], outs=[out_ap])
        self.cc_val += 1
        ins.then_inc(self.semh["cc"], 1)
        self.n_ins += 1
        self._record(("cc", self.cc_val), reads, writes)
        return ins

    def idma(self, out_ap, in_ap, idx_ap, reads=(), writes=()):
        q = "pool"
        self._deps(q, reads, writes)
        slot = self.dma_i[q] % self.ring
        self.dma_i[q] += 1
        k = (q, slot)
        prev = self.dma_val[k]
        if prev > 0:
            self._wait(q, (k, prev))
        self.dma_val[k] = prev + 16
        ins = self.engs[q].indirect_dma_start(out=out_ap, out_offset=None, in_=in_ap, in_offset=bass.IndirectOffsetOnAxis(ap=idx_ap, axis=0))
        ins.then_inc(self.semh[k], 16)
        self.n_ins += 1
        self._record((k, prev + 16), reads, writes)
        return ins

    def _wait(self, eng, ev):
        if ev is None:
            return
        key, val = ev
        if key == eng and (eng == "pe" or not self.same):
            return
        if self.seen[eng].get(key, 0) >= val:
            return
        self.engs[eng].wait_ge(self.semh[key], val)
        self.seen[eng][key] = val
        self.n_wait += 1

    def _deps(self, eng, reads, writes):
        for b in reads:
            self._wait(eng, b.w)
        for b in writes:
            self._wait(eng, b.w)
            for k, v in list(b.r.items()):
                self._wait(eng, (k, v))

    def _record(self, ev, reads, writes):
        k, v = ev
        for b in reads:
            if b.r.get(k, 0) < v:
                b.r[k] = v
        for b in writes:
            b.w = ev
            b.r = {}

    def op(self, eng, fn, reads=(), writes=(), signal=True):
        self._deps(eng, reads, writes)
        ins = fn(self.engs[eng])
        self.n_ins += 1
        if signal:
            self.cnt[eng] += 1
            ins.then_inc(self.semh[eng], 1)
            ev = (eng, self.cnt[eng])
        else:
            ev = (eng, self.cnt[eng] + 1)
        self._record(ev, reads, writes)
        return ins

    def dma(self, q, out_ap, in_ap, reads=(), writes=()):
        self._deps(q, reads, writes)
        slot = self.dma_i[q] % self.ring
        self.dma_i[q] += 1
        k = (q, slot)
        prev = self.dma_val[k]
        if prev > 0:
            self._wait(q, (k, prev))
        self.dma_val[k] = prev + 16
        ins = self.engs[q].dma_start(out=out_ap, in_=in_ap)
        ins.then_inc(self.semh[k], 16)
        self.n_ins += 1
        self._record((k, prev + 16), reads, writes)
        return ins

    def barrier(self):
        evs = [(e, self.cnt[e]) for e in self.ENG if self.cnt[e] > 0]
        evs += [(k, v) for k, v in self.dma_val.items() if v > 0]
        if self.cc_val > 0:
            evs.append(("cc", self.cc_val))
        for e in self.ENG:
            for ev in evs:
                if ev[0] == e and e == "pe":
                    continue
                key, val = ev
                if self.seen[e].get(key, 0) >= val:
                    continue
                self.engs[e].wait_ge(self.semh[key], val)
                self.seen[e][key] = val

    def act(self, out, in_, func, reads, writes, **kw):
        return self.op("act", lambda e: e.activation(out=out, in_=in_, func=func, **kw), reads, writes)

    def tt(self, eng, out, in0, in1, op, reads, writes):
        return self.op(eng, lambda e: e.tensor_tensor(out=out, in0=in0, in1=in1, op=op), reads, writes)

    def ts(self, eng, out, in0, s1, s2, op0, op1, reads, writes):
        if op1 is None:
            return self.op(eng, lambda e: e.tensor_scalar(out=out, in0=in0, scalar1=s1, scalar2=None, op0=op0), reads, writes)
        return self.op(eng, lambda e: e.tensor_scalar(out=out, in0=in0, scalar1=s1, scalar2=s2, op0=op0, op1=op1), reads, writes)

    def stt(self, out, in0, scalar, in1, op0, op1, reads, writes):
        return self.op("dve", lambda e: e.scalar_tensor_tensor(out=out, in0=in0, scalar=scalar, in1=in1, op0=op0, op1=op1), reads, writes)

    def mm(self, out, lhsT, rhs, reads, writes, start=True, stop=True, signal=True):
        return self.op("pe", lambda e: e.matmul(out, lhsT, rhs, start=start, stop=stop), reads, writes, signal=signal)

    def tr(self, out, in_, ident, reads, writes):
        return self.op("pe", lambda e: e.transpose(out, in_, ident), reads, writes)

    def copy(self, eng, out, in_, reads, writes):
        if eng == "act":
            return self.act(out, in_, AF.Copy, reads, writes)
        return self.op(eng, lambda e: e.tensor_copy(out=out, in_=in_), reads, writes)


class Cfg:
    def __init__(s, D, SEQ, CTX, NQ, DFF, DFE, NEXP=8, B=2):
        s.D, s.SEQ, s.CTX, s.NQ, s.DFF, s.DFE, s.NEXP, s.B = D, SEQ, CTX, NQ, DFF, DFE, NEXP, B
        s.KD = D // 128
        s.T = CTX + SEQ
        s.NCH = s.T // 128
        s.NCC = CTX // 128
        s.MIX = 1024 * NQ
        s.PC = 3344
        assert CTX % 128 == 0 and CTX <= 512 and SEQ % 512 == 0
        s.tiles = [(0, CTX)] + [(CTX + 512 * i, 512) for i in range(SEQ // 512)]
        s.GRID_W = 64


FULL = Cfg(4096, 8192, 256, 4, 11008, 4096)


def rope_tables(cfg, dh):
    seq = cfg.SEQ
    rows = seq // cfg.GRID_W
    r = np.repeat(np.arange(rows, dtype=np.float32), cfg.GRID_W)
    col = np.tile(np.arange(cfg.GRID_W, dtype=np.float32), rows)
    nf = dh // 4
    inv = (10000.0 ** (-np.arange(nf, dtype=np.float32) / nf)).astype(np.float32)
    ar = r[:, None] * inv
    ac = col[:, None] * inv
    ang = np.concatenate([ar, ar, ac, ac], axis=-1)
    cos = np.cos(ang).astype(np.float32)
    sin = np.sin(ang).astype(np.float32)
    cosT = np.concatenate([np.ones((dh, cfg.CTX), np.float32), cos.T], axis=1)
    sinT = np.concatenate([np.zeros((dh, cfg.CTX), np.float32), sin.T], axis=1)
    reps = 128 // dh
    return np.ascontiguousarray(np.tile(cosT, (reps, 1))), np.ascontiguousarray(np.tile(sinT, (reps, 1)))


def rot_matrix(dh):
    R = np.zeros((128, 128), np.float32)
    q = dh // 4
    for base in range(0, 128, dh):
        for m in range(dh):
            quarter = m // q
            if quarter % 2 == 0:
                R[base + m + q, base + m] = -1.0
            else:
                R[base + m - q, base + m] = 1.0
    return R


def const_pack():
    j = np.arange(128)[:, None].astype(np.float32)
    i = np.arange(128)[None, :].astype(np.float32)
    mats = {}
    mats["ident"] = np.eye(128, dtype=np.float32)
    mats["ones"] = np.ones((128, 128), np.float32)
    mats["dpos"] = np.maximum(i - j, 0.0)
    mats["ufs"] = (i > j).astype(np.float32)
    mats["dneg"] = np.maximum(j - i, 0.0)
    mats["ubs"] = (j > i).astype(np.float32)
    mats["i2"] = 2.0 * np.eye(128, dtype=np.float32)
    mats["uf"] = (j <= i).astype(np.float32)
    mats["ub"] = (j >= i).astype(np.float32)
    mats["sl"] = (j > i).astype(np.float32)
    mats["su"] = (j < i).astype(np.float32)
    mats["rowip1"] = np.broadcast_to(i + 1.0, (128, 128)).copy()
    mats["row128mi"] = np.broadcast_to(128.0 - i, (128, 128)).copy()
    mats["rot64"] = rot_matrix(64)
    mats["rot128"] = rot_matrix(128)
    cols = np.zeros((128, 128), np.float32)
    cols[:, 0] = 127.0 - np.arange(128)
    cols[:, 1] = np.arange(128)
    mats["cols"] = cols
    names = list(mats.keys())
    arr = np.stack([mats[n] for n in names], axis=1)
    return names, np.ascontiguousarray(arr)


CNAMES, CPACK = const_pack()
CI = {n: k for k, n in enumerate(CNAMES)}


def emit_k1(nc, S, ps, cfg, last, lam_init, I, pf, load_x, store_mix, d_mix):
    D, KD, T, NCH, NCC, CTX = cfg.D, cfg.KD, cfg.T, cfg.NCH, cfg.NCC, cfg.CTX
    tiles = cfg.tiles
    w_in, ada_w, ada_b, ccol, nmw, consts = I["w_in"], I["ada_w"], I["ada_b"], I["ccol"], I["nmw"], I["consts"]
    cosd, sind, cosr, sinr = I["cosd"], I["sind"], I["cosr"], I["sinr"]
    rdl, rnw, cw, cb, dtb, alog, dsk, snw, dlam, dnw = (I[k] for k in ("rdl", "rnw", "cw", "cb", "dtb", "alog", "dsk", "snw", "dlam", "dnw"))
    hT, projT, convT = I["hT"], I["projT"], I["convT"]
    d_hT = Buf(None, "d_hT"); d_proj = Buf(None, "d_proj"); d_conv = Buf(None, "d_conv")

    with contextlib.ExitStack() as es0:
        def SB(es, n, sh, dt=F32):
            return Buf(es.enter_context(nc.sbuf_tensor(pf + n, sh, dt)), n)
        C = SB(es0, "C", [128, len(CNAMES), 128])
        Cb = SB(es0, "Cb", [128, 2, 128], BF16)
        S.dma("sp", C.ap[:], consts, writes=[C])
        S.copy("dve", Cb.ap[:, 0, :], C.ap[:, CI["ones"], :], [C], [Cb])
        S.copy("dve", Cb.ap[:, 1, :], C.ap[:, CI["ident"], :], [C], [Cb])
        cm = lambda n: C.ap[:, CI[n], :]
        ones_bf = Cb.ap[:, 0, :]
        AB = SB(es0, "AB", [128, KD, 4])

        with contextlib.ExitStack() as es:
            sc = SB(es, "sc", [128, KD, 2]); adb = SB(es, "adb", [128, 2 * KD]); nm = SB(es, "nm", [128, KD])
            modc = SB(es, "modc", [128, 2 * KD, 2])
            wa = [SB(es, f"wa{i}", [128, KD, 512]) for i in range(2)]
            S.dma("sp", sc.ap[:], ccol, writes=[sc]); S.dma("sp", adb.ap[:], ada_b, writes=[adb]); S.dma("sp", nm.ap[:], nmw, writes=[nm])
            S.act(sc.ap[:], sc.ap[:], AF.Silu, [sc], [sc])
            nfc = 2 * KD
            pm = ps[0]
            for fg in range(0, nfc, 4):
                w = wa[(fg // 4) % 2]
                for kg in range(0, KD, 8):
                    k2 = min(KD, kg + 8)
                    S.dma("sp", w.ap[:, kg:k2, :], ada_w[kg * 128:k2 * 128, fg * 128:(fg + 4) * 128].rearrange("(k p) c -> p k c", p=128), writes=[w])
                for f in range(fg, fg + 4):
                    for k in range(KD):
                        S.mm(pm.ap[:, 2 * f:2 * f + 2], w.ap[:, k, (f - fg) * 128:(f - fg + 1) * 128], sc.ap[:, k, :], [w, sc], [pm],
                             start=(k == 0), stop=(k == KD - 1), signal=(k == KD - 1))
            for j in range(2):
                S.tt("dve", modc.ap[:, :, j], pm.ap[:, 0:2 * nfc].rearrange("p (f j) -> p f j", j=2)[:, :, j], adb.ap[:], ALU.add, [pm, adb], [modc])
            for j in range(2):
                S.stt(AB.ap[:, :, j], modc.ap[:, KD:2 * KD, j], 1.0, nm.ap[:], ALU.add, ALU.mult, [modc, nm], [AB])
                S.copy("dve", AB.ap[:, :, 2 + j], modc.ap[:, 0:KD, j], [modc], [AB])
        S.barrier()

        with contextlib.ExitStack() as es:
            xt = SB(es, "xt", [128, KD, 512]); ht = SB(es, "ht", [128, KD, 512], BF16)
            sq = [SB(es, f"sq{i}", [128, 512], BF16) for i in range(2)]
            tmp = [SB(es, f"tmp{i}", [128, 512]) for i in range(2)]
            rs = SB(es, "rs", [128, 512])
            for (t0, n) in tiles:
                j = 1 if t0 < CTX else 0
                load_x(S, xt, t0, n)
                for k in range(KD):
                    s_ = sq[k % 2]
                    S.act(s_.ap[:, :n], xt.ap[:, k, :n], AF.Square, [xt], [s_])
                    S.mm(ps[0].ap[:, :n], ones_bf, s_.ap[:, :n], [s_, Cb], [ps[0]], start=(k == 0), stop=(k == KD - 1))
                S.act(rs.ap[:, :n], ps[0].ap[:, :n], AF.Ln, [ps[0]], [rs], scale=1.0 / D, bias=EPS)
                S.act(rs.ap[:, :n], rs.ap[:, :n], AF.Exp, [rs], [rs], scale=-0.5)
                for k in range(KD):
                    t_ = tmp[k % 2]
                    S.tt("dve", t_.ap[:, :n], xt.ap[:, k, :n], rs.ap[:, :n], ALU.mult, [xt, rs], [t_])
                    S.act(ht.ap[:, k, :n], t_.ap[:, :n], AF.Identity, [t_, AB], [ht], scale=AB.ap[:, k, j:j + 1], bias=AB.ap[:, k, 2 + j:3 + j])
                for kg in range(0, KD, 8):
                    k2 = min(KD, kg + 8)
                    S.dma("sp", hT[kg * 128:k2 * 128, t0:t0 + n].rearrange("(k p) t -> p k t", p=128), ht.ap[:, kg:k2, :n], reads=[ht], writes=[d_hT])
        S.barrier()

        groups = [(0, 1024), (1024, 1552), (2576, 768)]
        with contextlib.ExitStack() as es:
            wg = SB(es, "wg", [128, KD, 1552], BF16)
            hb = [SB(es, f"hb{i}", [128, KD, 512], BF16) for i in range(2)]
            stg = [SB(es, f"stg{i}", [128, 512]) for i in range(4)]
            si = 0; pi = 0; ti = 0
            for (c0, ncols) in groups:
                for kg in range(0, KD, 8):
                    k2 = min(KD, kg + 8)
                    S.dma("pool", wg.ap[:, kg:k2, :ncols], w_in[kg * 128:k2 * 128, c0:c0 + ncols].rearrange("(k p) c -> p k c", p=128), writes=[wg])
                for (t0, n) in tiles:
                    h = hb[ti % 2]; ti += 1
                    for kg in range(0, KD, 8):
                        k2 = min(KD, kg + 8)
                        S.dma("sp", h.ap[:, kg:k2, :n], hT[kg * 128:k2 * 128, t0:t0 + n].rearrange("(k p) t -> p k t", p=128), reads=[d_hT], writes=[h])
                    cs = 0
                    while cs < ncols:
                        m = min(128, ncols - cs)
                        p = ps[pi % 4]; pi += 1
                        for k in range(KD):
                            S.mm(p.ap[:m, :n], wg.ap[:, k, cs:cs + m], h.ap[:, k, :n], [wg, h], [p], start=(k == 0), stop=(k == KD - 1), signal=(k == KD - 1))
                        st = stg[si % 4]; si += 1
                        S.copy("act" if si % 2 else "dve", st.ap[:m, :n], p.ap[:m, :n], [p], [st])
                        S.dma("sp", projT[c0 + cs:c0 + cs + m, t0:t0 + n], st.ap[:m, :n], reads=[st], writes=[d_proj])
                        cs += m
        S.barrier()

        base_d = 2576
        with contextlib.ExitStack() as es:
            QT = SB(es, "QT", [128, T], BF16); KT = SB(es, "KT", [128, T], BF16); Vtm = SB(es, "Vtm", [128, NCH, 128], BF16)
            ld = [SB(es, f"ld{i}", [128, 512]) for i in range(2)]
            cs_t = SB(es, "cs_t", [128, 512]); sn_t = SB(es, "sn_t", [128, 512])
            t1 = SB(es, "t1", [128, 512]); t2 = SB(es, "t2", [128, 512]); sqb = SB(es, "sqb", [128, 512], BF16)
            P = [SB(es, f"P{i}", [128, 512], BF16) for i in range(4)]
            mx = SB(es, "mx", [128, 8]); negc = SB(es, "negc", [128, 2]); lamt = SB(es, "lamt", [128, 8]); lv = SB(es, "lv", [128, 4, 64])
            nwd = SB(es, "nwd", [128, 1]); rl = [SB(es, f"rl{i}", [128, 512]) for i in range(2)]
            ob = SB(es, "ob", [128, 512]); rsd = SB(es, "rsd", [128, 512]); oo = SB(es, "oo", [128, 512])
            S.dma("sp", lv.ap[:], dlam, writes=[lv]); S.dma("sp", nwd.ap[:], dnw, writes=[nwd])
            S.tt("dve", lv.ap[:, 0, :], lv.ap[:, 0, :], lv.ap[:, 1, :], ALU.mult, [lv], [lv])
            S.tt("dve", lv.ap[:, 2, :], lv.ap[:, 2, :], lv.ap[:, 3, :], ALU.mult, [lv], [lv])
            S.op("dve", lambda e: e.tensor_reduce(out=lamt.ap[:, 0:1], in_=lv.ap[:, 0, :], axis=AX.X, op=ALU.add), [lv], [lamt])
            S.op("dve", lambda e: e.tensor_reduce(out=lamt.ap[:, 1:2], in_=lv.ap[:, 2, :], axis=AX.X, op=ALU.add), [lv], [lamt])
            S.act(lamt.ap[:, 0:2], lamt.ap[:, 0:2], AF.Exp, [lamt], [lamt])
            S.tt("dve", lamt.ap[:, 2:3], lamt.ap[:, 1:2], lamt.ap[:, 0:1], ALU.subtract, [lamt], [lamt])
            S.ts("dve", lamt.ap[:, 3:4], lamt.ap[:, 2:3], -lam_init, None, ALU.add, None, [lamt], [lamt])
            S.ts("dve", nwd.ap[:], nwd.ap[:], 1.0 - lam_init, None, ALU.mult, None, [nwd], [nwd])
            neglam = lamt.ap[:, 3:4]
            for hd in range(2):
                rq = base_d + hd * 128; rk = base_d + 256 + hd * 128; rv = base_d + 512 + hd * 128
                S.op("dve", lambda e: e.memset(mx.ap[:], 0.0), [], [mx])
                li = 0
                for (t0, n) in tiles:
                    S.dma("sp", cs_t.ap[:, :n], cosd[:, t0:t0 + n], writes=[cs_t]); S.dma("sp", sn_t.ap[:, :n], sind[:, t0:t0 + n], writes=[sn_t])
                    for which, row, dst in ((0, rq, QT), (1, rk, KT)):
                        l = ld[li % 2]; li += 1
                        S.dma("sp", l.ap[:, :n], projT[row:row + 128, t0:t0 + n], reads=[d_proj], writes=[l])
                        S.act(sqb.ap[:, :n], l.ap[:, :n], AF.Square, [l], [sqb])
                        for m in range(2):
                            S.mm(ps[m].ap[:, :n], ones_bf[m * 64:(m + 1) * 64, :], sqb.ap[m * 64:(m + 1) * 64, :n], [sqb, Cb], [ps[m]])
                            S.op("dve", lambda e: e.tensor_reduce(out=mx.ap[:, 4 + m:5 + m], in_=ps[m].ap[:, :n], axis=AX.X, op=ALU.max), [ps[m]], [mx])
                            c_ = which * 2 + m
                            S.tt("dve", mx.ap[:, c_:c_ + 1], mx.ap[:, c_:c_ + 1], mx.ap[:, 4 + m:5 + m], ALU.max, [mx], [mx])
                        S.mm(ps[2].ap[:, :n], cm("rot64"), l.ap[:, :n], [C, l], [ps[2]])
                        S.tt("dve", t1.ap[:, :n], l.ap[:, :n], cs_t.ap[:, :n], ALU.mult, [l, cs_t], [t1])
                        S.tt("dve", t2.ap[:, :n], ps[2].ap[:, :n], sn_t.ap[:, :n], ALU.mult, [ps[2], sn_t], [t2])
                        S.tt("pool", dst.ap[:, t0:t0 + n], t1.ap[:, :n], t2.ap[:, :n], ALU.add, [t1, t2], [dst])
                    l = ld[li % 2]; li += 1
                    S.dma("sp", l.ap[:, :n], projT[rv:rv + 128, t0:t0 + n], reads=[d_proj], writes=[l])
                    for cc in range(n // 128):
                        S.tr(ps[3].ap[:, 0:128], l.ap[:, cc * 128:(cc + 1) * 128], cm("ident"), [l, C], [ps[3]])
                        S.copy("act", Vtm.ap[:, t0 // 128 + cc, :], ps[3].ap[:, 0:128], [ps[3]], [Vtm])
                S.tt("dve", mx.ap[:, 6:8], mx.ap[:, 0:2], mx.ap[:, 2:4], ALU.mult, [mx], [mx])
                S.act(mx.ap[:, 6:8], mx.ap[:, 6:8], AF.Ln, [mx], [mx])
                S.act(mx.ap[:, 6:8], mx.ap[:, 6:8], AF.Exp, [mx], [mx], scale=0.5)
                S.ts("dve", negc.ap[:], mx.ap[:, 6:8], -0.125, None, ALU.mult, None, [mx], [negc])
                qtiles = [(t0, n, list(range(NCH))) for (t0, n) in tiles if t0 >= CTX]
                if not last:
                    qtiles = [(0, CTX, list(range(NCC)))] + qtiles
                pj = 0
                for (t0, n, kcs) in qtiles:
                    for i, kc in enumerate(kcs):
                        for m in range(2):
                            pS = ps[pj % 4]; Pm = P[pj % 4]; pj += 1
                            S.mm(pS.ap[:, :n], KT.ap[m * 64:(m + 1) * 64, kc * 128:(kc + 1) * 128], QT.ap[m * 64:(m + 1) * 64, t0:t0 + n], [KT, QT], [pS])
                            S.act(Pm.ap[:, :n], pS.ap[:, :n], AF.Exp, [pS, negc], [Pm], scale=0.125, bias=negc.ap[:, m:m + 1])
                            S.mm(ps[4 + m].ap[:, :n], Vtm.ap[:, kc, :], Pm.ap[:, :n], [Vtm, Pm], [ps[4 + m]], start=(i == 0), stop=(i == len(kcs) - 1), signal=False)
                            S.mm(ps[6 + m].ap[:, :n], ones_bf, Pm.ap[:, :n], [Cb, Pm], [ps[6 + m]], start=(i == 0), stop=(i == len(kcs) - 1))
                    for m in range(2):
                        S.op("dve", lambda e: e.reciprocal(out=rl[m].ap[:, :n], in_=ps[6 + m].ap[:, :n]), [ps[6 + m]], [rl[m]])
                    S.tt("dve", t1.ap[:, :n], ps[4].ap[:, :n], rl[0].ap[:, :n], ALU.mult, [ps[4], rl[0]], [t1])
                    S.tt("dve", t2.ap[:, :n], ps[5].ap[:, :n], rl[1].ap[:, :n], ALU.mult, [ps[5], rl[1]], [t2])
                    S.stt(ob.ap[:, :n], t2.ap[:, :n], neglam, t1.ap[:, :n], ALU.mult, ALU.add, [t1, t2, lamt], [ob])
                    S.act(sqb.ap[:, :n], ob.ap[:, :n], AF.Square, [ob], [sqb])
                    S.mm(ps[0].ap[:, :n], ones_bf, sqb.ap[:, :n], [sqb, Cb], [ps[0]])
                    S.act(rsd.ap[:, :n], ps[0].ap[:, :n], AF.Ln, [ps[0]], [rsd], scale=1.0 / 128, bias=EPS)
                    S.act(rsd.ap[:, :n], rsd.ap[:, :n], AF.Exp, [rsd], [rsd], scale=-0.5)
                    S.stt(oo.ap[:, :n], ob.ap[:, :n], nwd.ap[:, 0:1], rsd.ap[:, :n], ALU.mult, ALU.mult, [ob, nwd, rsd], [oo])
                    store_mix(S, 768 + hd * 128, 128, t0, n, oo.ap[:, :n], oo, d_mix)
        S.barrier()

        ksc = 128.0 ** -0.5
        with contextlib.ExitStack() as es:
            QT = SB(es, "rQT", [128, T], BF16); KT = SB(es, "rKT", [128, T], BF16)
            Vtm = SB(es, "rVtm", [128, NCH, 128], BF16); Kf = SB(es, "Kf", [128, NCH, 128], BF16); Kb = SB(es, "Kb", [128, NCH, 128], BF16)
            Sf = SB(es, "Sf", [128, NCH, 128], BF16); Sbk = SB(es, "Sbk", [128, NCH, 128], BF16)
            ld = [SB(es, f"rld{i}", [128, 512]) for i in range(2)]
            cs_t = SB(es, "rcs", [128, 512]); sn_t = SB(es, "rsn", [128, 512])
            t1 = SB(es, "rt1", [128, 512]); t2 = SB(es, "rt2", [128, 512]); kro = SB(es, "kro", [128, 512])
            lg = SB(es, "lg", [128, 4]); M = SB(es, "M", [128, 128]); M2 = SB(es, "M2", [128, 128]); wfb = SB(es, "wfb", [128, 2])
            G = SB(es, "G", [128, 2, 128]); gT = SB(es, "gT", [128, 2]); nwr = SB(es, "nwr", [128, 2])
            St = SB(es, "St", [128, 128]); AT = SB(es, "AT", [128, 128], BF16)
            Qf = SB(es, "Qf", [128, 128], BF16); Qb = SB(es, "Qb", [128, 128], BF16)
            yt = SB(es, "yt", [128, 512]); sqb = SB(es, "rsqb", [128, 512], BF16); rsd = SB(es, "rrsd", [128, 512])
            gl = SB(es, "gl", [128, 512]); oo = SB(es, "roo", [128, 512])
            S.dma("sp", lg.ap[:], rdl, writes=[lg]); S.dma("sp", nwr.ap[:], rnw, writes=[nwr])
            S.act(lg.ap[:], lg.ap[:], AF.Exp, [lg], [lg], scale=-1.0)
            S.act(lg.ap[:], lg.ap[:], AF.Ln, [lg], [lg], bias=1.0)
            S.ts("dve", lg.ap[:], lg.ap[:], -1.0, None, ALU.mult, None, [lg], [lg])
            for hr in range(2):
                lgf = lg.ap[:, hr:hr + 1]; lgb = lg.ap[:, 2 + hr:3 + hr]
                rq = hr * 128; rk = 256 + hr * 128; rv = 512 + hr * 128; rg = 768 + hr * 128
                S.act(M.ap[:], cm("dpos"), AF.Exp, [C, lg], [M], scale=lgf)
                S.tt("dve", M.ap[:], M.ap[:], cm("ufs"), ALU.mult, [M, C], [M])
                S.act(M2.ap[:], cm("dneg"), AF.Exp, [C, lg], [M2], scale=lgb)
                S.tt("dve", M2.ap[:], M2.ap[:], cm("ubs"), ALU.mult, [M2, C], [M2])
                S.tt("dve", M.ap[:], M.ap[:], M2.ap[:], ALU.add, [M, M2], [M])
                S.tt("dve", M.ap[:], M.ap[:], cm("i2"), ALU.add, [M, C], [M])
                S.ts("dve", M.ap[:], M.ap[:], ksc, None, ALU.mult, None, [M], [M])
                S.act(wfb.ap[:, 0:1], C.ap[:, CI["cols"], 0:1], AF.Exp, [C, lg], [wfb], scale=lgf)
                S.act(wfb.ap[:, 1:2], C.ap[:, CI["cols"], 1:2], AF.Exp, [C, lg], [wfb], scale=lgb)
                S.ts("dve", wfb.ap[:], wfb.ap[:], ksc, None, ALU.mult, None, [wfb], [wfb])
                S.act(G.ap[:, 0, :], cm("rowip1"), AF.Exp, [C, lg], [G], scale=lgf)
                S.act(G.ap[:, 1, :], cm("row128mi"), AF.Exp, [C, lg], [G], scale=lgb)
                S.act(gT.ap[:, 0:1], lgf, AF.Exp, [lg], [gT], scale=128.0)
                S.act(gT.ap[:, 1:2], lgb, AF.Exp, [lg], [gT], scale=128.0)
                li = 0
                for (t0, n) in tiles:
                    S.dma("sp", cs_t.ap[:, :n], cosr[:, t0:t0 + n], writes=[cs_t]); S.dma("sp", sn_t.ap[:, :n], sinr[:, t0:t0 + n], writes=[sn_t])
                    for which, row in ((0, rq), (1, rk)):
                        l = ld[li % 2]; li += 1
                        S.dma("sp", l.ap[:, :n], projT[row:row + 128, t0:t0 + n], reads=[d_proj], writes=[l])
                        S.mm(ps[2].ap[:, :n], cm("rot128"), l.ap[:, :n], [C, l], [ps[2]])
                        S.tt("dve", t1.ap[:, :n], l.ap[:, :n], cs_t.ap[:, :n], ALU.mult, [l, cs_t], [t1])
                        S.tt("dve", t2.ap[:, :n], ps[2].ap[:, :n], sn_t.ap[:, :n], ALU.mult, [ps[2], sn_t], [t2])
                        if which == 0:
                            S.tt("pool", QT.ap[:, t0:t0 + n], t1.ap[:, :n], t2.ap[:, :n], ALU.add, [t1, t2], [QT])
                        else:
                            S.tt("pool", kro.ap[:, :n], t1.ap[:, :n], t2.ap[:, :n], ALU.add, [t1, t2], [kro])
                            S.copy("act", KT.ap[:, t0:t0 + n], kro.ap[:, :n], [kro], [KT])
                            for cc in range(n // 128):
                                c = t0 // 128 + cc
                                S.tr(ps[3].ap[:, 0:128], kro.ap[:, cc * 128:(cc + 1) * 128], cm("ident"), [kro, C], [ps[3]])
                                S.act(Kf.ap[:, c, :], ps[3].ap[:, 0:128], AF.Copy, [ps[3], wfb], [Kf], scale=wfb.ap[:, 0:1])
                                S.act(Kb.ap[:, c, :], ps[3].ap[:, 0:128], AF.Copy, [ps[3], wfb], [Kb], scale=wfb.ap[:, 1:2])
                    l = ld[li % 2]; li += 1
                    S.dma("sp", l.ap[:, :n], projT[rv:rv + 128, t0:t0 + n], reads=[d_proj], writes=[l])
                    for cc in range(n // 128):
                        S.tr(ps[1].ap[:, 0:128], l.ap[:, cc * 128:(cc + 1) * 128], cm("ident"), [l, C], [ps[1]])
                        S.copy("act", Vtm.ap[:, t0 // 128 + cc, :], ps[1].ap[:, 0:128], [ps[1]], [Vtm])
                fwd = list(range(NCH))
                bwd = list(range(NCC - 1, -1, -1)) + list(range(NCH - 1, NCC - 1, -1))
                for order, Kx, Sx, gcol in ((fwd, Kf, Sf, 0), (bwd, Kb, Sbk, 1)):
                    S.op("dve", lambda e: e.memset(St.ap[:], 0.0), [], [St])
                    for idx, c in enumerate(order):
                        S.copy("act", Sx.ap[:, c, :], St.ap[:], [St], [Sx])
                        if idx < len(order) - 1:
                            S.mm(ps[0].ap[:, 0:128], Kx.ap[:, c, :], Vtm.ap[:, c, :], [Kx, Vtm], [ps[0]])
                            S.stt(St.ap[:], St.ap[:], gT.ap[:, gcol:gcol + 1], ps[0].ap[:, 0:128], ALU.mult, ALU.add, [St, gT, ps[0]], [St])
                for (t0, n) in tiles:
                    if last and t0 < CTX:
                        continue
                    for cc in range(n // 128):
                        c = t0 // 128 + cc
                        sl = slice(c * 128, (c + 1) * 128)
                        S.mm(ps[4].ap[:, 0:128], KT.ap[:, sl], QT.ap[:, sl], [KT, QT], [ps[4]])
                        S.tt("dve", AT.ap[:], ps[4].ap[:, 0:128], M.ap[:], ALU.mult, [ps[4], M], [AT])
                        S.tt("pool", Qf.ap[:], QT.ap[:, sl], G.ap[:, 0, :], ALU.mult, [QT, G], [Qf])
                        S.tt("pool", Qb.ap[:], QT.ap[:, sl], G.ap[:, 1, :], ALU.mult, [QT, G], [Qb])
                        S.mm(ps[5].ap[:, 0:128], Vtm.ap[:, c, :], AT.ap[:], [Vtm, AT], [ps[5]], start=True, stop=False)
                        S.mm(ps[5].ap[:, 0:128], Sf.ap[:, c, :], Qf.ap[:], [Sf, Qf], [ps[5]], start=False, stop=False)
                        S.mm(ps[5].ap[:, 0:128], Sbk.ap[:, c, :], Qb.ap[:], [Sbk, Qb], [ps[5]], start=False, stop=True)
                        S.copy("act", yt.ap[:, cc * 128:(cc + 1) * 128], ps[5].ap[:, 0:128], [ps[5]], [yt])
                    S.act(sqb.ap[:, :n], yt.ap[:, :n], AF.Square, [yt], [sqb])
                    S.mm(ps[6].ap[:, :n], ones_bf, sqb.ap[:, :n], [sqb, Cb], [ps[6]])
                    S.act(rsd.ap[:, :n], ps[6].ap[:, :n], AF.Ln, [ps[6]], [rsd], scale=1.0 / 128, bias=EPS)
                    S.act(rsd.ap[:, :n], rsd.ap[:, :n], AF.Exp, [rsd], [rsd], scale=-0.5)
                    S.dma("sp", gl.ap[:, :n], projT[rg:rg + 128, t0:t0 + n], reads=[d_proj], writes=[gl])
                    S.act(gl.ap[:, :n], gl.ap[:, :n], AF.Silu, [gl], [gl])
                    S.stt(oo.ap[:, :n], yt.ap[:, :n], nwr.ap[:, hr:hr + 1], rsd.ap[:, :n], ALU.mult, ALU.mult, [yt, nwr, rsd], [oo])
                    S.tt("dve", oo.ap[:, :n], oo.ap[:, :n], gl.ap[:, :n], ALU.mult, [oo, gl], [oo])
                    store_mix(S, hr * 128, 128, t0, n, oo.ap[:, :n], oo, d_mix)
        S.barrier()

        base_s = 1024
        segs = [(0, CTX), (CTX, T)]
        with contextlib.ExitStack() as es:
            cwt = SB(es, "cwt", [128, 8, 5]); cbt = SB(es, "cbt", [128, 8])
            S.dma("sp", cwt.ap[:], cw, writes=[cwt]); S.dma("sp", cbt.ap[:], cb, writes=[cbt])
            with contextlib.ExitStack() as es2:
                xr = SB(es2, "xr", [128, T]); yc = SB(es2, "yc", [128, T])
                for ch in range(8):
                    r0 = base_s + 512 + ch * 128
                    S.dma("sp", xr.ap[:], projT[r0:r0 + 128, :], reads=[d_proj], writes=[xr])
                    for (a, b) in segs:
                        S.ts("dve", yc.ap[:, a:b], xr.ap[:, a:b], cwt.ap[:, ch, 2:3], cbt.ap[:, ch:ch + 1], ALU.mult, ALU.add, [xr, cwt, cbt], [yc])
                        for k in (0, 1, 3, 4):
                            off = k - 2
                            lo = max(a, a - off); hi = min(b, b - off)
                            S.stt(yc.ap[:, lo:hi], xr.ap[:, lo + off:hi + off], cwt.ap[:, ch, k:k + 1], yc.ap[:, lo:hi], ALU.mult, ALU.add, [xr, cwt, yc], [yc])
                    S.act(yc.ap[:], yc.ap[:], AF.Silu, [yc], [yc])
                    S.dma("sp", convT[ch * 128:(ch + 1) * 128, :], yc.ap[:], reads=[yc], writes=[d_conv])
            S.barrier()
            dt_tm = SB(es, "dt_tm", [128, NCH, 16]); a_tm = SB(es, "a_tm", [128, NCH, 16]); aneg = SB(es, "aneg", [128, 16])
            dsk_t = SB(es, "dsk_t", [128, 8]); snw_t = SB(es, "snw_t", [64, 8])
            S.dma("sp", aneg.ap[:], alog, writes=[aneg]); S.dma("sp", dsk_t.ap[:], dsk, writes=[dsk_t]); S.dma("sp", snw_t.ap[:], snw, writes=[snw_t])
            S.act(aneg.ap[:], aneg.ap[:], AF.Exp, [aneg], [aneg])
            S.ts("dve", aneg.ap[:], aneg.ap[:], -1.0, None, ALU.mult, None, [aneg], [aneg])
            with contextlib.ExitStack() as es2:
                dx = SB(es2, "dx", [16, T]); da = SB(es2, "da", [16, T]); dtb_t = SB(es2, "dtb_t", [16, 1])
                S.dma("sp", dx.ap[:], projT[base_s + 1536:base_s + 1552, :], reads=[d_proj], writes=[dx])
                S.dma("sp", dtb_t.ap[:], dtb, writes=[dtb_t])
                S.ts("dve", dx.ap[:], dx.ap[:], dtb_t.ap[:, 0:1], None, ALU.add, None, [dx, dtb_t], [dx])
                S.act(da.ap[:], dx.ap[:], AF.Abs, [dx], [da])
                S.act(da.ap[:], da.ap[:], AF.Exp, [da], [da], scale=-1.0)
                S.act(da.ap[:], da.ap[:], AF.Ln, [da], [da], bias=1.0)
                S.ts("dve", dx.ap[:], dx.ap[:], 0.0, None, ALU.max, None, [dx], [dx])
                S.tt("dve", dx.ap[:], dx.ap[:], da.ap[:], ALU.add, [dx, da], [dx])
                for c in range(NCH):
                    S.tr(ps[0].ap[:, 0:16], dx.ap[:, c * 128:(c + 1) * 128], C.ap[0:16, CI["ident"], 0:16], [dx, C], [ps[0]])
                    S.copy("act", dt_tm.ap[:, c, :], ps[0].ap[:, 0:16], [ps[0]], [dt_tm])
                    S.tt("dve", a_tm.ap[:, c, :], dt_tm.ap[:, c, :], aneg.ap[:], ALU.mult, [dt_tm, aneg], [a_tm])
            S.barrier()
            BT = SB(es, "BT", [128, T], BF16); CT = SB(es, "CT", [128, T], BF16)
            Btm = SB(es, "Btm", [128, NCH, 128], BF16); xsb = SB(es, "xsb", [128, NCH, 256], BF16)
            Sf = SB(es, "sSf", [128, NCH, 256], BF16); Sbk = SB(es, "sSb", [128, NCH, 256], BF16)
            wfb = SB(es, "swfb", [128, NCH, 8]); etot = SB(es, "etot", [128, NCH, 8]); ew = SB(es, "ew", [128, 16])
            ld = [SB(es, f"sld{i}", [128, 512]) for i in range(2)]
            St = SB(es, "sSt", [128, 256]); vp = SB(es, "vp", [128, 256], BF16)
            scs = SB(es, "scs", [128, 128])
            rf_l = [SB(es, f"rf{i}", [128, 128]) for i in range(2)]; rb_l = [SB(es, f"rb{i}", [128, 128]) for i in range(2)]
            E_l = [SB(es, f"E{i}", [128, 512]) for i in range(2)]
            u1_l = [SB(es, f"u1{i}", [128, 128]) for i in range(2)]; u2_l = [SB(es, f"u2{i}", [128, 128]) for i in range(2)]
            AT_l = [SB(es, f"sAT{i}", [128, 128], BF16) for i in range(2)]
            Qf_l = [SB(es, f"sQf{i}", [128, 128], BF16) for i in range(2)]; Qb_l = [SB(es, f"sQb{i}", [128, 128], BF16) for i in range(2)]
            xs_t = SB(es, "xs_t", [64, 512]); z_t = SB(es, "z_t", [64, 512]); yd = [SB(es, f"yd{h}", [64, 512]) for h in range(4)]
            sq4 = SB(es, "sq4", [64, 512], BF16); rsd = SB(es, "srsd", [64, 512]); oo = SB(es, "soo", [64, 512])
            for gs in range(2):
                fc = gs * 4; bc = 8 + gs * 4
                li = 0
                for (t0, n) in tiles:
                    for which, dst in ((0, BT), (1, CT)):
                        l = ld[li % 2]; li += 1
                        r0 = 512 + which * 256 + gs * 128
                        S.dma("sp", l.ap[:, :n], convT[r0:r0 + 128, t0:t0 + n], reads=[d_conv], writes=[l])
                        S.copy("act", dst.ap[:, t0:t0 + n], l.ap[:, :n], [l], [dst])
                        if which == 0:
                            for cc in range(n // 128):
                                S.tr(ps[0].ap[:, 0:128], l.ap[:, cc * 128:(cc + 1) * 128], cm("ident"), [l, C], [ps[0]])
                                S.copy("act", Btm.ap[:, t0 // 128 + cc, :], ps[0].ap[:, 0:128], [ps[0]], [Btm])
                    for half in range(2):
                        l = ld[li % 2]; li += 1
                        r0 = gs * 256 + half * 128
                        S.dma("sp", l.ap[:, :n], convT[r0:r0 + 128, t0:t0 + n], reads=[d_conv], writes=[l])
                        for cc in range(n // 128):
                            S.tr(ps[1].ap[:, 0:128], l.ap[:, cc * 128:(cc + 1) * 128], cm("ident"), [l, C], [ps[1]])
                            S.copy("act", xsb.ap[:, t0 // 128 + cc, half * 128:(half + 1) * 128], ps[1].ap[:, 0:128], [ps[1]], [xsb])
                for c in range(NCH):
                    S.mm(ps[2].ap[:, 0:4], cm("sl"), a_tm.ap[:, c, fc:fc + 4], [C, a_tm], [ps[2]])
                    S.mm(ps[2].ap[:, 4:8], cm("su"), a_tm.ap[:, c, bc:bc + 4], [C, a_tm], [ps[2]])
                    S.mm(ps[2].ap[:, 8:12], cm("ones"), a_tm.ap[:, c, fc:fc + 4], [C, a_tm], [ps[2]])
                    S.mm(ps[2].ap[:, 12:16], cm("ones"), a_tm.ap[:, c, bc:bc + 4], [C, a_tm], [ps[2]])
                    S.act(ew.ap[:], ps[2].ap[:, 0:16], AF.Exp, [ps[2]], [ew])
                    S.tt("dve", wfb.ap[:, c, 0:4], ew.ap[:, 0:4], dt_tm.ap[:, c, fc:fc + 4], ALU.mult, [ew, dt_tm], [wfb])
                    S.tt("dve", wfb.ap[:, c, 4:8], ew.ap[:, 4:8], dt_tm.ap[:, c, bc:bc + 4], ALU.mult, [ew, dt_tm], [wfb])
                    S.copy("dve", etot.ap[:, c, :], ew.ap[:, 8:16], [ew], [etot])
                fwd = list(range(NCH))
                bwd = list(range(NCC - 1, -1, -1)) + list(range(NCH - 1, NCC - 1, -1))
                for order, Sx, o4 in ((fwd, Sf, 0), (bwd, Sbk, 4)):
                    S.op("dve", lambda e: e.memset(St.ap[:], 0.0), [], [St])
                    for idx, c in enumerate(order):
                        S.copy("act", Sx.ap[:, c, :], St.ap[:], [St], [Sx])
                        if idx < len(order) - 1:
                            for h in range(4):
                                S.ts("pool", vp.ap[:, h * 64:(h + 1) * 64], xsb.ap[:, c, h * 64:(h + 1) * 64], wfb.ap[:, c, o4 + h:o4 + h + 1], None, ALU.mult, None, [xsb, wfb], [vp])
                            S.mm(ps[3].ap[:, 0:256], Btm.ap[:, c, :], vp.ap[:], [Btm, vp], [ps[3]])
                            for h in range(4):
                                S.stt(St.ap[:, h * 64:(h + 1) * 64], St.ap[:, h * 64:(h + 1) * 64], etot.ap[:, c, o4 + h:o4 + h + 1], ps[3].ap[:, h * 64:(h + 1) * 64],
                                      ALU.mult, ALU.add, [St, etot, ps[3]], [St])
                for (t0, n) in tiles:
                    if last and t0 < CTX:
                        continue
                    for cc in range(n // 128):
                        c = t0 // 128 + cc
                        sl = slice(c * 128, (c + 1) * 128)
                        S.mm(ps[0].ap[:, 0:128], BT.ap[:, sl], CT.ap[:, sl], [BT, CT], [ps[0]])
                        S.copy("act", scs.ap[:], ps[0].ap[:, 0:128], [ps[0]], [scs])
                        for h in range(4):
                            rf, rb, E, u1, u2, AT, Qf, Qb = (x_[h % 2] for x_ in (rf_l, rb_l, E_l, u1_l, u2_l, AT_l, Qf_l, Qb_l))
                            pE = ps[1 + h % 2]
                            S.ts("dve", rf.ap[:], cm("uf"), a_tm.ap[:, c, fc + h:fc + h + 1], None, ALU.mult, None, [C, a_tm], [rf])
                            S.ts("dve", rb.ap[:], cm("ub"), a_tm.ap[:, c, bc + h:bc + h + 1], None, ALU.mult, None, [C, a_tm], [rb])
                            S.mm(pE.ap[:, 0:128], cm("sl"), rf.ap[:], [C, rf], [pE])
                            S.mm(pE.ap[:, 128:256], cm("su"), rb.ap[:], [C, rb], [pE])
                            S.mm(pE.ap[:, 256:384], cm("ones"), rf.ap[:], [C, rf], [pE])
                            S.mm(pE.ap[:, 384:512], cm("ones"), rb.ap[:], [C, rb], [pE])
                            S.act(E.ap[:], pE.ap[:], AF.Exp, [pE], [E])
                            S.stt(u1.ap[:], E.ap[:, 0:128], dt_tm.ap[:, c, fc + h:fc + h + 1], cm("uf"), ALU.mult, ALU.mult, [E, dt_tm, C], [u1])
                            S.stt(u2.ap[:], E.ap[:, 128:256], dt_tm.ap[:, c, bc + h:bc + h + 1], cm("ub"), ALU.mult, ALU.mult, [E, dt_tm, C], [u2])
                            S.tt("pool", u1.ap[:], u1.ap[:], u2.ap[:], ALU.add, [u1, u2], [u1])
                            S.tt("pool", AT.ap[:], u1.ap[:], scs.ap[:], ALU.mult, [u1, scs], [AT])
                            S.tt("pool", Qf.ap[:], CT.ap[:, sl], E.ap[:, 256:384], ALU.mult, [CT, E], [Qf])
                            S.tt("pool", Qb.ap[:], CT.ap[:, sl], E.ap[:, 384:512], ALU.mult, [CT, E], [Qb])
                            py = ps[4 + h]
                            S.mm(py.ap[0:64, cc * 128:(cc + 1) * 128], xsb.ap[:, c, h * 64:(h + 1) * 64], AT.ap[:], [xsb, AT], [py], start=True, stop=False)
                            S.mm(py.ap[0:64, cc * 128:(cc + 1) * 128], Sf.ap[:, c, h * 64:(h + 1) * 64], Qf.ap[:], [Sf, Qf], [py], start=False, stop=False)
                            S.mm(py.ap[0:64, cc * 128:(cc + 1) * 128], Sbk.ap[:, c, h * 64:(h + 1) * 64], Qb.ap[:], [Sbk, Qb], [py], start=False, stop=True)
                    for h in range(4):
                        rx = gs * 256 + h * 64
                        S.dma("sp", xs_t.ap[:, :n], convT[rx:rx + 64, t0:t0 + n], reads=[d_conv], writes=[xs_t])
                        S.dma("sp", z_t.ap[:, :n], projT[base_s + rx:base_s + rx + 64, t0:t0 + n], reads=[d_proj], writes=[z_t])
                        S.stt(yd[h].ap[:, :n], xs_t.ap[:, :n], dsk_t.ap[0:64, gs * 4 + h:gs * 4 + h + 1], ps[4 + h].ap[0:64, :n], ALU.mult, ALU.add, [xs_t, dsk_t, ps[4 + h]], [yd[h]])
                        S.act(z_t.ap[:, :n], z_t.ap[:, :n], AF.Silu, [z_t], [z_t])
                        S.tt("dve", yd[h].ap[:, :n], yd[h].ap[:, :n], z_t.ap[:, :n], ALU.mult, [yd[h], z_t], [yd[h]])
                        S.act(sq4.ap[:, :n], yd[h].ap[:, :n], AF.Square, [yd[h]], [sq4])
                        S.mm(ps[3].ap[0:64, :n], ones_bf[0:64, 0:64], sq4.ap[:, :n], [Cb, sq4], [ps[3]], start=(h == 0), stop=(h == 3))
                    S.act(rsd.ap[:, :n], ps[3].ap[0:64, :n], AF.Ln, [ps[3]], [rsd], scale=1.0 / 256, bias=EPS)
                    S.act(rsd.ap[:, :n], rsd.ap[:, :n], AF.Exp, [rsd], [rsd], scale=-0.5)
                    for h in range(4):
                        S.stt(oo.ap[:, :n], yd[h].ap[:, :n], snw_t.ap[:, gs * 4 + h:gs * 4 + h + 1], rsd.ap[:, :n], ALU.mult, ALU.mult, [yd[h], snw_t, rsd], [oo])
                        r0 = 256 + gs * 256 + h * 64
                        store_mix(S, r0, 64, t0, n, oo.ap[:, :n], oo, d_mix)
        S.barrier()


K1_SMALL = (("rdl", [128, 4]), ("rnw", [128, 2]), ("cw", [128, 8, 5]), ("cb", [128, 8]), ("dtb", [16, 1]), ("alog", [128, 16]),
            ("dsk", [128, 8]), ("snw", [64, 8]), ("dlam", [128, 4, 64]), ("dnw", [128, 1]))


def build_k1(cfg, last, lam_init):
    D, KD, T = cfg.D, cfg.KD, cfg.T
    nc = bass.Bass("TRN2", target_bir_lowering=False)
    dt_in = lambda n, sh: nc.dram_tensor(n, sh, F32, kind="ExternalInput").ap()
    xT = dt_in("xT", [D, T])
    I = {"w_in": dt_in("w_in", [D, cfg.PC]), "ada_w": dt_in("ada_w", [D, 2 * D]), "ada_b": dt_in("ada_b", [128, 2 * KD]),
         "ccol": dt_in("ccol", [128, KD, 2]), "nmw": dt_in("nmw", [128, KD]), "consts": dt_in("consts", [128, len(CNAMES), 128])}
    for n_ in ("cosd", "sind", "cosr", "sinr"):
        I[n_] = dt_in(n_, [128, T])
    for n_, sh in K1_SMALL:
        I[n_] = dt_in(n_, sh)
    mixT = nc.dram_tensor("mixT", [1024, T], F32, kind="ExternalOutput").ap()
    I["hT"] = nc.dram_tensor("hT", [D, T], BF16, kind="Internal").ap()
    I["projT"] = nc.dram_tensor("projT", [cfg.PC, T], F32, kind="Internal").ap()
    I["convT"] = nc.dram_tensor("convT", [1024, T], F32, kind="Internal").ap()

    def load_x(S, xt, t0, n):
        for kg in range(0, KD, 8):
            k2 = min(KD, kg + 8)
            S.dma("sp", xt.ap[:, kg:k2, :n], xT[kg * 128:k2 * 128, t0:t0 + n].rearrange("(k p) t -> p k t", p=128), writes=[xt])

    def store_mix(S, r0, nr, t0, n, src, sbuf, d_mix):
        S.dma("sp", mixT[r0:r0 + nr, t0:t0 + n], src, reads=[sbuf], writes=[d_mix])

    with contextlib.ExitStack() as es:
        S = Sched(nc)
        ps = [Buf(es.enter_context(nc.psum_tensor(f"ps{i}", [128, 512], F32)), f"ps{i}") for i in range(8)]
        emit_k1(nc, S, ps, cfg, last, lam_init, I, "", load_x, store_mix, Buf(None, "d_mix"))
    return nc


def col_layout(v):
    v = np.asarray(v, np.float32)
    return np.ascontiguousarray(v.reshape(-1, 128).T)


def rep128(v):
    v = np.asarray(v, np.float32)
    return np.ascontiguousarray(np.broadcast_to(v[None], (128,) + v.shape))


def k1_cols(cfg, q):
    NQ = cfg.NQ
    RW, SW, DW, G, SH = 256 * NQ, 512 * NQ, 256 * NQ, 2 * NQ, 8 * NQ
    sizes = (RW, RW, RW, RW, SW, SW + 2 * G * 128, 2 * SH, DW, DW, DW)
    o = np.concatenate([[0], np.cumsum(sizes)]).astype(int)
    heads = (2 * q, 2 * q + 1)
    cols = []
    for part in range(4):
        for h in heads:
            cols += list(range(o[part] + h * 128, o[part] + (h + 1) * 128))
    for g in heads:
        cols += list(range(o[4] + g * 256, o[4] + (g + 1) * 256))
    conv_ch = []
    for g in heads:
        conv_ch += list(range(g * 256, (g + 1) * 256))
    for g in heads:
        conv_ch += list(range(SW + g * 128, SW + (g + 1) * 128))
    for g in heads:
        conv_ch += list(range(SW + G * 128 + g * 128, SW + G * 128 + (g + 1) * 128))
    cols += [o[5] + c for c in conv_ch]
    dt_idx = [d * SH + g * 4 + r for d in range(2) for g in heads for r in range(4)]
    cols += [o[6] + i for i in dt_idx]
    for part in (7, 8, 9):
        for h in heads:
            cols += list(range(o[part] + h * 128, o[part] + (h + 1) * 128))
    assert len(cols) == cfg.PC
    return np.array(cols), np.array(conv_ch), dt_idx


def k1_inputs(cfg, inp, layer, b, q, xT_b, tabs):
    D, KD = cfg.D, cfg.KD
    cols, conv_ch, dt_idx = k1_cols(cfg, q)
    heads = [2 * q, 2 * q + 1]
    m = {}
    m["xT"] = xT_b
    m["w_in"] = np.ascontiguousarray(inp["w_in"][layer][:, cols])
    m["ada_w"] = np.ascontiguousarray(inp["ada_w"][layer][:, 0:2 * D])
    m["ada_b"] = col_layout(inp["ada_b"][layer][0:2 * D])
    m["ccol"] = np.ascontiguousarray(np.stack([col_layout(inp["c"][b]), col_layout(inp["c_ctx"])], axis=-1))
    m["nmw"] = col_layout(inp["norm_mix_w"][layer])
    m["consts"] = CPACK
    m["cosd"], m["sind"], m["cosr"], m["sinr"] = tabs
    rd = inp["ret_decay_logit"][layer]
    m["rdl"] = rep128(np.array([rd[0, heads[0]], rd[0, heads[1]], rd[1, heads[0]], rd[1, heads[1]]], np.float32))
    m["rnw"] = np.ascontiguousarray(inp["ret_norm_w"][layer].reshape(-1, 128)[heads].T)
    cwf = inp["ssd_conv_w"][layer][:, conv_ch]
    m["cw"] = np.ascontiguousarray(cwf.reshape(5, 8, 128).transpose(2, 1, 0))
    m["cb"] = np.ascontiguousarray(inp["ssd_conv_b"][layer][conv_ch].reshape(8, 128).T)
    m["dtb"] = np.ascontiguousarray(inp["ssd_dt_bias"][layer].reshape(-1)[dt_idx].reshape(16, 1))
    m["alog"] = rep128(inp["ssd_a_log"][layer].reshape(-1)[dt_idx])
    hidx = [g * 4 + r for g in heads for r in range(4)]
    m["dsk"] = rep128(inp["ssd_d"][layer][hidx])
    nw = inp["ssd_norm_w"][layer].reshape(-1, 4, 64)[heads]
    m["snw"] = np.ascontiguousarray(nw.reshape(8, 64).T)
    m["dlam"] = rep128(inp["diff_lambda"][layer])
    m["dnw"] = np.ascontiguousarray(inp["diff_norm_w"][layer].reshape(128, 1))
    return {k: np.ascontiguousarray(v, dtype=np.float32) for k, v in m.items()}


def assemble_mix(cfg, per_q):
    NQ = cfg.NQ
    RW, SW = 256 * NQ, 512 * NQ
    out = np.empty((cfg.MIX, per_q[0].shape[1]), np.float32)
    for q, loc in enumerate(per_q):
        out[q * 256:(q + 1) * 256] = loc[0:256]
        out[RW + q * 512:RW + (q + 1) * 512] = loc[256:768]
        out[RW + SW + q * 256:RW + SW + (q + 1) * 256] = loc[768:1024]
    return out


def k2_tiles(cfg, ntq, last):
    tl_c = 0 if last else cfg.CTX // ntq
    tl_l = cfg.SEQ // ntq
    tiles = []
    if tl_c:
        tiles.append((0, tl_c, 1))
    t = tl_c
    while t < tl_c + tl_l:
        n = min(512, tl_c + tl_l - t)
        tiles.append((t, n, 0))
        t += n
    return tiles, tl_c + tl_l


def emit_k2(nc, S, ps, cfg, ntq, last, moe, I, pf, x_src, load_mix, out_ap, d_out):
    D, KD, MIX = cfg.D, cfg.KD, cfg.MIX
    KM = MIX // 128
    tiles, TL = k2_tiles(cfg, ntq, last)
    NF = (cfg.DFE if moe else cfg.DFF) // 128
    NE = cfg.NEXP if moe else 1
    FS = 22 if not moe else 16
    splits = []
    f = 0
    while f < NF:
        nf = min(FS, NF - f)
        splits.append((f, nf)); f += nf
    w_out, ada_w, ada_b, ccol, nfw, consts = I["w_out"], I["ada_w"], I["ada_b"], I["ccol"], I["nfw"], I["consts"]
    wg_d, wu_d, wd_d = I["wg"], I["wu"], I["wd"]
    if moe:
        rw, selc = I["rw"], I["selc"]
    if last:
        fnw = I["fnw"]
    xT = x_src
    xout = out_ap
    WBE = 32 * 256

    with contextlib.ExitStack() as es0:
        def SB(es, n, sh, dt=F32):
            return Buf(es.enter_context(nc.sbuf_tensor(pf + n, sh, dt)), n)
        C = SB(es0, "C", [128, 2, 128]); Cb = SB(es0, "Cb", [128, 2, 128], BF16)
        S.dma("sp", C.ap[:], consts[:, 0:2, :], writes=[C])
        S.copy("dve", Cb.ap[:, 0, :], C.ap[:, CI["ones"], :], [C], [Cb])
        S.copy("dve", Cb.ap[:, 1, :], C.ap[:, CI["ident"], :], [C], [Cb])
        cm = lambda n: C.ap[:, CI[n], :]
        ones_bf = Cb.ap[:, 0, :]
        MV = SB(es0, "MV", [128, KD, 8])
        with contextlib.ExitStack() as es:
            sc = SB(es, "sc", [128, KD, 2]); adb = SB(es, "adb", [128, 4 * KD]); nm = SB(es, "nm", [128, KD])
            modc = SB(es, "modc", [128, 4 * KD, 2])
            wa = [SB(es, f"wa{i}", [128, KD, 512]) for i in range(2)]
            S.dma("sp", sc.ap[:], ccol, writes=[sc]); S.dma("sp", adb.ap[:], ada_b, writes=[adb]); S.dma("sp", nm.ap[:], nfw, writes=[nm])
            S.act(sc.ap[:], sc.ap[:], AF.Silu, [sc], [sc])
            nfc = 4 * KD
            pm = ps[0]
            for fg in range(0, nfc, 4):
                w = wa[(fg // 4) % 2]
                for kg in range(0, KD, 8):
                    k2 = min(KD, kg + 8)
                    S.dma("sp", w.ap[:, kg:k2, :], ada_w[kg * 128:k2 * 128, fg * 128:(fg + 4) * 128].rearrange("(k p) c -> p k c", p=128), writes=[w])
                for f in range(fg, fg + 4):
                    for k in range(KD):
                        S.mm(pm.ap[:, 2 * f:2 * f + 2], w.ap[:, k, (f - fg) * 128:(f - fg + 1) * 128], sc.ap[:, k, :], [w, sc], [pm],
                             start=(k == 0), stop=(k == KD - 1), signal=(k == KD - 1))
            for j in range(2):
                S.tt("dve", modc.ap[:, :, j], pm.ap[:, 0:2 * nfc].rearrange("p (f j) -> p f j", j=2)[:, :, j], adb.ap[:], ALU.add, [pm, adb], [modc])
            for j in range(2):
                S.copy("dve", MV.ap[:, :, 0 + j], modc.ap[:, 0:KD, j], [modc], [MV])
                S.stt(MV.ap[:, :, 2 + j], modc.ap[:, 2 * KD:3 * KD, j], 1.0, nm.ap[:], ALU.add, ALU.mult, [modc, nm], [MV])
                S.copy("dve", MV.ap[:, :, 4 + j], modc.ap[:, KD:2 * KD, j], [modc], [MV])
                S.copy("dve", MV.ap[:, :, 6 + j], modc.ap[:, 3 * KD:4 * KD, j], [modc], [MV])
        S.barrier()

        with contextlib.ExitStack() as es:
            x1 = SB(es, "x1", [128, KD, 512]); mh = SB(es, "mh", [128, max(KD, KM), 512], BF16)
            aT = SB(es, "aT", [128, FS, 512], BF16)
            wbuf = [SB(es, f"wbuf{i}", [128, WBE], BF16) for i in range(4)]
            sq = [SB(es, f"sq{i}", [128, 512], BF16) for i in range(2)]
            tmp = [SB(es, f"tmp{i}", [128, 512]) for i in range(1 if moe else 2)]
            sg = [SB(es, f"sg{i}", [128, 512]) for i in range(2)]
            rs = SB(es, "rs", [128, 512])
            if moe:
                rwt = SB(es, "rwt", [128, KD, 8]); sel = SB(es, "sel", [8, 8, 128]); lgt = SB(es, "lgt", [128, 8]); m8 = SB(es, "m8", [128, 8])
                mk = SB(es, "mk", [128, 16]); gg = SB(es, "gg", [128, 4]); Gm = SB(es, "Gm", [128, 8]); GT = SB(es, "GT", [8, 512])
                gb = SB(es, "gb", [128, 8, 512], BF16); hf = [SB(es, "hf0", [128, 512])]
                S.dma("sp", rwt.ap[:], rw, writes=[rwt]); S.dma("sp", sel.ap[:], selc, writes=[sel])
            if last:
                fn = SB(es, "fn", [128, KD]); S.dma("sp", fn.ap[:], fnw, writes=[fn])
            wi = [0]
            cache = I.get("wcache")
            d_cache = Buf(None, "d_cache")
            blk = [0]
            first = [True]

            def wload(src, nk, ncol):
                b = wbuf[wi[0] % 4]; wi[0] += 1
                view = b.ap[:, 0:nk * ncol].rearrange("p (k c) -> p k c", c=ncol)
                bid = blk[0]; blk[0] += 1
                if cache is None or first[0]:
                    S.dma("pool", view, src.rearrange("(k p) c -> p k c", p=128), writes=[b])
                    if cache is not None:
                        S.dma("sp", cache[bid // 120][bid % 120, :, 0:nk * ncol], b.ap[:, 0:nk * ncol], reads=[b], writes=[d_cache])
                else:
                    S.dma("sp", b.ap[:, 0:nk * ncol], cache[bid // 120][bid % 120, :, 0:nk * ncol], reads=[d_cache], writes=[b])
                return b, view

            pi = [0, 0, 0]
            for ti_, (t0, n, kind) in enumerate(tiles):
                blk[0] = 0
                first[0] = (ti_ == 0)
                for kg in range(0, KD, 8):
                    k2 = min(KD, kg + 8)
                    S.dma("sp", x1.ap[:, kg:k2, :n], xT[kg * 128:k2 * 128, t0:t0 + n].rearrange("(k p) t -> p k t", p=128), writes=[x1])
                load_mix(S, mh, t0, n, kind)
                for db in range(0, KD, 2):
                    ncb = min(2, KD - db)
                    b, v = wload(w_out[:, db * 128:(db + ncb) * 128], KM, ncb * 128)
                    for j in range(ncb):
                        dc = db + j
                        p = ps[pi[0] % 2]; pi[0] += 1
                        for k in range(KM):
                            S.mm(p.ap[:, :n], v[:, k, j * 128:(j + 1) * 128], mh.ap[:, k, :n], [b, mh], [p], start=(k == 0), stop=(k == KM - 1), signal=(k == KM - 1))
                        S.stt(x1.ap[:, dc, :n], p.ap[:, :n], MV.ap[:, dc, kind:kind + 1], x1.ap[:, dc, :n], ALU.mult, ALU.add, [p, MV, x1], [x1])
                for k in range(KD):
                    s_ = sq[k % 2]
                    S.act(s_.ap[:, :n], x1.ap[:, k, :n], AF.Square, [x1], [s_])
                    S.mm(ps[6].ap[:, :n], ones_bf, s_.ap[:, :n], [s_, Cb], [ps[6]], start=(k == 0), stop=(k == KD - 1))
                S.act(rs.ap[:, :n], ps[6].ap[:, :n], AF.Ln, [ps[6]], [rs], scale=1.0 / D, bias=EPS)
                S.act(rs.ap[:, :n], rs.ap[:, :n], AF.Exp, [rs], [rs], scale=-0.5)
                nsub = (n + 127) // 128
                for k in range(KD):
                    t_ = tmp[k % len(tmp)]
                    S.tt("dve", t_.ap[:, :n], x1.ap[:, k, :n], rs.ap[:, :n], ALU.mult, [x1, rs], [t_])
                    S.act(mh.ap[:, k, :n], t_.ap[:, :n], AF.Identity, [t_, MV], [mh], scale=MV.ap[:, k, 2 + kind:3 + kind], bias=MV.ap[:, k, 4 + kind:5 + kind])
                    if moe:
                        h_ = hf[0]
                        S.act(h_.ap[:, :n], t_.ap[:, :n], AF.Identity, [t_, MV], [h_], scale=MV.ap[:, k, 2 + kind:3 + kind], bias=MV.ap[:, k, 4 + kind:5 + kind])
                        for sub in range(nsub):
                            m_ = min(128, n - sub * 128)
                            S.mm(ps[2 + sub].ap[:m_, 0:8], h_.ap[:, sub * 128:sub * 128 + m_], rwt.ap[:, k, :], [h_, rwt], [ps[2 + sub]], start=(k == 0), stop=(k == KD - 1))
                if moe:
                    for sub in range(nsub):
                        m_ = min(128, n - sub * 128)
                        S.copy("dve", lgt.ap[:m_, :], ps[2 + sub].ap[:m_, 0:8], [ps[2 + sub]], [lgt])
                        S.op("dve", lambda e: e.max(out=m8.ap[:m_, :], in_=lgt.ap[:m_, :]), [lgt], [m8])
                        S.ts("dve", mk.ap[:m_, 0:8], lgt.ap[:m_, :], m8.ap[:m_, 0:1], None, ALU.is_equal, None, [lgt, m8], [mk])
                        S.ts("dve", mk.ap[:m_, 8:16], lgt.ap[:m_, :], m8.ap[:m_, 1:2], None, ALU.is_equal, None, [lgt, m8], [mk])
                        S.tt("dve", gg.ap[:m_, 0:1], m8.ap[:m_, 1:2], m8.ap[:m_, 0:1], ALU.subtract, [m8], [gg])
                        S.act(gg.ap[:m_, 1:2], gg.ap[:m_, 0:1], AF.Exp, [gg], [gg])
                        S.ts("dve", gg.ap[:m_, 2:3], gg.ap[:m_, 1:2], 1.0, None, ALU.add, None, [gg], [gg])
                        S.op("dve", lambda e: e.reciprocal(out=gg.ap[:m_, 2:3], in_=gg.ap[:m_, 2:3]), [gg], [gg])
                        S.tt("dve", gg.ap[:m_, 3:4], gg.ap[:m_, 1:2], gg.ap[:m_, 2:3], ALU.mult, [gg], [gg])
                        S.ts("dve", Gm.ap[:m_, :], mk.ap[:m_, 0:8], gg.ap[:m_, 2:3], None, ALU.mult, None, [mk, gg], [Gm])
                        S.stt(Gm.ap[:m_, :], mk.ap[:m_, 8:16], gg.ap[:m_, 3:4], Gm.ap[:m_, :], ALU.mult, ALU.add, [mk, gg, Gm], [Gm])
                        S.tr(ps[6].ap[0:8, 0:m_], Gm.ap[:m_, :], C.ap[:m_, CI["ident"], 0:m_], [Gm, C], [ps[6]])
                        S.copy("dve", GT.ap[:, sub * 128:sub * 128 + m_], ps[6].ap[0:8, 0:m_], [ps[6]], [GT])
                    for e_ in range(NE):
                        S.mm(ps[7].ap[:, :n], sel.ap[:, e_, :], GT.ap[:, :n], [sel, GT], [ps[7]])
                        S.copy("act", gb.ap[:, e_, :n], ps[7].ap[:, :n], [ps[7]], [gb])
                for e_ in range(NE):
                    wg_e = wg_d[e_] if moe else wg_d
                    wu_e = wu_d[e_] if moe else wu_d
                    wd_e = wd_d[e_] if moe else wd_d
                    for (f0, nf) in splits:
                        for fb in range(f0, f0 + nf, 2):
                            ncb = min(2, f0 + nf - fb)
                            bg, vg = wload(wg_e[:, fb * 128:(fb + ncb) * 128], KD, ncb * 128)
                            bu, vu = wload(wu_e[:, fb * 128:(fb + ncb) * 128], KD, ncb * 128)
                            for j in range(ncb):
                                fc = fb + j
                                pg = ps[2 + pi[1] % 2]; pu = ps[4 + pi[1] % 2]; s_ = sg[pi[1] % 2]; pi[1] += 1
                                for k in range(KD):
                                    S.mm(pg.ap[:, :n], vg[:, k, j * 128:(j + 1) * 128], mh.ap[:, k, :n], [bg, mh], [pg], start=(k == 0), stop=(k == KD - 1), signal=(k == KD - 1))
                                for k in range(KD):
                                    S.mm(pu.ap[:, :n], vu[:, k, j * 128:(j + 1) * 128], mh.ap[:, k, :n], [bu, mh], [pu], start=(k == 0), stop=(k == KD - 1), signal=(k == KD - 1))
                                S.act(s_.ap[:, :n], pg.ap[:, :n], AF.Silu, [pg], [s_])
                                if moe:
                                    S.tt("dve", s_.ap[:, :n], s_.ap[:, :n], pu.ap[:, :n], ALU.mult, [s_, pu], [s_])
                                    S.tt("pool", aT.ap[:, fc - f0, :n], s_.ap[:, :n], gb.ap[:, e_, :n], ALU.mult, [s_, gb], [aT])
                                else:
                                    S.tt("dve", aT.ap[:, fc - f0, :n], s_.ap[:, :n], pu.ap[:, :n], ALU.mult, [s_, pu], [aT])
                        for db in range(0, KD, 2):
                            ncb = min(2, KD - db)
                            b, v = wload(wd_e[f0 * 128:(f0 + nf) * 128, db * 128:(db + ncb) * 128], nf, ncb * 128)
                            for j in range(ncb):
                                dc = db + j
                                p = ps[pi[0] % 2]; pi[0] += 1
                                for k in range(nf):
                                    S.mm(p.ap[:, :n], v[:, k, j * 128:(j + 1) * 128], aT.ap[:, k, :n], [b, aT], [p], start=(k == 0), stop=(k == nf - 1), signal=(k == nf - 1))
                                S.stt(x1.ap[:, dc, :n], p.ap[:, :n], MV.ap[:, dc, 6 + kind:7 + kind], x1.ap[:, dc, :n], ALU.mult, ALU.add, [p, MV, x1], [x1])
                if last:
                    for k in range(KD):
                        s_ = sq[k % 2]
                        S.act(s_.ap[:, :n], x1.ap[:, k, :n], AF.Square, [x1], [s_])
                        S.mm(ps[6].ap[:, :n], ones_bf, s_.ap[:, :n], [s_, Cb], [ps[6]], start=(k == 0), stop=(k == KD - 1))
                    S.act(rs.ap[:, :n], ps[6].ap[:, :n], AF.Ln, [ps[6]], [rs], scale=1.0 / D, bias=EPS)
                    S.act(rs.ap[:, :n], rs.ap[:, :n], AF.Exp, [rs], [rs], scale=-0.5)
                    for k in range(KD):
                        S.stt(x1.ap[:, k, :n], x1.ap[:, k, :n], fn.ap[:, k:k + 1], rs.ap[:, :n], ALU.mult, ALU.mult, [x1, fn, rs], [x1])
                for kg in range(0, KD, 8):
                    k2 = min(KD, kg + 8)
                    S.dma("sp", xout[kg * 128:k2 * 128, t0:t0 + n].rearrange("(k p) t -> p k t", p=128), x1.ap[:, kg:k2, :n], reads=[x1], writes=[d_out])
        S.barrier()


def build_k2(cfg, ntq, last, moe):
    D, KD, MIX = cfg.D, cfg.KD, cfg.MIX
    KM = MIX // 128
    tiles, TL = k2_tiles(cfg, ntq, last)
    NF = (cfg.DFE if moe else cfg.DFF) // 128
    NE = cfg.NEXP
    nc = bass.Bass("TRN2", target_bir_lowering=False)
    dt_in = lambda n, sh: nc.dram_tensor(n, sh, F32, kind="ExternalInput").ap()
    xT = dt_in("xT", [D, TL]); mixT = dt_in("mixT", [MIX, TL])
    I = {"w_out": dt_in("w_out", [MIX, D]), "ada_w": dt_in("ada_w", [D, 4 * D]), "ada_b": dt_in("ada_b", [128, 4 * KD]),
         "ccol": dt_in("ccol", [128, KD, 2]), "nfw": dt_in("nfw", [128, KD]), "consts": dt_in("consts", [128, len(CNAMES), 128])}
    if moe:
        I["wg"] = dt_in("wg", [NE, D, NF * 128]); I["wu"] = dt_in("wu", [NE, D, NF * 128]); I["wd"] = dt_in("wd", [NE, NF * 128, D])
        I["rw"] = dt_in("rw", [128, KD, 8]); I["selc"] = dt_in("selc", [8, 8, 128])
    else:
        I["wg"] = dt_in("wg", [D, NF * 128]); I["wu"] = dt_in("wu", [D, NF * 128]); I["wd"] = dt_in("wd", [NF * 128, D])
    if last:
        I["fnw"] = dt_in("fnw", [128, KD])
    xout = nc.dram_tensor("xout", [D, TL], F32, kind="ExternalOutput").ap()

    def load_mix(S, mh, t0, n, kind):
        for kg in range(0, KM, 8):
            k2 = min(KM, kg + 8)
            S.dma("pool", mh.ap[:, kg:k2, :n], mixT[kg * 128:k2 * 128, t0:t0 + n].rearrange("(k p) t -> p k t", p=128), writes=[mh])

    with contextlib.ExitStack() as es:
        S = Sched(nc)
        ps = [Buf(es.enter_context(nc.psum_tensor(f"ps{i}", [128, 512], F32)), f"ps{i}") for i in range(8)]
        emit_k2(nc, S, ps, cfg, ntq, last, moe, I, "", xT, load_mix, xout, Buf(None, "d_out"))
    return nc


SELC = np.zeros((8, 8, 128), np.float32)
for _e in range(8):
    SELC[_e, _e, :] = 1.0


def k2_inputs(cfg, inp, layer, b, xT_loc, mixT_loc, last, moe):
    D = cfg.D
    m = {"xT": xT_loc, "mixT": mixT_loc, "w_out": inp["w_out"][layer],
         "ada_w": np.ascontiguousarray(inp["ada_w"][layer][:, 2 * D:6 * D]),
         "ada_b": col_layout(inp["ada_b"][layer][2 * D:6 * D]),
         "ccol": np.ascontiguousarray(np.stack([col_layout(inp["c"][b]), col_layout(inp["c_ctx"])], axis=-1)),
         "nfw": col_layout(inp["norm_ffn_w"][layer]), "consts": CPACK}
    i = layer // 2
    if moe:
        m["wg"] = inp["moe_w_gate"][i]; m["wu"] = inp["moe_w_up"][i]; m["wd"] = inp["moe_w_down"][i]
        m["rw"] = np.ascontiguousarray(inp["moe_router"][i].reshape(cfg.KD, 128, 8).transpose(1, 0, 2))
        m["selc"] = SELC
    else:
        m["wg"] = inp["dense_w_gate"][i]; m["wu"] = inp["dense_w_up"][i]; m["wd"] = inp["dense_w_down"][i]
    if last:
        m["fnw"] = col_layout(inp["final_norm_w"])
    return {k: np.ascontiguousarray(v, dtype=np.float32) for k, v in m.items()}


def tq_index(cfg, ntq, tq, last):
    lat = cfg.CTX + np.arange(tq * (cfg.SEQ // ntq), (tq + 1) * (cfg.SEQ // ntq))
    if last:
        return lat
    c = np.arange(tq * (cfg.CTX // ntq), (tq + 1) * (cfg.CTX // ntq))
    return np.concatenate([c, lat])


def run_pipeline(cfg, inp, ntq, depth=2, verbose=False):
    B, NQ = cfg.B, cfg.NQ
    tabs = rope_tables(cfg, 64) + rope_tables(cfg, 128)
    xT = [np.ascontiguousarray(np.concatenate([inp["ctx"][b], inp["x"][b]], axis=0).T) for b in range(B)]
    out = None
    for layer in range(depth):
        last = layer == depth - 1
        moe = layer % 2 == 1
        lam_init = 0.8 - 0.6 * math.exp(-0.3 * layer)
        nc1 = build_k1(cfg, last, lam_init)
        maps = [k1_inputs(cfg, inp, layer, b, q, xT[b], tabs) for b in range(B) for q in range(NQ)]
        res = run_bass_kernel_spmd(nc1, maps, core_ids=list(range(len(maps))))
        mix = [assemble_mix(cfg, [res.results[b * NQ + q]["mixT"] for q in range(NQ)]) for b in range(B)]
        del maps, res
        nc2 = build_k2(cfg, ntq, last, moe)
        maps = []
        for b in range(B):
            for tq in range(ntq):
                idx = tq_index(cfg, ntq, tq, last)
                maps.append(k2_inputs(cfg, inp, layer, b, xT[b][:, idx], mix[b][:, idx], last, moe))
        res = run_bass_kernel_spmd(nc2, maps, core_ids=list(range(len(maps))))
        if last:
            out = np.empty((B, cfg.SEQ, cfg.D), np.float32)
            for b in range(B):
                for tq in range(ntq):
                    idx = tq_index(cfg, ntq, tq, True) - cfg.CTX
                    out[b, idx, :] = res.results[b * ntq + tq]["xout"].T
        else:
            for b in range(B):
                for tq in range(ntq):
                    idx = tq_index(cfg, ntq, tq, False)
                    xT[b][:, idx] = res.results[b * ntq + tq]["xout"]
        del maps, res
    return out, xT


def mix_perm(cfg):
    NQ = cfg.NQ
    RW, SW = 256 * NQ, 512 * NQ
    idx = []
    for q in range(NQ):
        idx += list(range(q * 256, (q + 1) * 256))
        idx += list(range(RW + q * 512, RW + (q + 1) * 512))
        idx += list(range(RW + SW + q * 256, RW + SW + (q + 1) * 256))
    return np.array(idx)


def _splits(NF, FS):
    out = []
    f = 0
    while f < NF:
        out.append(min(FS, NF - f)); f += out[-1]
    return out


def build_fused(cfg, stop=99):
    D, KD, T, NQ, CTX, SEQ, MIX = cfg.D, cfg.KD, cfg.T, cfg.NQ, cfg.CTX, cfg.SEQ, cfg.MIX
    KM = MIX // 128
    TLc, TLl = CTX // NQ, SEQ // NQ
    TL0 = TLc + TLl
    NTJ = TLl // 512
    NE = cfg.NEXP
    groups = [[b * NQ + q for q in range(NQ)] for b in range(cfg.B)]
    nc = bass.Bass("TRN2", target_bir_lowering=False)
    dt_in = lambda n, sh, dt=F32: nc.dram_tensor(n, sh, dt, kind="ExternalInput").ap()
    dt_sc = lambda n, sh, dt=F32: nc.dram_tensor(n, sh, dt, kind="Internal").ap()
    xTb = dt_in("xTb", [D, T]); xTl = dt_in("xTl", [D, TL0])
    sh = {"ccol": dt_in("ccol", [128, KD, 2]), "consts": dt_in("consts", [128, len(CNAMES), 128]), "selc": dt_in("selc", [8, 8, 128])}
    for n_ in ("cosd", "sind", "cosr", "sinr"):
        sh[n_] = dt_in(n_, [128, T])
    L = []
    for l in range(2):
        d = {"w_in": dt_in(f"w_in{l}", [D, cfg.PC]), "ada_w": dt_in(f"ada_w{l}", [D, 6 * D]), "ada_b": dt_in(f"ada_b{l}", [128, 6 * KD]),
             "nmw": dt_in(f"nmw{l}", [128, KD]), "nfw": dt_in(f"nfw{l}", [128, KD]), "w_out": dt_in(f"w_out{l}", [MIX, D]),
             "midx": dt_in(f"midx{l}", [128, KM, NTJ + 1], mybir.dt.int32)}
        for n_, shp in K1_SMALL:
            d[n_] = dt_in(f"{n_}{l}", shp)
        L.append(d)
    NFd = cfg.DFF // 128; NFe = cfg.DFE // 128
    L[0]["wg"] = dt_in("wg0", [D, NFd * 128]); L[0]["wu"] = dt_in("wu0", [D, NFd * 128]); L[0]["wd"] = dt_in("wd0", [NFd * 128, D])
    L[1]["wg"] = dt_in("wg1", [NE, D, NFe * 128]); L[1]["wu"] = dt_in("wu1", [NE, D, NFe * 128]); L[1]["wd"] = dt_in("wd1", [NE, NFe * 128, D])
    L[1]["rw"] = dt_in("rw1", [128, KD, 8]); L[1]["fnw"] = dt_in("fnw", [128, KD])
    xout = nc.dram_tensor("xout", [D, TLl], F32, kind="ExternalOutput").ap()
    hT = dt_sc("hT", [D, T], BF16); projT = dt_sc("projT", [cfg.PC, T]); convT = dt_sc("convT", [1024, T])
    mixl = [dt_sc(f"mixl{l}", [NQ * NTJ * 1024, 512], BF16) for l in range(2)]
    mixlg = [dt_sc(f"mixlg{l}", [NQ * NQ * NTJ * 1024, 512], BF16) for l in range(2)]
    mixc = dt_sc("mixc", [NQ * 1024, TLc], BF16); mixcg = dt_sc("mixcg", [NQ * NQ * 1024, TLc], BF16)
    x1loc = dt_sc("x1loc", [D, TL0]); xg = dt_sc("xg", [NQ * D, TL0])
    d_xg = Buf(None, "d_xg"); d_x1 = Buf(None, "d_x1")
    nblk0 = (KD + 1) // 2 + sum(2 * ((nf + 1) // 2) + (KD + 1) // 2 for nf in _splits(NFd, 22))
    nblk1 = (KD + 1) // 2 + NE * sum(2 * ((nf + 1) // 2) + (KD + 1) // 2 for nf in _splits(NFe, 16))
    CB = 120
    L[0]["wcache"] = [dt_sc(f"wcache0_{i}", [min(CB, nblk0 - i * CB), 128, 32 * 256], BF16) for i in range((nblk0 + CB - 1) // CB)]
    L[1]["wcache"] = [dt_sc(f"wcache1_{i}", [min(CB, nblk1 - i * CB), 128, 32 * 256], BF16) for i in range((nblk1 + CB - 1) // CB)]
    xg5 = xg.rearrange("(k h q r) t -> h q r k t", h=2, q=NQ, r=64)

    with contextlib.ExitStack() as es:
        S = Sched(nc)
        ps = [Buf(es.enter_context(nc.psum_tensor(f"ps{i}", [128, 512], F32)), f"ps{i}") for i in range(8)]
        midx_sb = [Buf(es.enter_context(nc.sbuf_tensor(f"midx_sb{l}", [128, KM, NTJ + 1], mybir.dt.int32)), f"midx{l}") for l in range(2)]
        for l in range(2):
            S.dma("sp", midx_sb[l].ap[:], L[l]["midx"], writes=[midx_sb[l]])
        for l in range(2):
            last = l == 1
            lam_init = 0.8 - 0.6 * math.exp(-0.3 * l)
            I1 = dict(sh); I1.update(L[l]); I1["ada_w"] = L[l]["ada_w"][:, 0:2 * D]; I1["ada_b"] = L[l]["ada_b"][:, 0:2 * KD]
            I1["hT"], I1["projT"], I1["convT"] = hT, projT, convT
            d_mix = Buf(None, f"d_mix{l}"); d_mixg = Buf(None, f"d_mixg{l}")

            def load_x(S_, xt, t0, n, l=l):
                if l == 0:
                    for kg in range(0, KD, 8):
                        k2 = min(KD, kg + 8)
                        S_.dma("sp", xt.ap[:, kg:k2, :n], xTb[kg * 128:k2 * 128, t0:t0 + n].rearrange("(k p) t -> p k t", p=128), writes=[xt])
                elif t0 < CTX:
                    for tq in range(NQ):
                        for kg in range(0, KD, 8):
                            k2 = min(KD, kg + 8)
                            for h in range(2):
                                S_.dma("sp", xt.ap[h * 64:(h + 1) * 64, kg:k2, tq * TLc:(tq + 1) * TLc], xg5[h, tq, :, kg:k2, 0:TLc], reads=[d_xg], writes=[xt])
                else:
                    lt = t0 - CTX
                    tq = lt // TLl; off = TLc + lt % TLl
                    for kg in range(0, KD, 8):
                        k2 = min(KD, kg + 8)
                        for h in range(2):
                            S_.dma("sp", xt.ap[h * 64:(h + 1) * 64, kg:k2, :n], xg5[h, tq, :, kg:k2, off:off + n], reads=[d_xg], writes=[xt])

            def store_mix(S_, r0, nr, t0, n, src, sbuf, dm, l=l):
                if t0 < CTX:
                    for tq in range(NQ):
                        S_.dma("pool", mixc[tq * 1024 + r0:tq * 1024 + r0 + nr, :], src[:, tq * TLc:(tq + 1) * TLc], reads=[sbuf], writes=[dm])
                else:
                    lt = t0 - CTX
                    tq = lt // TLl; j = (lt % TLl) // 512
                    base = (tq * NTJ + j) * 1024 + r0
                    S_.dma("pool", mixl[l][base:base + nr, :], src, reads=[sbuf], writes=[dm])

            emit_k1(nc, S, ps, cfg, last, lam_init, I1, f"a{l}_", load_x, store_mix, d_mix)
            if stop <= 4 * l + 0:
                break
            for c in range(NQ * NTJ):
                S.cc_allgather(mixl[l][c * 1024:(c + 1) * 1024, :], mixlg[l][c * NQ * 1024:(c + 1) * NQ * 1024, :], groups, reads=[d_mix], writes=[d_mixg])
            if not last:
                S.cc_allgather(mixc, mixcg, groups, reads=[d_mix], writes=[d_mixg])
            S.barrier()
            if stop <= 4 * l + 1:
                break
            I2 = dict(sh); I2.update(L[l]); I2["ada_w"] = L[l]["ada_w"][:, 2 * D:6 * D]; I2["ada_b"] = L[l]["ada_b"][:, 2 * KD:6 * KD]
            tl_c = 0 if last else TLc

            def load_mix(S_, mh, t0, n, kind, l=l, tl_c=tl_c):
                if kind == 1:
                    src = mixcg; col = NTJ
                else:
                    src = mixlg[l]; col = (t0 - tl_c) // 512
                for km in range(KM):
                    S_.idma(mh.ap[:, km, :n], src, midx_sb[l].ap[:, km, col:col + 1], reads=[d_mixg, midx_sb[l]], writes=[mh])

            if not last:
                emit_k2(nc, S, ps, cfg, NQ, False, False, I2, f"b{l}_", xTl, load_mix, x1loc, d_x1)
                if stop <= 4 * l + 2:
                    break
                for c in range(D // 64):
                    S.cc_allgather(x1loc[c * 64:(c + 1) * 64, :], xg[c * NQ * 64:(c + 1) * NQ * 64, :], groups, reads=[d_x1], writes=[d_xg])
                S.barrier()
            else:
                d_out = Buf(None, "d_out")
                emit_k2(nc, S, ps, cfg, NQ, True, True, I2, f"b{l}_", x1loc[:, TLc:TL0], load_mix, xout, d_out)
    return nc


def fused_inputs(cfg, inp, b, q, shared):
    D, KD, NQ, CTX, SEQ, MIX = cfg.D, cfg.KD, cfg.NQ, cfg.CTX, cfg.SEQ, cfg.MIX
    KM = MIX // 128
    TLc, TLl = CTX // NQ, SEQ // NQ
    NTJ = TLl // 512
    xTb = shared["xT"][b]
    m = {"xTb": xTb, "xTl": np.ascontiguousarray(xTb[:, tq_index(cfg, NQ, q, False)])}
    m["ccol"] = np.ascontiguousarray(np.stack([col_layout(inp["c"][b]), col_layout(inp["c_ctx"])], axis=-1))
    m["consts"] = CPACK; m["selc"] = SELC
    m["cosd"], m["sind"], m["cosr"], m["sinr"] = shared["tabs"]
    for l in range(2):
        k1 = k1_inputs(cfg, inp, l, b, q, xTb, shared["tabs"])
        m[f"w_in{l}"] = k1["w_in"]
        for n_, _ in K1_SMALL:
            m[f"{n_}{l}"] = k1[n_]
        m[f"ada_w{l}"] = inp["ada_w"][l]
        m[f"ada_b{l}"] = col_layout(inp["ada_b"][l])
        m[f"nmw{l}"] = col_layout(inp["norm_mix_w"][l]); m[f"nfw{l}"] = col_layout(inp["norm_ffn_w"][l])
        m[f"w_out{l}"] = shared["w_out"][l]
        ntj1 = NTJ + 1
        idx = np.zeros((128, KM, ntj1), np.int64)
        p = np.arange(128)
        for km in range(KM):
            qq = km // 8; rr = (km % 8) * 128 + p
            for j in range(NTJ):
                idx[:, km, j] = (q * NTJ + j) * (NQ * 1024) + qq * 1024 + rr
            idx[:, km, NTJ] = qq * (NQ * 1024) + q * 1024 + rr
        m[f"midx{l}"] = idx.astype(np.int32)
    m["wg0"] = inp["dense_w_gate"][0]; m["wu0"] = inp["dense_w_up"][0]; m["wd0"] = inp["dense_w_down"][0]
    m["wg1"] = inp["moe_w_gate"][0]; m["wu1"] = inp["moe_w_up"][0]; m["wd1"] = inp["moe_w_down"][0]
    m["rw1"] = np.ascontiguousarray(inp["moe_router"][0].reshape(KD, 128, 8).transpose(1, 0, 2))
    m["fnw"] = col_layout(inp["final_norm_w"])
    out = {}
    for k, v in m.items():
        out[k] = np.ascontiguousarray(v, dtype=(np.int32 if k.startswith("midx") else np.float32))
    return out


def run_fused(cfg, inp, stop=99):
    B, NQ = cfg.B, cfg.NQ
    TLl = cfg.SEQ // NQ
    perm = mix_perm(cfg)
    shared = {"tabs": rope_tables(cfg, 64) + rope_tables(cfg, 128),
              "xT": [np.ascontiguousarray(np.concatenate([inp["ctx"][b], inp["x"][b]], axis=0).T) for b in range(B)],
              "w_out": [np.ascontiguousarray(inp["w_out"][l][perm]) for l in range(2)]}
    nc = build_fused(cfg, stop)
    maps = [fused_inputs(cfg, inp, b, q, shared) for b in range(B) for q in range(NQ)]
    res = run_bass_kernel_spmd(nc, maps, core_ids=list(range(len(maps))))
    out = np.empty((B, cfg.SEQ, cfg.D), np.float32)
    for b in range(B):
        for q in range(NQ):
            out[b, q * TLl:(q + 1) * TLl, :] = res.results[b * NQ + q]["xout"].T
    return out


def kernel(**inputs):
    inp = {k: np.asarray(v) for k, v in inputs.items()}
    return run_fused(FULL, inp)
```

```python
import contextlib
import math
import numpy as np
import concourse.bass as bass
import concourse.mybir as mybir
from concourse.bass_utils import run_bass_kernel_spmd

F32 = mybir.dt.float32
BF16 = mybir.dt.bfloat16
AF = mybir.ActivationFunctionType
ALU = mybir.AluOpType
AX = mybir.AxisListType
EPS = 1e-6
SAME_SYNC = True


class Buf:
    __slots__ = ("ap", "name", "w", "r")

    def __init__(self, ap=None, name=""):
        self.ap = ap
        self.name = name
        self.w = None
        self.r = {}


class Sched:
    ENG = ("pe", "act", "dve", "pool", "sp")

    def __init__(self, nc, same_engine_sync=SAME_SYNC, ring=12):
        self.nc = nc
        self.engs = {"pe": nc.tensor, "act": nc.scalar, "dve": nc.vector, "pool": nc.gpsimd, "sp": nc.sync}
        self.semh = {}
        self.cnt = {}
        self.seen = {e: {} for e in self.ENG}
        self.same = same_engine_sync
        for e in self.ENG:
            self.semh[e] = nc.alloc_semaphore(name=f"s_{e}")
            self.cnt[e] = 0
        self.ring = ring
        self.dma_i = {}
        self.dma_val = {}
        for q in ("sp", "pool"):
            self.dma_i[q] = 0
            for s in range(ring):
                k = (q, s)
                self.semh[k] = nc.alloc_semaphore(name=f"d_{q}{s}")
                self.dma_val[k] = 0
        self.n_ins = 0
        self.n_wait = 0
        self.semh["cc"] = nc.alloc_semaphore(name="s_cc")
        self.cc_val = 0

    def cc_allgather(self, in_ap, out_ap, groups, reads=(), writes=()):
        self._deps("pool", reads, writes)
        ins = self.engs["pool"].collective_compute("AllGather", ALU.bypass, replica_groups=groups, ins=[in_ap], outs=[out_ap])
        self.cc_val += 1
        ins.then_inc(self.semh["cc"], 1)
        self.n_ins += 1
        self._record(("cc", self.cc_val), reads, writes)
        return ins

    def idma(self, out_ap, in_ap, idx_ap, reads=(), writes=()):
        q = "pool"
        self._deps(q, reads, writes)
        slot = self.dma_i[q] % self.ring
        self.dma_i[q] += 1
        k = (q, slot)
        prev = self.dma_val[k]
        if prev > 0:
            self._wait(q, (k, prev))
        self.dma_val[k] = prev + 16
        ins = self.engs[q].indirect_dma_start(out=out_ap, out_offset=None, in_=in_ap, in_offset=bass.IndirectOffsetOnAxis(ap=idx_ap, axis=0))
        ins.then_inc(self.semh[k], 16)
        self.n_ins += 1
        self._record((k, prev + 16), reads, writes)
        return ins

    def _wait(self, eng, ev):
        if ev is None:
            return
        key, val = ev
        if key == eng and (eng == "pe" or not self.same):
            return
        if self.seen[eng].get(key, 0) >= val:
            return
        self.engs[eng].wait_ge(self.semh[key], val)
        self.seen[eng][key] = val
        self.n_wait += 1

    def _deps(self, eng, reads, writes):
        for b in reads:
            self._wait(eng, b.w)
        for b in writes:
            self._wait(eng, b.w)
            for k, v in list(b.r.items()):
                self._wait(eng, (k, v))

    def _record(self, ev, reads, writes):
        k, v = ev
        for b in reads:
            if b.r.get(k, 0) < v:
                b.r[k] = v
        for b in writes:
            b.w = ev
            b.r = {}

    def op(self, eng, fn, reads=(), writes=(), signal=True):
        self._deps(eng, reads, writes)
        ins = fn(self.engs[eng])
        self.n_ins += 1
        if signal:
            self.cnt[eng] += 1
            ins.then_inc(self.semh[eng], 1)
            ev = (eng, self.cnt[eng])
        else:
            ev = (eng, self.cnt[eng] + 1)
        self._record(ev, reads, writes)
        return ins

    def dma(self, q, out_ap, in_ap, reads=(), writes=()):
        self._deps(q, reads, writes)
        slot = self.dma_i[q] % self.ring
        self.dma_i[q] += 1
        k = (q, slot)
        prev = self.dma_val[k]
        if prev > 0:
            self._wait(q, (k, prev))
        self.dma_val[k] = prev + 16
        ins = self.engs[q].dma_start(out=out_ap, in_=in_ap)
        ins.then_inc(self.semh[k], 16)
        self.n_ins += 1
        self._record((k, prev + 16), reads, writes)
        return ins

    def barrier(self):
        evs = [(e, self.cnt[e]) for e in self.ENG if self.cnt[e] > 0]
        evs += [(k, v) for k, v in self.dma_val.items() if v > 0]
        if self.cc_val > 0:
            evs.append(("cc", self.cc_val))
        for e in self.ENG:
            for ev in evs:
                if ev[0] == e and e == "pe":
                    continue
                key, val = ev
                if self.seen[e].get(key, 0) >= val:
                    continue
                self.engs[e].wait_ge(self.semh[key], val)
                self.seen[e][key] = val

    def act(self, out, in_, func, reads, writes, **kw):
        return self.op("act", lambda e: e.activation(out=out, in_=in_, func=func, **kw), reads, writes)

    def tt(self, eng, out, in0, in1, op, reads, writes):
        return self.op(eng, lambda e: e.tensor_tensor(out=out, in0=in0, in1=in1, op=op), reads, writes)

    def ts(self, eng, out, in0, s1, s2, op0, op1, reads, writes):
        if op1 is None:
            return self.op(eng, lambda e: e.tensor_scalar(out=out, in0=in0, scalar1=s1, scalar2=None, op0=op0), reads, writes)
        return self.op(eng, lambda e: e.tensor_scalar(out=out, in0=in0, scalar1=s1, scalar2=s2, op0=op0, op1=op1), reads, writes)

    def stt(self, out, in0, scalar, in1, op0, op1, reads, writes):
        return self.op("dve", lambda e: e.scalar_tensor_tensor(out=out, in0=in0, scalar=scalar, in1=in1, op0=op0, op1=op1), reads, writes)

    def mm(self, out, lhsT, rhs, reads, writes, start=True, stop=True, signal=True):
        return self.op("pe", lambda e: e.matmul(out, lhsT, rhs, start=start, stop=stop), reads, writes, signal=signal)

    def tr(self, out, in_, ident, reads, writes):
        return self.op("pe", lambda e: e.transpose(out, in_, ident), reads, writes)

    def copy(self, eng, out, in_, reads, writes):
        if eng == "act":
            return self.act(out, in_, AF.Copy, reads, writes)
        return self.op(eng, lambda e: e.tensor_copy(out=out, in_=in_), reads, writes)


class Cfg:
    def __init__(s, D, SEQ, CTX, NQ, DFF, DFE, NEXP=8, B=2):
        s.D, s.SEQ, s.CTX, s.NQ, s.DFF, s.DFE, s.NEXP, s.B = D, SEQ, CTX, NQ, DFF, DFE, NEXP, B
        s.KD = D // 128
        s.T = CTX + SEQ
        s.NCH = s.T // 128
        s.NCC = CTX // 128
        s.MIX = 1024 * NQ
        s.PC = 3344
        assert CTX % 128 == 0 and CTX <= 512 and SEQ % 512 == 0
        s.tiles = [(0, CTX)] + [(CTX + 512 * i, 512) for i in range(SEQ // 512)]
        s.GRID_W = 64


FULL = Cfg(4096, 8192, 256, 4, 11008, 4096)


def rope_tables(cfg, dh):
    seq = cfg.SEQ
    rows = seq // cfg.GRID_W
    r = np.repeat(np.arange(rows, dtype=np.float32), cfg.GRID_W)
    col = np.tile(np.arange(cfg.GRID_W, dtype=np.float32), rows)
    nf = dh // 4
    inv = (10000.0 ** (-np.arange(nf, dtype=np.float32) / nf)).astype(np.float32)
    ar = r[:, None] * inv
    ac = col[:, None] * inv
    ang = np.concatenate([ar, ar, ac, ac], axis=-1)
    cos = np.cos(ang).astype(np.float32)
    sin = np.sin(ang).astype(np.float32)
    cosT = np.concatenate([np.ones((dh, cfg.CTX), np.float32), cos.T], axis=1)
    sinT = np.concatenate([np.zeros((dh, cfg.CTX), np.float32), sin.T], axis=1)
    reps = 128 // dh
    return np.ascontiguousarray(np.tile(cosT, (reps, 1))), np.ascontiguousarray(np.tile(sinT, (reps, 1)))


def rot_matrix(dh):
    R = np.zeros((128, 128), np.float32)
    q = dh // 4
    for base in range(0, 128, dh):
        for m in range(dh):
            quarter = m // q
            if quarter % 2 == 0:
                R[base + m + q, base + m] = -1.0
            else:
                R[base + m - q, base + m] = 1.0
    return R


def const_pack():
    j = np.arange(128)[:, None].astype(np.float32)
    i = np.arange(128)[None, :].astype(np.float32)
    mats = {}
    mats["ident"] = np.eye(128, dtype=np.float32)
    mats["ones"] = np.ones((128, 128), np.float32)
    mats["dpos"] = np.maximum(i - j, 0.0)
    mats["ufs"] = (i > j).astype(np.float32)
    mats["dneg"] = np.maximum(j - i, 0.0)
    mats["ubs"] = (j > i).astype(np.float32)
    mats["i2"] = 2.0 * np.eye(128, dtype=np.float32)
    mats["uf"] = (j <= i).astype(np.float32)
    mats["ub"] = (j >= i).astype(np.float32)
    mats["sl"] = (j > i).astype(np.float32)
    mats["su"] = (j < i).astype(np.float32)
    mats["rowip1"] = np.broadcast_to(i + 1.0, (128, 128)).copy()
    mats["row128mi"] = np.broadcast_to(128.0 - i, (128, 128)).copy()
    mats["rot64"] = rot_matrix(64)
    mats["rot128"] = rot_matrix(128)
    cols = np.zeros((128, 128), np.float32)
    cols[:, 0] = 127.0 - np.arange(128)
    cols[:, 1] = np.arange(128)
    mats["cols"] = cols
    names = list(mats.keys())
    arr = np.stack([mats[n] for n in names], axis=1)
    return names, np.ascontiguousarray(arr)


CNAMES, CPACK = const_pack()
CI = {n: k for k, n in enumerate(CNAMES)}


def emit_k1(nc, S, ps, cfg, last, lam_init, I, pf, load_x, store_mix, d_mix):
    D, KD, T, NCH, NCC, CTX = cfg.D, cfg.KD, cfg.T, cfg.NCH, cfg.NCC, cfg.CTX
    tiles = cfg.tiles
    w_in, ada_w, ada_b, ccol, nmw, consts = I["w_in"], I["ada_w"], I["ada_b"], I["ccol"], I["nmw"], I["consts"]
    cosd, sind, cosr, sinr = I["cosd"], I["sind"], I["cosr"], I["sinr"]
    rdl, rnw, cw, cb, dtb, alog, dsk, snw, dlam, dnw = (I[k] for k in ("rdl", "rnw", "cw", "cb", "dtb", "alog", "dsk", "snw", "dlam", "dnw"))
    hT, projT, convT = I["hT"], I["projT"], I["convT"]
    d_hT = Buf(None, "d_hT"); d_proj = Buf(None, "d_proj"); d_conv = Buf(None, "d_conv")

    with contextlib.ExitStack() as es0:
        def SB(es, n, sh, dt=F32):
            return Buf(es.enter_context(nc.sbuf_tensor(pf + n, sh, dt)), n)
        C = SB(es0, "C", [128, len(CNAMES), 128])
        Cb = SB(es0, "Cb", [128, 2, 128], BF16)
        S.dma("sp", C.ap[:], consts, writes=[C])
        S.copy("dve", Cb.ap[:, 0, :], C.ap[:, CI["ones"], :], [C], [Cb])
        S.copy("dve", Cb.ap[:, 1, :], C.ap[:, CI["ident"], :], [C], [Cb])
        cm = lambda n: C.ap[:, CI[n], :]
        ones_bf = Cb.ap[:, 0, :]
        AB = SB(es0, "AB", [128, KD, 4])

        with contextlib.ExitStack() as es:
            sc = SB(es, "sc", [128, KD, 2]); adb = SB(es, "adb", [128, 2 * KD]); nm = SB(es, "nm", [128, KD])
            modc = SB(es, "modc", [128, 2 * KD, 2])
            wa = [SB(es, f"wa{i}", [128, KD, 512]) for i in range(2)]
            S.dma("sp", sc.ap[:], ccol, writes=[sc]); S.dma("sp", adb.ap[:], ada_b, writes=[adb]); S.dma("sp", nm.ap[:], nmw, writes=[nm])
            S.act(sc.ap[:], sc.ap[:], AF.Silu, [sc], [sc])
            nfc = 2 * KD
            pm = ps[0]
            for fg in range(0, nfc, 4):
                w = wa[(fg // 4) % 2]
                for kg in range(0, KD, 8):
                    k2 = min(KD, kg + 8)
                    S.dma("sp", w.ap[:, kg:k2, :], ada_w[kg * 128:k2 * 128, fg * 128:(fg + 4) * 128].rearrange("(k p) c -> p k c", p=128), writes=[w])
                for f in range(fg, fg + 4):
                    for k in range(KD):
                        S.mm(pm.ap[:, 2 * f:2 * f + 2], w.ap[:, k, (f - fg) * 128:(f - fg + 1) * 128], sc.ap[:, k, :], [w, sc], [pm],
                             start=(k == 0), stop=(k == KD - 1), signal=(k == KD - 1))
            for j in range(2):
                S.tt("dve", modc.ap[:, :, j], pm.ap[:, 0:2 * nfc].rearrange("p (f j) -> p f j", j=2)[:, :, j], adb.ap[:], ALU.add, [pm, adb], [modc])
            for j in range(2):
                S.stt(AB.ap[:, :, j], modc.ap[:, KD:2 * KD, j], 1.0, nm.ap[:], ALU.add, ALU.mult, [modc, nm], [AB])
                S.copy("dve", AB.ap[:, :, 2 + j], modc.ap[:, 0:KD, j], [modc], [AB])
        S.barrier()

        with contextlib.ExitStack() as es:
            xt = SB(es, "xt", [128, KD, 512]); ht = SB(es, "ht", [128, KD, 512], BF16)
            sq = [SB(es, f"sq{i}", [128, 512], BF16) for i in range(2)]
            tmp = [SB(es, f"tmp{i}", [128, 512]) for i in range(2)]
            rs = SB(es, "rs", [128, 512])
            for (t0, n) in tiles:
                j = 1 if t0 < CTX else 0
                load_x(S, xt, t0, n)
                for k in range(KD):
                    s_ = sq[k % 2]
                    S.act(s_.ap[:, :n], xt.ap[:, k, :n], AF.Square, [xt], [s_])
                    S.mm(ps[0].ap[:, :n], ones_bf, s_.ap[:, :n], [s_, Cb], [ps[0]], start=(k == 0), stop=(k == KD - 1))
                S.act(rs.ap[:, :n], ps[0].ap[:, :n], AF.Ln, [ps[0]], [rs], scale=1.0 / D, bias=EPS)
                S.act(rs.ap[:, :n], rs.ap[:, :n], AF.Exp, [rs], [rs], scale=-0.5)
                for k in range(KD):
                    t_ = tmp[k % 2]
                    S.tt("dve", t_.ap[:, :n], xt.ap[:, k, :n], rs.ap[:, :n], ALU.mult, [xt, rs], [t_])
                    S.act(ht.ap[:, k, :n], t_.ap[:, :n], AF.Identity, [t_, AB], [ht], scale=AB.ap[:, k, j:j + 1], bias=AB.ap[:, k, 2 + j:3 + j])
                for kg in range(0, KD, 8):
                    k2 = min(KD, kg + 8)
                    S.dma("sp", hT[kg * 128:k2 * 128, t0:t0 + n].rearrange("(k p) t -> p k t", p=128), ht.ap[:, kg:k2, :n], reads=[ht], writes=[d_hT])
        S.barrier()

        groups = [(0, 1024), (1024, 1552), (2576, 768)]
        with contextlib.ExitStack() as es:
            wg = SB(es, "wg", [128, KD, 1552], BF16)
            hb = [SB(es, f"hb{i}", [128, KD, 512], BF16) for i in range(2)]
            stg = [SB(es, f"stg{i}", [128, 512]) for i in range(4)]
            si = 0; pi = 0; ti = 0
            for (c0, ncols) in groups:
                for kg in range(0, KD, 8):
                    k2 = min(KD, kg + 8)
                    S.dma("pool", wg.ap[:, kg:k2, :ncols], w_in[kg * 128:k2 * 128, c0:c0 + ncols].rearrange("(k p) c -> p k c", p=128), writes=[wg])
                def load_h(h_, t0_, n_):
                    for kg in range(0, KD, 8):
                        k2 = min(KD, kg + 8)
                        S.dma("sp", h_.ap[:, kg:k2, :n_], hT[kg * 128:k2 * 128, t0_:t0_ + n_].rearrange("(k p) t -> p k t", p=128), reads=[d_hT], writes=[h_])

                load_h(hb[ti % 2], tiles[0][0], tiles[0][1])
                for tix, (t0, n) in enumerate(tiles):
                    h = hb[ti % 2]; ti += 1
                    if tix + 1 < len(tiles):
                        load_h(hb[ti % 2], tiles[tix + 1][0], tiles[tix + 1][1])
                    cs = 0
                    while cs < ncols:
                        m = min(128, ncols - cs)
                        p = ps[pi % 4]; pi += 1
                        for k in range(KD):
                            S.mm(p.ap[:m, :n], wg.ap[:, k, cs:cs + m], h.ap[:, k, :n], [wg, h], [p], start=(k == 0), stop=(k == KD - 1), signal=(k == KD - 1))
                        st = stg[si % 4]; si += 1
                        S.copy("act" if si % 2 else "dve", st.ap[:m, :n], p.ap[:m, :n], [p], [st])
                        S.dma("sp", projT[c0 + cs:c0 + cs + m, t0:t0 + n], st.ap[:m, :n], reads=[st], writes=[d_proj])
                        cs += m
        S.barrier()

        base_d = 2576
        with contextlib.ExitStack() as es:
            QT = SB(es, "QT", [128, T], BF16); KT = SB(es, "KT", [128, T], BF16); Vtm = SB(es, "Vtm", [128, NCH, 128], BF16)
            ld = [SB(es, f"ld{i}", [128, 512]) for i in range(2)]
            cs_t = SB(es, "cs_t", [128, 512]); sn_t = SB(es, "sn_t", [128, 512])
            t1 = SB(es, "t1", [128, 512]); t2 = SB(es, "t2", [128, 512]); sqb = SB(es, "sqb", [128, 512], BF16)
            P = [SB(es, f"P{i}", [128, 512], BF16) for i in range(4)]
            mx = SB(es, "mx", [128, 8]); negc = SB(es, "negc", [128, 2]); lamt = SB(es, "lamt", [128, 8]); lv = SB(es, "lv", [128, 4, 64])
            nwd = SB(es, "nwd", [128, 1]); rl = [SB(es, f"rl{i}", [128, 512]) for i in range(2)]
            ob = SB(es, "ob", [128, 512]); rsd = SB(es, "rsd", [128, 512]); oo = SB(es, "oo", [128, 512])
            S.dma("sp", lv.ap[:], dlam, writes=[lv]); S.dma("sp", nwd.ap[:], dnw, writes=[nwd])
            S.tt("dve", lv.ap[:, 0, :], lv.ap[:, 0, :], lv.ap[:, 1, :], ALU.mult, [lv], [lv])
            S.tt("dve", lv.ap[:, 2, :], lv.ap[:, 2, :], lv.ap[:, 3, :], ALU.mult, [lv], [lv])
            S.op("dve", lambda e: e.tensor_reduce(out=lamt.ap[:, 0:1], in_=lv.ap[:, 0, :], axis=AX.X, op=ALU.add), [lv], [lamt])
            S.op("dve", lambda e: e.tensor_reduce(out=lamt.ap[:, 1:2], in_=lv.ap[:, 2, :], axis=AX.X, op=ALU.add), [lv], [lamt])
            S.act(lamt.ap[:, 0:2], lamt.ap[:, 0:2], AF.Exp, [lamt], [lamt])
            S.tt("dve", lamt.ap[:, 2:3], lamt.ap[:, 1:2], lamt.ap[:, 0:1], ALU.subtract, [lamt], [lamt])
            S.ts("dve", lamt.ap[:, 3:4], lamt.ap[:, 2:3], -lam_init, None, ALU.add, None, [lamt], [lamt])
            S.ts("dve", nwd.ap[:], nwd.ap[:], 1.0 - lam_init, None, ALU.mult, None, [nwd], [nwd])
            neglam = lamt.ap[:, 3:4]
            for hd in range(2):
                rq = base_d + hd * 128; rk = base_d + 256 + hd * 128; rv = base_d + 512 + hd * 128
                S.op("dve", lambda e: e.memset(mx.ap[:], 0.0), [], [mx])
                li = 0
                for (t0, n) in tiles:
                    S.dma("sp", cs_t.ap[:, :n], cosd[:, t0:t0 + n], writes=[cs_t]); S.dma("sp", sn_t.ap[:, :n], sind[:, t0:t0 + n], writes=[sn_t])
                    for which, row, dst in ((0, rq, QT), (1, rk, KT)):
                        l = ld[li % 2]; li += 1
                        S.dma("sp", l.ap[:, :n], projT[row:row + 128, t0:t0 + n], reads=[d_proj], writes=[l])
                        S.act(sqb.ap[:, :n], l.ap[:, :n], AF.Square, [l], [sqb])
                        for m in range(2):
                            S.mm(ps[m].ap[:, :n], ones_bf[m * 64:(m + 1) * 64, :], sqb.ap[m * 64:(m + 1) * 64, :n], [sqb, Cb], [ps[m]])
                            S.op("dve", lambda e: e.tensor_reduce(out=mx.ap[:, 4 + m:5 + m], in_=ps[m].ap[:, :n], axis=AX.X, op=ALU.max), [ps[m]], [mx])
                            c_ = which * 2 + m
                            S.tt("dve", mx.ap[:, c_:c_ + 1], mx.ap[:, c_:c_ + 1], mx.ap[:, 4 + m:5 + m], ALU.max, [mx], [mx])
                        S.mm(ps[2].ap[:, :n], cm("rot64"), l.ap[:, :n], [C, l], [ps[2]])
                        S.tt("dve", t1.ap[:, :n], l.ap[:, :n], cs_t.ap[:, :n], ALU.mult, [l, cs_t], [t1])
                        S.tt("dve", t2.ap[:, :n], ps[2].ap[:, :n], sn_t.ap[:, :n], ALU.mult, [ps[2], sn_t], [t2])
                        S.tt("pool", dst.ap[:, t0:t0 + n], t1.ap[:, :n], t2.ap[:, :n], ALU.add, [t1, t2], [dst])
                    l = ld[li % 2]; li += 1
                    S.dma("sp", l.ap[:, :n], projT[rv:rv + 128, t0:t0 + n], reads=[d_proj], writes=[l])
                    for cc in range(n // 128):
                        S.tr(ps[3].ap[:, 0:128], l.ap[:, cc * 128:(cc + 1) * 128], cm("ident"), [l, C], [ps[3]])
                        S.copy("act", Vtm.ap[:, t0 // 128 + cc, :], ps[3].ap[:, 0:128], [ps[3]], [Vtm])
                S.tt("dve", mx.ap[:, 6:8], mx.ap[:, 0:2], mx.ap[:, 2:4], ALU.mult, [mx], [mx])
                S.act(mx.ap[:, 6:8], mx.ap[:, 6:8], AF.Ln, [mx], [mx])
                S.act(mx.ap[:, 6:8], mx.ap[:, 6:8], AF.Exp, [mx], [mx], scale=0.5)
                S.ts("dve", negc.ap[:], mx.ap[:, 6:8], -0.125, None, ALU.mult, None, [mx], [negc])
                qtiles = [(t0, n, list(range(NCH))) for (t0, n) in tiles if t0 >= CTX]
                if not last:
                    qtiles = [(0, CTX, list(range(NCC)))] + qtiles
                pj = 0
                LA = 2
                for (t0, n, kcs) in qtiles:
                    its = [(i, kc, m) for i, kc in enumerate(kcs) for m in range(2)]

                    def issue_score(idx, pj=pj, t0=t0, n=n, its=its):
                        i, kc, m = its[idx]
                        pS = ps[(pj + idx) % 4]
                        S.mm(pS.ap[:, :n], KT.ap[m * 64:(m + 1) * 64, kc * 128:(kc + 1) * 128], QT.ap[m * 64:(m + 1) * 64, t0:t0 + n], [KT, QT], [pS])

                    for idx in range(min(LA, len(its))):
                        issue_score(idx)
                    for idx, (i, kc, m) in enumerate(its):
                        if idx % 2 == 0:
                            for idx2 in (idx + LA, idx + LA + 1):
                                if idx2 < len(its):
                                    issue_score(idx2)
                        pS = ps[(pj + idx) % 4]; Pm = P[(pj + idx) % 4]
                        S.act(Pm.ap[:, :n], pS.ap[:, :n], AF.Exp, [pS, negc], [Pm], scale=0.125, bias=negc.ap[:, m:m + 1])
                        S.mm(ps[4 + m].ap[:, :n], Vtm.ap[:, kc, :], Pm.ap[:, :n], [Vtm, Pm], [ps[4 + m]], start=(i == 0), stop=(i == len(kcs) - 1), signal=False)
                        S.mm(ps[6 + m].ap[:, :n], ones_bf, Pm.ap[:, :n], [Cb, Pm], [ps[6 + m]], start=(i == 0), stop=(i == len(kcs) - 1))
                    pj += len(its)
                    for m in range(2):
                        S.op("dve", lambda e: e.reciprocal(out=rl[m].ap[:, :n], in_=ps[6 + m].ap[:, :n]), [ps[6 + m]], [rl[m]])
                    S.tt("dve", t1.ap[:, :n], ps[4].ap[:, :n], rl[0].ap[:, :n], ALU.mult, [ps[4], rl[0]], [t1])
                    S.tt("dve", t2.ap[:, :n], ps[5].ap[:, :n], rl[1].ap[:, :n], ALU.mult, [ps[5], rl[1]], [t2])
                    S.stt(ob.ap[:, :n], t2.ap[:, :n], neglam, t1.ap[:, :n], ALU.mult, ALU.add, [t1, t2, lamt], [ob])
                    S.act(sqb.ap[:, :n], ob.ap[:, :n], AF.Square, [ob], [sqb])
                    S.mm(ps[0].ap[:, :n], ones_bf, sqb.ap[:, :n], [sqb, Cb], [ps[0]])
                    S.act(rsd.ap[:, :n], ps[0].ap[:, :n], AF.Ln, [ps[0]], [rsd], scale=1.0 / 128, bias=EPS)
                    S.act(rsd.ap[:, :n], rsd.ap[:, :n], AF.Exp, [rsd], [rsd], scale=-0.5)
                    S.stt(oo.ap[:, :n], ob.ap[:, :n], nwd.ap[:, 0:1], rsd.ap[:, :n], ALU.mult, ALU.mult, [ob, nwd, rsd], [oo])
                    store_mix(S, 768 + hd * 128, 128, t0, n, oo.ap[:, :n], oo, d_mix)
        S.barrier()

        ksc = 128.0 ** -0.5
        with contextlib.ExitStack() as es:
            QT = SB(es, "rQT", [128, T], BF16); KT = SB(es, "rKT", [128, T], BF16)
            Vtm = SB(es, "rVtm", [128, NCH, 128], BF16); Kf = SB(es, "Kf", [128, NCH, 128], BF16); Kb = SB(es, "Kb", [128, NCH, 128], BF16)
            Sf = SB(es, "Sf", [128, NCH, 128], BF16); Sbk = SB(es, "Sbk", [128, NCH, 128], BF16)
            ld = [SB(es, f"rld{i}", [128, 512]) for i in range(2)]
            cs_t = SB(es, "rcs", [128, 512]); sn_t = SB(es, "rsn", [128, 512])
            t1 = SB(es, "rt1", [128, 512]); t2 = SB(es, "rt2", [128, 512]); kro = SB(es, "kro", [128, 512])
            lg = SB(es, "lg", [128, 4]); M = SB(es, "M", [128, 128]); M2 = SB(es, "M2", [128, 128]); wfb = SB(es, "wfb", [128, 2])
            G = SB(es, "G", [128, 2, 128]); gT = SB(es, "gT", [128, 2]); nwr = SB(es, "nwr", [128, 2])
            St = SB(es, "St", [128, 128]); AT = SB(es, "AT", [128, 128], BF16)
            Qf = SB(es, "Qf", [128, 128], BF16); Qb = SB(es, "Qb", [128, 128], BF16)
            yt = SB(es, "yt", [128, 512]); sqb = SB(es, "rsqb", [128, 512], BF16); rsd = SB(es, "rrsd", [128, 512])
            gl = SB(es, "gl", [128, 512]); oo = SB(es, "roo", [128, 512])
            S.dma("sp", lg.ap[:], rdl, writes=[lg]); S.dma("sp", nwr.ap[:], rnw, writes=[nwr])
            S.act(lg.ap[:], lg.ap[:], AF.Exp, [lg], [lg], scale=-1.0)
            S.act(lg.ap[:], lg.ap[:], AF.Ln, [lg], [lg], bias=1.0)
            S.ts("dve", lg.ap[:], lg.ap[:], -1.0, None, ALU.mult, None, [lg], [lg])
            for hr in range(2):
                lgf = lg.ap[:, hr:hr + 1]; lgb = lg.ap[:, 2 + hr:3 + hr]
                rq = hr * 128; rk = 256 + hr * 128; rv = 512 + hr * 128; rg = 768 + hr * 128
                S.act(M.ap[:], cm("dpos"), AF.Exp, [C, lg], [M], scale=lgf)
                S.tt("dve", M.ap[:], M.ap[:], cm("ufs"), ALU.mult, [M, C], [M])
                S.act(M2.ap[:], cm("dneg"), AF.Exp, [C, lg], [M2], scale=lgb)
                S.tt("dve", M2.ap[:], M2.ap[:], cm("ubs"), ALU.mult, [M2, C], [M2])
                S.tt("dve", M.ap[:], M.ap[:], M2.ap[:], ALU.add, [M, M2], [M])
                S.tt("dve", M.ap[:], M.ap[:], cm("i2"), ALU.add, [M, C], [M])
                S.ts("dve", M.ap[:], M.ap[:], ksc, None, ALU.mult, None, [M], [M])
                S.act(wfb.ap[:, 0:1], C.ap[:, CI["cols"], 0:1], AF.Exp, [C, lg], [wfb], scale=lgf)
                S.act(wfb.ap[:, 1:2], C.ap[:, CI["cols"], 1:2], AF.Exp, [C, lg], [wfb], scale=lgb)
                S.ts("dve", wfb.ap[:], wfb.ap[:], ksc, None, ALU.mult, None, [wfb], [wfb])
                S.act(G.ap[:, 0, :], cm("rowip1"), AF.Exp, [C, lg], [G], scale=lgf)
                S.act(G.ap[:, 1, :], cm("row128mi"), AF.Exp, [C, lg], [G], scale=lgb)
                S.act(gT.ap[:, 0:1], lgf, AF.Exp, [lg], [gT], scale=128.0)
                S.act(gT.ap[:, 1:2], lgb, AF.Exp, [lg], [gT], scale=128.0)
                li = 0
                for (t0, n) in tiles:
                    S.dma("sp", cs_t.ap[:, :n], cosr[:, t0:t0 + n], writes=[cs_t]); S.dma("sp", sn_t.ap[:, :n], sinr[:, t0:t0 + n], writes=[sn_t])
                    for which, row in ((0, rq), (1, rk)):
                        l = ld[li % 2]; li += 1
                        S.dma("sp", l.ap[:, :n], projT[row:row + 128, t0:t0 + n], reads=[d_proj], writes=[l])
                        S.mm(ps[2].ap[:, :n], cm("rot128"), l.ap[:, :n], [C, l], [ps[2]])
                        S.tt("dve", t1.ap[:, :n], l.ap[:, :n], cs_t.ap[:, :n], ALU.mult, [l, cs_t], [t1])
                        S.tt("dve", t2.ap[:, :n], ps[2].ap[:, :n], sn_t.ap[:, :n], ALU.mult, [ps[2], sn_t], [t2])
                        if which == 0:
                            S.tt("pool", QT.ap[:, t0:t0 + n], t1.ap[:, :n], t2.ap[:, :n], ALU.add, [t1, t2], [QT])
                        else:
                            S.tt("pool", kro.ap[:, :n], t1.ap[:, :n], t2.ap[:, :n], ALU.add, [t1, t2], [kro])
                            S.copy("act", KT.ap[:, t0:t0 + n], kro.ap[:, :n], [kro], [KT])
                            for cc in range(n // 128):
                                c = t0 // 128 + cc
                                S.tr(ps[3].ap[:, 0:128], kro.ap[:, cc * 128:(cc + 1) * 128], cm("ident"), [kro, C], [ps[3]])
                                S.act(Kf.ap[:, c, :], ps[3].ap[:, 0:128], AF.Copy, [ps[3], wfb], [Kf], scale=wfb.ap[:, 0:1])
                                S.act(Kb.ap[:, c, :], ps[3].ap[:, 0:128], AF.Copy, [ps[3], wfb], [Kb], scale=wfb.ap[:, 1:2])
                    l = ld[li % 2]; li += 1
                    S.dma("sp", l.ap[:, :n], projT[rv:rv + 128, t0:t0 + n], reads=[d_proj], writes=[l])
                    for cc in range(n // 128):
                        S.tr(ps[1].ap[:, 0:128], l.ap[:, cc * 128:(cc + 1) * 128], cm("ident"), [l, C], [ps[1]])
                        S.copy("act", Vtm.ap[:, t0 // 128 + cc, :], ps[1].ap[:, 0:128], [ps[1]], [Vtm])
                fwd = list(range(NCH))
                bwd = list(range(NCC - 1, -1, -1)) + list(range(NCH - 1, NCC - 1, -1))
                for order, Kx, Sx, gcol in ((fwd, Kf, Sf, 0), (bwd, Kb, Sbk, 1)):
                    S.op("dve", lambda e: e.memset(St.ap[:], 0.0), [], [St])
                    for idx, c in enumerate(order):
                        S.copy("act", Sx.ap[:, c, :], St.ap[:], [St], [Sx])
                        if idx < len(order) - 1:
                            S.mm(ps[0].ap[:, 0:128], Kx.ap[:, c, :], Vtm.ap[:, c, :], [Kx, Vtm], [ps[0]])
                            S.stt(St.ap[:], St.ap[:], gT.ap[:, gcol:gcol + 1], ps[0].ap[:, 0:128], ALU.mult, ALU.add, [St, gT, ps[0]], [St])
                for (t0, n) in tiles:
                    if last and t0 < CTX:
                        continue
                    for cc in range(n // 128):
                        c = t0 // 128 + cc
                        sl = slice(c * 128, (c + 1) * 128)
                        S.mm(ps[4].ap[:, 0:128], KT.ap[:, sl], QT.ap[:, sl], [KT, QT], [ps[4]])
                        S.tt("dve", AT.ap[:], ps[4].ap[:, 0:128], M.ap[:], ALU.mult, [ps[4], M], [AT])
                        S.tt("pool", Qf.ap[:], QT.ap[:, sl], G.ap[:, 0, :], ALU.mult, [QT, G], [Qf])
                        S.tt("pool", Qb.ap[:], QT.ap[:, sl], G.ap[:, 1, :], ALU.mult, [QT, G], [Qb])
                        S.mm(ps[5].ap[:, 0:128], Vtm.ap[:, c, :], AT.ap[:], [Vtm, AT], [ps[5]], start=True, stop=False)
                        S.mm(ps[5].ap[:, 0:128], Sf.ap[:, c, :], Qf.ap[:], [Sf, Qf], [ps[5]], start=False, stop=False)
                        S.mm(ps[5].ap[:, 0:128], Sbk.ap[:, c, :], Qb.ap[:], [Sbk, Qb], [ps[5]], start=False, stop=True)
                        S.copy("act", yt.ap[:, cc * 128:(cc + 1) * 128], ps[5].ap[:, 0:128], [ps[5]], [yt])
                    S.act(sqb.ap[:, :n], yt.ap[:, :n], AF.Square, [yt], [sqb])
                    S.mm(ps[6].ap[:, :n], ones_bf, sqb.ap[:, :n], [sqb, Cb], [ps[6]])
                    S.act(rsd.ap[:, :n], ps[6].ap[:, :n], AF.Ln, [ps[6]], [rsd], scale=1.0 / 128, bias=EPS)
                    S.act(rsd.ap[:, :n], rsd.ap[:, :n], AF.Exp, [rsd], [rsd], scale=-0.5)
                    S.dma("sp", gl.ap[:, :n], projT[rg:rg + 128, t0:t0 + n], reads=[d_proj], writes=[gl])
                    S.act(gl.ap[:, :n], gl.ap[:, :n], AF.Silu, [gl], [gl])
                    S.stt(oo.ap[:, :n], yt.ap[:, :n], nwr.ap[:, hr:hr + 1], rsd.ap[:, :n], ALU.mult, ALU.mult, [yt, nwr, rsd], [oo])
                    S.tt("dve", oo.ap[:, :n], oo.ap[:, :n], gl.ap[:, :n], ALU.mult, [oo, gl], [oo])
                    store_mix(S, hr * 128, 128, t0, n, oo.ap[:, :n], oo, d_mix)
        S.barrier()

        base_s = 1024
        segs = [(0, CTX), (CTX, T)]
        with contextlib.ExitStack() as es:
            cwt = SB(es, "cwt", [128, 8, 5]); cbt = SB(es, "cbt", [128, 8])
            S.dma("sp", cwt.ap[:], cw, writes=[cwt]); S.dma("sp", cbt.ap[:], cb, writes=[cbt])
            with contextlib.ExitStack() as es2:
                xr = SB(es2, "xr", [128, T]); yc = SB(es2, "yc", [128, T])
                for ch in range(8):
                    r0 = base_s + 512 + ch * 128
                    S.dma("sp", xr.ap[:], projT[r0:r0 + 128, :], reads=[d_proj], writes=[xr])
                    for (a, b) in segs:
                        S.ts("dve", yc.ap[:, a:b], xr.ap[:, a:b], cwt.ap[:, ch, 2:3], cbt.ap[:, ch:ch + 1], ALU.mult, ALU.add, [xr, cwt, cbt], [yc])
                        for k in (0, 1, 3, 4):
                            off = k - 2
                            lo = max(a, a - off); hi = min(b, b - off)
                            S.stt(yc.ap[:, lo:hi], xr.ap[:, lo + off:hi + off], cwt.ap[:, ch, k:k + 1], yc.ap[:, lo:hi], ALU.mult, ALU.add, [xr, cwt, yc], [yc])
                    S.act(yc.ap[:], yc.ap[:], AF.Silu, [yc], [yc])
                    S.dma("sp", convT[ch * 128:(ch + 1) * 128, :], yc.ap[:], reads=[yc], writes=[d_conv])
            S.barrier()
            dt_tm = SB(es, "dt_tm", [128, NCH, 16]); a_tm = SB(es, "a_tm", [128, NCH, 16]); aneg = SB(es, "aneg", [128, 16])
            dsk_t = SB(es, "dsk_t", [128, 8]); snw_t = SB(es, "snw_t", [64, 8])
            S.dma("sp", aneg.ap[:], alog, writes=[aneg]); S.dma("sp", dsk_t.ap[:], dsk, writes=[dsk_t]); S.dma("sp", snw_t.ap[:], snw, writes=[snw_t])
            S.act(aneg.ap[:], aneg.ap[:], AF.Exp, [aneg], [aneg])
            S.ts("dve", aneg.ap[:], aneg.ap[:], -1.0, None, ALU.mult, None, [aneg], [aneg])
            with contextlib.ExitStack() as es2:
                dx = SB(es2, "dx", [16, T]); da = SB(es2, "da", [16, T]); dtb_t = SB(es2, "dtb_t", [16, 1])
                S.dma("sp", dx.ap[:], projT[base_s + 1536:base_s + 1552, :], reads=[d_proj], writes=[dx])
                S.dma("sp", dtb_t.ap[:], dtb, writes=[dtb_t])
                S.ts("dve", dx.ap[:], dx.ap[:], dtb_t.ap[:, 0:1], None, ALU.add, None, [dx, dtb_t], [dx])
                S.act(da.ap[:], dx.ap[:], AF.Abs, [dx], [da])
                S.act(da.ap[:], da.ap[:], AF.Exp, [da], [da], scale=-1.0)
                S.act(da.ap[:], da.ap[:], AF.Ln, [da], [da], bias=1.0)
                S.ts("dve", dx.ap[:], dx.ap[:], 0.0, None, ALU.max, None, [dx], [dx])
                S.tt("dve", dx.ap[:], dx.ap[:], da.ap[:], ALU.add, [dx, da], [dx])
                for c in range(NCH):
                    S.tr(ps[0].ap[:, 0:16], dx.ap[:, c * 128:(c + 1) * 128], C.ap[0:16, CI["ident"], 0:16], [dx, C], [ps[0]])
                    S.copy("act", dt_tm.ap[:, c, :], ps[0].ap[:, 0:16], [ps[0]], [dt_tm])
                    S.tt("dve", a_tm.ap[:, c, :], dt_tm.ap[:, c, :], aneg.ap[:], ALU.mult, [dt_tm, aneg], [a_tm])
            S.barrier()
            BT = SB(es, "BT", [128, T], BF16); CT = SB(es, "CT", [128, T], BF16)
            Btm = SB(es, "Btm", [128, NCH, 128], BF16); xsb = SB(es, "xsb", [128, NCH, 256], BF16)
            Sf = SB(es, "sSf", [128, NCH, 256], BF16); Sbk = SB(es, "sSb", [128, NCH, 256], BF16)
            wfb = SB(es, "swfb", [128, NCH, 8]); etot = SB(es, "etot", [128, NCH, 8]); ew = SB(es, "ew", [128, 16])
            ld = [SB(es, f"sld{i}", [128, 512]) for i in range(2)]
            St = SB(es, "sSt", [128, 256]); vp = SB(es, "vp", [128, 256], BF16)
            scs = SB(es, "scs", [128, 128])
            rf_l = [SB(es, f"rf{i}", [128, 128]) for i in range(2)]; rb_l = [SB(es, f"rb{i}", [128, 128]) for i in range(2)]
            E_l = [SB(es, f"E{i}", [128, 512]) for i in range(2)]
            u1_l = [SB(es, f"u1{i}", [128, 128]) for i in range(2)]; u2_l = [SB(es, f"u2{i}", [128, 128]) for i in range(2)]
            AT_l = [SB(es, f"sAT{i}", [128, 128], BF16) for i in range(2)]
            Qf_l = [SB(es, f"sQf{i}", [128, 128], BF16) for i in range(2)]; Qb_l = [SB(es, f"sQb{i}", [128, 128], BF16) for i in range(2)]
            xs_t = SB(es, "xs_t", [64, 512]); z_t = SB(es, "z_t", [64, 512]); yd = [SB(es, f"yd{h}", [64, 512]) for h in range(4)]
            sq4 = SB(es, "sq4", [64, 512], BF16); rsd = SB(es, "srsd", [64, 512]); oo = SB(es, "soo", [64, 512])
            for gs in range(2):
                fc = gs * 4; bc = 8 + gs * 4
                li = 0
                for (t0, n) in tiles:
                    for which, dst in ((0, BT), (1, CT)):
                        l = ld[li % 2]; li += 1
                        r0 = 512 + which * 256 + gs * 128
                        S.dma("sp", l.ap[:, :n], convT[r0:r0 + 128, t0:t0 + n], reads=[d_conv], writes=[l])
                        S.copy("act", dst.ap[:, t0:t0 + n], l.ap[:, :n], [l], [dst])
                        if which == 0:
                            for cc in range(n // 128):
                                S.tr(ps[0].ap[:, 0:128], l.ap[:, cc * 128:(cc + 1) * 128], cm("ident"), [l, C], [ps[0]])
                                S.copy("act", Btm.ap[:, t0 // 128 + cc, :], ps[0].ap[:, 0:128], [ps[0]], [Btm])
                    for half in range(2):
                        l = ld[li % 2]; li += 1
                        r0 = gs * 256 + half * 128
                        S.dma("sp", l.ap[:, :n], convT[r0:r0 + 128, t0:t0 + n], reads=[d_conv], writes=[l])
                        for cc in range(n // 128):
                            S.tr(ps[1].ap[:, 0:128], l.ap[:, cc * 128:(cc + 1) * 128], cm("ident"), [l, C], [ps[1]])
                            S.copy("act", xsb.ap[:, t0 // 128 + cc, half * 128:(half + 1) * 128], ps[1].ap[:, 0:128], [ps[1]], [xsb])
                for c in range(NCH):
                    S.mm(ps[2].ap[:, 0:4], cm("sl"), a_tm.ap[:, c, fc:fc + 4], [C, a_tm], [ps[2]])
                    S.mm(ps[2].ap[:, 4:8], cm("su"), a_tm.ap[:, c, bc:bc + 4], [C, a_tm], [ps[2]])
                    S.mm(ps[2].ap[:, 8:12], cm("ones"), a_tm.ap[:, c, fc:fc + 4], [C, a_tm], [ps[2]])
                    S.mm(ps[2].ap[:, 12:16], cm("ones"), a_tm.ap[:, c, bc:bc + 4], [C, a_tm], [ps[2]])
                    S.act(ew.ap[:], ps[2].ap[:, 0:16], AF.Exp, [ps[2]], [ew])
                    S.tt("dve", wfb.ap[:, c, 0:4], ew.ap[:, 0:4], dt_tm.ap[:, c, fc:fc + 4], ALU.mult, [ew, dt_tm], [wfb])
                    S.tt("dve", wfb.ap[:, c, 4:8], ew.ap[:, 4:8], dt_tm.ap[:, c, bc:bc + 4], ALU.mult, [ew, dt_tm], [wfb])
                    S.copy("dve", etot.ap[:, c, :], ew.ap[:, 8:16], [ew], [etot])
                fwd = list(range(NCH))
                bwd = list(range(NCC - 1, -1, -1)) + list(range(NCH - 1, NCC - 1, -1))
                for order, Sx, o4 in ((fwd, Sf, 0), (bwd, Sbk, 4)):
                    S.op("dve", lambda e: e.memset(St.ap[:], 0.0), [], [St])
                    for idx, c in enumerate(order):
                        S.copy("act", Sx.ap[:, c, :], St.ap[:], [St], [Sx])
                        if idx < len(order) - 1:
                            for h in range(4):
                                S.ts("pool", vp.ap[:, h * 64:(h + 1) * 64], xsb.ap[:, c, h * 64:(h + 1) * 64], wfb.ap[:, c, o4 + h:o4 + h + 1], None, ALU.mult, None, [xsb, wfb], [vp])
                            S.mm(ps[3].ap[:, 0:256], Btm.ap[:, c, :], vp.ap[:], [Btm, vp], [ps[3]])
                            for h in range(4):
                                S.stt(St.ap[:, h * 64:(h + 1) * 64], St.ap[:, h * 64:(h + 1) * 64], etot.ap[:, c, o4 + h:o4 + h + 1], ps[3].ap[:, h * 64:(h + 1) * 64],
                                      ALU.mult, ALU.add, [St, etot, ps[3]], [St])
                for (t0, n) in tiles:
                    if last and t0 < CTX:
                        continue
                    for cc in range(n // 128):
                        c = t0 // 128 + cc
                        sl = slice(c * 128, (c + 1) * 128)
                        S.mm(ps[0].ap[:, 0:128], BT.ap[:, sl], CT.ap[:, sl], [BT, CT], [ps[0]])
                        S.copy("act", scs.ap[:], ps[0].ap[:, 0:128], [ps[0]], [scs])
                        for h in range(4):
                            rf, rb, E, u1, u2, AT, Qf, Qb = (x_[h % 2] for x_ in (rf_l, rb_l, E_l, u1_l, u2_l, AT_l, Qf_l, Qb_l))
                            pE = ps[1 + h % 2]
                            S.ts("dve", rf.ap[:], cm("uf"), a_tm.ap[:, c, fc + h:fc + h + 1], None, ALU.mult, None, [C, a_tm], [rf])
                            S.ts("dve", rb.ap[:], cm("ub"), a_tm.ap[:, c, bc + h:bc + h + 1], None, ALU.mult, None, [C, a_tm], [rb])
                            S.mm(pE.ap[:, 0:128], cm("sl"), rf.ap[:], [C, rf], [pE])
                            S.mm(pE.ap[:, 128:256], cm("su"), rb.ap[:], [C, rb], [pE])
                            S.mm(pE.ap[:, 256:384], cm("ones"), rf.ap[:], [C, rf], [pE])
                            S.mm(pE.ap[:, 384:512], cm("ones"), rb.ap[:], [C, rb], [pE])
                            S.act(E.ap[:], pE.ap[:], AF.Exp, [pE], [E])
                            S.stt(u1.ap[:], E.ap[:, 0:128], dt_tm.ap[:, c, fc + h:fc + h + 1], cm("uf"), ALU.mult, ALU.mult, [E, dt_tm, C], [u1])
                            S.stt(u2.ap[:], E.ap[:, 128:256], dt_tm.ap[:, c, bc + h:bc + h + 1], cm("ub"), ALU.mult, ALU.mult, [E, dt_tm, C], [u2])
                            S.tt("pool", u1.ap[:], u1.ap[:], u2.ap[:], ALU.add, [u1, u2], [u1])
                            S.tt("pool", AT.ap[:], u1.ap[:], scs.ap[:], ALU.mult, [u1, scs], [AT])
                            S.tt("pool", Qf.ap[:], CT.ap[:, sl], E.ap[:, 256:384], ALU.mult, [CT, E], [Qf])
                            S.tt("pool", Qb.ap[:], CT.ap[:, sl], E.ap[:, 384:512], ALU.mult, [CT, E], [Qb])
                            py = ps[4 + h]
                            S.mm(py.ap[0:64, cc * 128:(cc + 1) * 128], xsb.ap[:, c, h * 64:(h + 1) * 64], AT.ap[:], [xsb, AT], [py], start=True, stop=False)
                            S.mm(py.ap[0:64, cc * 128:(cc + 1) * 128], Sf.ap[:, c, h * 64:(h + 1) * 64], Qf.ap[:], [Sf, Qf], [py], start=False, stop=False)
                            S.mm(py.ap[0:64, cc * 128:(cc + 1) * 128], Sbk.ap[:, c, h * 64:(h + 1) * 64], Qb.ap[:], [Sbk, Qb], [py], start=False, stop=True)
                    for h in range(4):
                        rx = gs * 256 + h * 64
                        S.dma("sp", xs_t.ap[:, :n], convT[rx:rx + 64, t0:t0 + n], reads=[d_conv], writes=[xs_t])
                        S.dma("sp", z_t.ap[:, :n], projT[base_s + rx:base_s + rx + 64, t0:t0 + n], reads=[d_proj], writes=[z_t])
                        S.stt(yd[h].ap[:, :n], xs_t.ap[:, :n], dsk_t.ap[0:64, gs * 4 + h:gs * 4 + h + 1], ps[4 + h].ap[0:64, :n], ALU.mult, ALU.add, [xs_t, dsk_t, ps[4 + h]], [yd[h]])
                        S.act(z_t.ap[:, :n], z_t.ap[:, :n], AF.Silu, [z_t], [z_t])
                        S.tt("dve", yd[h].ap[:, :n], yd[h].ap[:, :n], z_t.ap[:, :n], ALU.mult, [yd[h], z_t], [yd[h]])
                        S.act(sq4.ap[:, :n], yd[h].ap[:, :n], AF.Square, [yd[h]], [sq4])
                        S.mm(ps[3].ap[0:64, :n], ones_bf[0:64, 0:64], sq4.ap[:, :n], [Cb, sq4], [ps[3]], start=(h == 0), stop=(h == 3))
                    S.act(rsd.ap[:, :n], ps[3].ap[0:64, :n], AF.Ln, [ps[3]], [rsd], scale=1.0 / 256, bias=EPS)
                    S.act(rsd.ap[:, :n], rsd.ap[:, :n], AF.Exp, [rsd], [rsd], scale=-0.5)
                    for h in range(4):
                        S.stt(oo.ap[:, :n], yd[h].ap[:, :n], snw_t.ap[:, gs * 4 + h:gs * 4 + h + 1], rsd.ap[:, :n], ALU.mult, ALU.mult, [yd[h], snw_t, rsd], [oo])
                        r0 = 256 + gs * 256 + h * 64
                        store_mix(S, r0, 64, t0, n, oo.ap[:, :n], oo, d_mix)
        S.barrier()


K1_SMALL = (("rdl", [128, 4]), ("rnw", [128, 2]), ("cw", [128, 8, 5]), ("cb", [128, 8]), ("dtb", [16, 1]), ("alog", [128, 16]),
            ("dsk", [128, 8]), ("snw", [64, 8]), ("dlam", [128, 4, 64]), ("dnw", [128, 1]))


def build_k1(cfg, last, lam_init):
    D, KD, T = cfg.D, cfg.KD, cfg.T
    nc = bass.Bass("TRN2", target_bir_lowering=False)
    dt_in = lambda n, sh: nc.dram_tensor(n, sh, F32, kind="ExternalInput").ap()
    xT = dt_in("xT", [D, T])
    I = {"w_in": dt_in("w_in", [D, cfg.PC]), "ada_w": dt_in("ada_w", [D, 2 * D]), "ada_b": dt_in("ada_b", [128, 2 * KD]),
         "ccol": dt_in("ccol", [128, KD, 2]), "nmw": dt_in("nmw", [128, KD]), "consts": dt_in("consts", [128, len(CNAMES), 128])}
    for n_ in ("cosd", "sind", "cosr", "sinr"):
        I[n_] = dt_in(n_, [128, T])
    for n_, sh in K1_SMALL:
        I[n_] = dt_in(n_, sh)
    mixT = nc.dram_tensor("mixT", [1024, T], F32, kind="ExternalOutput").ap()
    I["hT"] = nc.dram_tensor("hT", [D, T], BF16, kind="Internal").ap()
    I["projT"] = nc.dram_tensor("projT", [cfg.PC, T], F32, kind="Internal").ap()
    I["convT"] = nc.dram_tensor("convT", [1024, T], F32, kind="Internal").ap()

    def load_x(S, xt, t0, n):
        for kg in range(0, KD, 8):
            k2 = min(KD, kg + 8)
            S.dma("sp", xt.ap[:, kg:k2, :n], xT[kg * 128:k2 * 128, t0:t0 + n].rearrange("(k p) t -> p k t", p=128), writes=[xt])

    def store_mix(S, r0, nr, t0, n, src, sbuf, d_mix):
        S.dma("sp", mixT[r0:r0 + nr, t0:t0 + n], src, reads=[sbuf], writes=[d_mix])

    with contextlib.ExitStack() as es:
        S = Sched(nc)
        ps = [Buf(es.enter_context(nc.psum_tensor(f"ps{i}", [128, 512], F32)), f"ps{i}") for i in range(8)]
        emit_k1(nc, S, ps, cfg, last, lam_init, I, "", load_x, store_mix, Buf(None, "d_mix"))
    return nc


def col_layout(v):
    v = np.asarray(v, np.float32)
    return np.ascontiguousarray(v.reshape(-1, 128).T)


def rep128(v):
    v = np.asarray(v, np.float32)
    return np.ascontiguousarray(np.broadcast_to(v[None], (128,) + v.shape))


def k1_cols(cfg, q):
    NQ = cfg.NQ
    RW, SW, DW, G, SH = 256 * NQ, 512 * NQ, 256 * NQ, 2 * NQ, 8 * NQ
    sizes = (RW, RW, RW, RW, SW, SW + 2 * G * 128, 2 * SH, DW, DW, DW)
    o = np.concatenate([[0], np.cumsum(sizes)]).astype(int)
    heads = (2 * q, 2 * q + 1)
    cols = []
    for part in range(4):
        for h in heads:
            cols += list(range(o[part] + h * 128, o[part] + (h + 1) * 128))
    for g in heads:
        cols += list(range(o[4] + g * 256, o[4] + (g + 1) * 256))
    conv_ch = []
    for g in heads:
        conv_ch += list(range(g * 256, (g + 1) * 256))
    for g in heads:
        conv_ch += list(range(SW + g * 128, SW + (g + 1) * 128))
    for g in heads:
        conv_ch += list(range(SW + G * 128 + g * 128, SW + G * 128 + (g + 1) * 128))
    cols += [o[5] + c for c in conv_ch]
    dt_idx = [d * SH + g * 4 + r for d in range(2) for g in heads for r in range(4)]
    cols += [o[6] + i for i in dt_idx]
    for part in (7, 8, 9):
        for h in heads:
            cols += list(range(o[part] + h * 128, o[part] + (h + 1) * 128))
    assert len(cols) == cfg.PC
    return np.array(cols), np.array(conv_ch), dt_idx


def k1_inputs(cfg, inp, layer, b, q, xT_b, tabs):
    D, KD = cfg.D, cfg.KD
    cols, conv_ch, dt_idx = k1_cols(cfg, q)
    heads = [2 * q, 2 * q + 1]
    m = {}
    m["xT"] = xT_b
    m["w_in"] = np.ascontiguousarray(inp["w_in"][layer][:, cols])
    m["ada_w"] = np.ascontiguousarray(inp["ada_w"][layer][:, 0:2 * D])
    m["ada_b"] = col_layout(inp["ada_b"][layer][0:2 * D])
    m["ccol"] = np.ascontiguousarray(np.stack([col_layout(inp["c"][b]), col_layout(inp["c_ctx"])], axis=-1))
    m["nmw"] = col_layout(inp["norm_mix_w"][layer])
    m["consts"] = CPACK
    m["cosd"], m["sind"], m["cosr"], m["sinr"] = tabs
    rd = inp["ret_decay_logit"][layer]
    m["rdl"] = rep128(np.array([rd[0, heads[0]], rd[0, heads[1]], rd[1, heads[0]], rd[1, heads[1]]], np.float32))
    m["rnw"] = np.ascontiguousarray(inp["ret_norm_w"][layer].reshape(-1, 128)[heads].T)
    cwf = inp["ssd_conv_w"][layer][:, conv_ch]
    m["cw"] = np.ascontiguousarray(cwf.reshape(5, 8, 128).transpose(2, 1, 0))
    m["cb"] = np.ascontiguousarray(inp["ssd_conv_b"][layer][conv_ch].reshape(8, 128).T)
    m["dtb"] = np.ascontiguousarray(inp["ssd_dt_bias"][layer].reshape(-1)[dt_idx].reshape(16, 1))
    m["alog"] = rep128(inp["ssd_a_log"][layer].reshape(-1)[dt_idx])
    hidx = [g * 4 + r for g in heads for r in range(4)]
    m["dsk"] = rep128(inp["ssd_d"][layer][hidx])
    nw = inp["ssd_norm_w"][layer].reshape(-1, 4, 64)[heads]
    m["snw"] = np.ascontiguousarray(nw.reshape(8, 64).T)
    m["dlam"] = rep128(inp["diff_lambda"][layer])
    m["dnw"] = np.ascontiguousarray(inp["diff_norm_w"][layer].reshape(128, 1))
    return {k: np.ascontiguousarray(v, dtype=np.float32) for k, v in m.items()}


def assemble_mix(cfg, per_q):
    NQ = cfg.NQ
    RW, SW = 256 * NQ, 512 * NQ
    out = np.empty((cfg.MIX, per_q[0].shape[1]), np.float32)
    for q, loc in enumerate(per_q):
        out[q * 256:(q + 1) * 256] = loc[0:256]
        out[RW + q * 512:RW + (q + 1) * 512] = loc[256:768]
        out[RW + SW + q * 256:RW + SW + (q + 1) * 256] = loc[768:1024]
    return out


def k2_tiles(cfg, ntq, last):
    tl_c = 0 if last else cfg.CTX // ntq
    tl_l = cfg.SEQ // ntq
    tiles = []
    if tl_c:
        tiles.append((0, tl_c, 1))
    t = tl_c
    while t < tl_c + tl_l:
        n = min(512, tl_c + tl_l - t)
        tiles.append((t, n, 0))
        t += n
    return tiles, tl_c + tl_l


def emit_k2(nc, S, ps, cfg, ntq, last, moe, I, pf, x_src, load_mix, out_ap, d_out):
    D, KD, MIX = cfg.D, cfg.KD, cfg.MIX
    KM = MIX // 128
    tiles, TL = k2_tiles(cfg, ntq, last)
    if len(tiles) > 1 and tiles[0][2] == 1:
        tiles = tiles[1:] + tiles[:1]
    NF = (cfg.DFE if moe else cfg.DFF) // 128
    NE = cfg.NEXP if moe else 1
    FS = 22 if not moe else 16
    splits = []
    f = 0
    while f < NF:
        nf = min(FS, NF - f)
        splits.append((f, nf)); f += nf
    w_out, ada_w, ada_b, ccol, nfw, consts = I["w_out"], I["ada_w"], I["ada_b"], I["ccol"], I["nfw"], I["consts"]
    wg_d, wu_d, wd_d = I["wg"], I["wu"], I["wd"]
    if moe:
        rw, selc = I["rw"], I["selc"]
    if last:
        fnw = I["fnw"]
    xT = x_src
    xout = out_ap
    WBE = 32 * 256

    with contextlib.ExitStack() as es0:
        def SB(es, n, sh, dt=F32):
            return Buf(es.enter_context(nc.sbuf_tensor(pf + n, sh, dt)), n)
        C = SB(es0, "C", [128, 2, 128]); Cb = SB(es0, "Cb", [128, 2, 128], BF16)
        S.dma("sp", C.ap[:], consts[:, 0:2, :], writes=[C])
        S.copy("dve", Cb.ap[:, 0, :], C.ap[:, CI["ones"], :], [C], [Cb])
        S.copy("dve", Cb.ap[:, 1, :], C.ap[:, CI["ident"], :], [C], [Cb])
        cm = lambda n: C.ap[:, CI[n], :]
        ones_bf = Cb.ap[:, 0, :]
        MV = SB(es0, "MV", [128, KD, 8])
        with contextlib.ExitStack() as es:
            sc = SB(es, "sc", [128, KD, 2]); adb = SB(es, "adb", [128, 4 * KD]); nm = SB(es, "nm", [128, KD])
            modc = SB(es, "modc", [128, 4 * KD, 2])
            wa = [SB(es, f"wa{i}", [128, KD, 512]) for i in range(2)]
            S.dma("sp", sc.ap[:], ccol, writes=[sc]); S.dma("sp", adb.ap[:], ada_b, writes=[adb]); S.dma("sp", nm.ap[:], nfw, writes=[nm])
            S.act(sc.ap[:], sc.ap[:], AF.Silu, [sc], [sc])
            nfc = 4 * KD
            pm = ps[0]
            for fg in range(0, nfc, 4):
                w = wa[(fg // 4) % 2]
                for kg in range(0, KD, 8):
                    k2 = min(KD, kg + 8)
                    S.dma("sp", w.ap[:, kg:k2, :], ada_w[kg * 128:k2 * 128, fg * 128:(fg + 4) * 128].rearrange("(k p) c -> p k c", p=128), writes=[w])
                for f in range(fg, fg + 4):
                    for k in range(KD):
                        S.mm(pm.ap[:, 2 * f:2 * f + 2], w.ap[:, k, (f - fg) * 128:(f - fg + 1) * 128], sc.ap[:, k, :], [w, sc], [pm],
                             start=(k == 0), stop=(k == KD - 1), signal=(k == KD - 1))
            for j in range(2):
                S.tt("dve", modc.ap[:, :, j], pm.ap[:, 0:2 * nfc].rearrange("p (f j) -> p f j", j=2)[:, :, j], adb.ap[:], ALU.add, [pm, adb], [modc])
            for j in range(2):
                S.copy("dve", MV.ap[:, :, 0 + j], modc.ap[:, 0:KD, j], [modc], [MV])
                S.stt(MV.ap[:, :, 2 + j], modc.ap[:, 2 * KD:3 * KD, j], 1.0, nm.ap[:], ALU.add, ALU.mult, [modc, nm], [MV])
                S.copy("dve", MV.ap[:, :, 4 + j], modc.ap[:, KD:2 * KD, j], [modc], [MV])
                S.copy("dve", MV.ap[:, :, 6 + j], modc.ap[:, 3 * KD:4 * KD, j], [modc], [MV])
        S.barrier()

        with contextlib.ExitStack() as es:
            x1 = SB(es, "x1", [128, KD, 512]); mh = SB(es, "mh", [128, max(KD, KM), 512], BF16)
            aT = SB(es, "aT", [128, FS, 512], BF16)
            wbuf = [SB(es, f"wbuf{i}", [128, WBE], BF16) for i in range(4)]
            sq = [SB(es, f"sq{i}", [128, 512], BF16) for i in range(2)]
            tmp = [SB(es, f"tmp{i}", [128, 512]) for i in range(1 if moe else 2)]
            sg = [SB(es, f"sg{i}", [128, 512]) for i in range(2)]
            rs = SB(es, "rs", [128, 512])
            if moe:
                rwt = SB(es, "rwt", [128, KD, 8]); sel = SB(es, "sel", [8, 8, 128]); lgt = SB(es, "lgt", [128, 8]); m8 = SB(es, "m8", [128, 8])
                mk = SB(es, "mk", [128, 16]); gg = SB(es, "gg", [128, 4]); Gm = SB(es, "Gm", [128, 8]); GT = SB(es, "GT", [8, 512])
                gb = SB(es, "gb", [128, 8, 512], BF16); hf = [SB(es, "hf0", [128, 512])]
                S.dma("sp", rwt.ap[:], rw, writes=[rwt]); S.dma("sp", sel.ap[:], selc, writes=[sel])
            if last:
                fn = SB(es, "fn", [128, KD]); S.dma("sp", fn.ap[:], fnw, writes=[fn])
            wi = [0]
            cache = I.get("wcache")
            d_cache = Buf(None, "d_cache")
            blk = [0]
            first = [True]

            def wload(src, nk, ncol):
                b = wbuf[wi[0] % 4]; wi[0] += 1
                view = b.ap[:, 0:nk * ncol].rearrange("p (k c) -> p k c", c=ncol)
                bid = blk[0]; blk[0] += 1
                if cache is None or first[0]:
                    S.dma("pool", view, src.rearrange("(k p) c -> p k c", p=128), writes=[b])
                    if cache is not None:
                        S.dma("sp", cache[bid // 120][bid % 120, :, 0:nk * ncol], b.ap[:, 0:nk * ncol], reads=[b], writes=[d_cache])
                else:
                    S.dma("sp", b.ap[:, 0:nk * ncol], cache[bid // 120][bid % 120, :, 0:nk * ncol], reads=[d_cache], writes=[b])
                return b, view

            pi = [0, 0, 0]
            for ti_, (t0, n, kind) in enumerate(tiles):
                blk[0] = 0
                first[0] = (ti_ == 0)
                for kg in range(0, KD, 8):
                    k2 = min(KD, kg + 8)
                    S.dma("sp", x1.ap[:, kg:k2, :n], xT[kg * 128:k2 * 128, t0:t0 + n].rearrange("(k p) t -> p k t", p=128), writes=[x1])
                load_mix(S, mh, t0, n, kind)
                for db in range(0, KD, 2):
                    ncb = min(2, KD - db)
                    b, v = wload(w_out[:, db * 128:(db + ncb) * 128], KM, ncb * 128)
                    for j in range(ncb):
                        dc = db + j
                        p = ps[pi[0] % 2]; pi[0] += 1
                        for k in range(KM):
                            S.mm(p.ap[:, :n], v[:, k, j * 128:(j + 1) * 128], mh.ap[:, k, :n], [b, mh], [p], start=(k == 0), stop=(k == KM - 1), signal=(k == KM - 1))
                        S.stt(x1.ap[:, dc, :n], p.ap[:, :n], MV.ap[:, dc, kind:kind + 1], x1.ap[:, dc, :n], ALU.mult, ALU.add, [p, MV, x1], [x1])
                for k in range(KD):
                    s_ = sq[k % 2]
                    S.act(s_.ap[:, :n], x1.ap[:, k, :n], AF.Square, [x1], [s_])
                    S.mm(ps[6].ap[:, :n], ones_bf, s_.ap[:, :n], [s_, Cb], [ps[6]], start=(k == 0), stop=(k == KD - 1))
                S.act(rs.ap[:, :n], ps[6].ap[:, :n], AF.Ln, [ps[6]], [rs], scale=1.0 / D, bias=EPS)
                S.act(rs.ap[:, :n], rs.ap[:, :n], AF.Exp, [rs], [rs], scale=-0.5)
                nsub = (n + 127) // 128
                for k in range(KD):
                    t_ = tmp[k % len(tmp)]
                    S.tt("dve", t_.ap[:, :n], x1.ap[:, k, :n], rs.ap[:, :n], ALU.mult, [x1, rs], [t_])
                    S.act(mh.ap[:, k, :n], t_.ap[:, :n], AF.Identity, [t_, MV], [mh], scale=MV.ap[:, k, 2 + kind:3 + kind], bias=MV.ap[:, k, 4 + kind:5 + kind])
                    if moe:
                        h_ = hf[0]
                        S.act(h_.ap[:, :n], t_.ap[:, :n], AF.Identity, [t_, MV], [h_], scale=MV.ap[:, k, 2 + kind:3 + kind], bias=MV.ap[:, k, 4 + kind:5 + kind])
                        for sub in range(nsub):
                            m_ = min(128, n - sub * 128)
                            S.mm(ps[2 + sub].ap[:m_, 0:8], h_.ap[:, sub * 128:sub * 128 + m_], rwt.ap[:, k, :], [h_, rwt], [ps[2 + sub]], start=(k == 0), stop=(k == KD - 1))
                if moe:
                    for sub in range(nsub):
                        m_ = min(128, n - sub * 128)
                        S.copy("dve", lgt.ap[:m_, :], ps[2 + sub].ap[:m_, 0:8], [ps[2 + sub]], [lgt])
                        S.op("dve", lambda e: e.max(out=m8.ap[:m_, :], in_=lgt.ap[:m_, :]), [lgt], [m8])
                        S.ts("dve", mk.ap[:m_, 0:8], lgt.ap[:m_, :], m8.ap[:m_, 0:1], None, ALU.is_equal, None, [lgt, m8], [mk])
                        S.ts("dve", mk.ap[:m_, 8:16], lgt.ap[:m_, :], m8.ap[:m_, 1:2], None, ALU.is_equal, None, [lgt, m8], [mk])
                        S.tt("dve", gg.ap[:m_, 0:1], m8.ap[:m_, 1:2], m8.ap[:m_, 0:1], ALU.subtract, [m8], [gg])
                        S.act(gg.ap[:m_, 1:2], gg.ap[:m_, 0:1], AF.Exp, [gg], [gg])
                        S.ts("dve", gg.ap[:m_, 2:3], gg.ap[:m_, 1:2], 1.0, None, ALU.add, None, [gg], [gg])
                        S.op("dve", lambda e: e.reciprocal(out=gg.ap[:m_, 2:3], in_=gg.ap[:m_, 2:3]), [gg], [gg])
                        S.tt("dve", gg.ap[:m_, 3:4], gg.ap[:m_, 1:2], gg.ap[:m_, 2:3], ALU.mult, [gg], [gg])
                        S.ts("dve", Gm.ap[:m_, :], mk.ap[:m_, 0:8], gg.ap[:m_, 2:3], None, ALU.mult, None, [mk, gg], [Gm])
                        S.stt(Gm.ap[:m_, :], mk.ap[:m_, 8:16], gg.ap[:m_, 3:4], Gm.ap[:m_, :], ALU.mult, ALU.add, [mk, gg, Gm], [Gm])
                        S.tr(ps[6].ap[0:8, 0:m_], Gm.ap[:m_, :], C.ap[:m_, CI["ident"], 0:m_], [Gm, C], [ps[6]])
                        S.copy("dve", GT.ap[:, sub * 128:sub * 128 + m_], ps[6].ap[0:8, 0:m_], [ps[6]], [GT])
                    for e_ in range(NE):
                        S.mm(ps[7].ap[:, :n], sel.ap[:, e_, :], GT.ap[:, :n], [sel, GT], [ps[7]])
                        S.copy("act", gb.ap[:, e_, :n], ps[7].ap[:, :n], [ps[7]], [gb])
                for e_ in range(NE):
                    wg_e = wg_d[e_] if moe else wg_d
                    wu_e = wu_d[e_] if moe else wu_d
                    wd_e = wd_d[e_] if moe else wd_d
                    for (f0, nf) in splits:
                        for fb in range(f0, f0 + nf, 2):
                            ncb = min(2, f0 + nf - fb)
                            bg, vg = wload(wg_e[:, fb * 128:(fb + ncb) * 128], KD, ncb * 128)
                            bu, vu = wload(wu_e[:, fb * 128:(fb + ncb) * 128], KD, ncb * 128)
                            for j in range(ncb):
                                fc = fb + j
                                pg = ps[2 + pi[1] % 2]; pu = ps[4 + pi[1] % 2]; s_ = sg[pi[1] % 2]; pi[1] += 1
                                for k in range(KD):
                                    S.mm(pg.ap[:, :n], vg[:, k, j * 128:(j + 1) * 128], mh.ap[:, k, :n], [bg, mh], [pg], start=(k == 0), stop=(k == KD - 1), signal=(k == KD - 1))
                                for k in range(KD):
                                    S.mm(pu.ap[:, :n], vu[:, k, j * 128:(j + 1) * 128], mh.ap[:, k, :n], [bu, mh], [pu], start=(k == 0), stop=(k == KD - 1), signal=(k == KD - 1))
                                S.act(s_.ap[:, :n], pg.ap[:, :n], AF.Silu, [pg], [s_])
                                if moe:
                                    S.tt("dve", s_.ap[:, :n], s_.ap[:, :n], pu.ap[:, :n], ALU.mult, [s_, pu], [s_])
                                    S.tt("pool", aT.ap[:, fc - f0, :n], s_.ap[:, :n], gb.ap[:, e_, :n], ALU.mult, [s_, gb], [aT])
                                else:
                                    S.tt("dve", aT.ap[:, fc - f0, :n], s_.ap[:, :n], pu.ap[:, :n], ALU.mult, [s_, pu], [aT])
                        for db in range(0, KD, 2):
                            ncb = min(2, KD - db)
                            b, v = wload(wd_e[f0 * 128:(f0 + nf) * 128, db * 128:(db + ncb) * 128], nf, ncb * 128)
                            for j in range(ncb):
                                dc = db + j
                                p = ps[pi[0] % 2]; pi[0] += 1
                                for k in range(nf):
                                    S.mm(p.ap[:, :n], v[:, k, j * 128:(j + 1) * 128], aT.ap[:, k, :n], [b, aT], [p], start=(k == 0), stop=(k == nf - 1), signal=(k == nf - 1))
                                S.stt(x1.ap[:, dc, :n], p.ap[:, :n], MV.ap[:, dc, 6 + kind:7 + kind], x1.ap[:, dc, :n], ALU.mult, ALU.add, [p, MV, x1], [x1])
                if last:
                    for k in range(KD):
                        s_ = sq[k % 2]
                        S.act(s_.ap[:, :n], x1.ap[:, k, :n], AF.Square, [x1], [s_])
                        S.mm(ps[6].ap[:, :n], ones_bf, s_.ap[:, :n], [s_, Cb], [ps[6]], start=(k == 0), stop=(k == KD - 1))
                    S.act(rs.ap[:, :n], ps[6].ap[:, :n], AF.Ln, [ps[6]], [rs], scale=1.0 / D, bias=EPS)
                    S.act(rs.ap[:, :n], rs.ap[:, :n], AF.Exp, [rs], [rs], scale=-0.5)
                    for k in range(KD):
                        S.stt(x1.ap[:, k, :n], x1.ap[:, k, :n], fn.ap[:, k:k + 1], rs.ap[:, :n], ALU.mult, ALU.mult, [x1, fn, rs], [x1])
                for kg in range(0, KD, 8):
                    k2 = min(KD, kg + 8)
                    S.dma("sp", xout[kg * 128:k2 * 128, t0:t0 + n].rearrange("(k p) t -> p k t", p=128), x1.ap[:, kg:k2, :n], reads=[x1], writes=[d_out])
        S.barrier()


def build_k2(cfg, ntq, last, moe):
    D, KD, MIX = cfg.D, cfg.KD, cfg.MIX
    KM = MIX // 128
    tiles, TL = k2_tiles(cfg, ntq, last)
    NF = (cfg.DFE if moe else cfg.DFF) // 128
    NE = cfg.NEXP
    nc = bass.Bass("TRN2", target_bir_lowering=False)
    dt_in = lambda n, sh: nc.dram_tensor(n, sh, F32, kind="ExternalInput").ap()
    xT = dt_in("xT", [D, TL]); mixT = dt_in("mixT", [MIX, TL])
    I = {"w_out": dt_in("w_out", [MIX, D]), "ada_w": dt_in("ada_w", [D, 4 * D]), "ada_b": dt_in("ada_b", [128, 4 * KD]),
         "ccol": dt_in("ccol", [128, KD, 2]), "nfw": dt_in("nfw", [128, KD]), "consts": dt_in("consts", [128, len(CNAMES), 128])}
    if moe:
        I["wg"] = dt_in("wg", [NE, D, NF * 128]); I["wu"] = dt_in("wu", [NE, D, NF * 128]); I["wd"] = dt_in("wd", [NE, NF * 128, D])
        I["rw"] = dt_in("rw", [128, KD, 8]); I["selc"] = dt_in("selc", [8, 8, 128])
    else:
        I["wg"] = dt_in("wg", [D, NF * 128]); I["wu"] = dt_in("wu", [D, NF * 128]); I["wd"] = dt_in("wd", [NF * 128, D])
    if last:
        I["fnw"] = dt_in("fnw", [128, KD])
    xout = nc.dram_tensor("xout", [D, TL], F32, kind="ExternalOutput").ap()

    def load_mix(S, mh, t0, n, kind):
        for kg in range(0, KM, 8):
            k2 = min(KM, kg + 8)
            S.dma("pool", mh.ap[:, kg:k2, :n], mixT[kg * 128:k2 * 128, t0:t0 + n].rearrange("(k p) t -> p k t", p=128), writes=[mh])

    with contextlib.ExitStack() as es:
        S = Sched(nc)
        ps = [Buf(es.enter_context(nc.psum_tensor(f"ps{i}", [128, 512], F32)), f"ps{i}") for i in range(8)]
        emit_k2(nc, S, ps, cfg, ntq, last, moe, I, "", xT, load_mix, xout, Buf(None, "d_out"))
    return nc


SELC = np.zeros((8, 8, 128), np.float32)
for _e in range(8):
    SELC[_e, _e, :] = 1.0


def k2_inputs(cfg, inp, layer, b, xT_loc, mixT_loc, last, moe):
    D = cfg.D
    m = {"xT": xT_loc, "mixT": mixT_loc, "w_out": inp["w_out"][layer],
         "ada_w": np.ascontiguousarray(inp["ada_w"][layer][:, 2 * D:6 * D]),
         "ada_b": col_layout(inp["ada_b"][layer][2 * D:6 * D]),
         "ccol": np.ascontiguousarray(np.stack([col_layout(inp["c"][b]), col_layout(inp["c_ctx"])], axis=-1)),
         "nfw": col_layout(inp["norm_ffn_w"][layer]), "consts": CPACK}
    i = layer // 2
    if moe:
        m["wg"] = inp["moe_w_gate"][i]; m["wu"] = inp["moe_w_up"][i]; m["wd"] = inp["moe_w_down"][i]
        m["rw"] = np.ascontiguousarray(inp["moe_router"][i].reshape(cfg.KD, 128, 8).transpose(1, 0, 2))
        m["selc"] = SELC
    else:
        m["wg"] = inp["dense_w_gate"][i]; m["wu"] = inp["dense_w_up"][i]; m["wd"] = inp["dense_w_down"][i]
    if last:
        m["fnw"] = col_layout(inp["final_norm_w"])
    return {k: np.ascontiguousarray(v, dtype=np.float32) for k, v in m.items()}


def tq_index(cfg, ntq, tq, last):
    lat = cfg.CTX + np.arange(tq * (cfg.SEQ // ntq), (tq + 1) * (cfg.SEQ // ntq))
    if last:
        return lat
    c = np.arange(tq * (cfg.CTX // ntq), (tq + 1) * (cfg.CTX // ntq))
    return np.concatenate([c, lat])


def run_pipeline(cfg, inp, ntq, depth=2, verbose=False):
    B, NQ = cfg.B, cfg.NQ
    tabs = rope_tables(cfg, 64) + rope_tables(cfg, 128)
    xT = [np.ascontiguousarray(np.concatenate([inp["ctx"][b], inp["x"][b]], axis=0).T) for b in range(B)]
    out = None
    for layer in range(depth):
        last = layer == depth - 1
        moe = layer % 2 == 1
        lam_init = 0.8 - 0.6 * math.exp(-0.3 * layer)
        nc1 = build_k1(cfg, last, lam_init)
        maps = [k1_inputs(cfg, inp, layer, b, q, xT[b], tabs) for b in range(B) for q in range(NQ)]
        res = run_bass_kernel_spmd(nc1, maps, core_ids=list(range(len(maps))))
        mix = [assemble_mix(cfg, [res.results[b * NQ + q]["mixT"] for q in range(NQ)]) for b in range(B)]
        del maps, res
        nc2 = build_k2(cfg, ntq, last, moe)
        maps = []
        for b in range(B):
            for tq in range(ntq):
                idx = tq_index(cfg, ntq, tq, last)
                maps.append(k2_inputs(cfg, inp, layer, b, xT[b][:, idx], mix[b][:, idx], last, moe))
        res = run_bass_kernel_spmd(nc2, maps, core_ids=list(range(len(maps))))
        if last:
            out = np.empty((B, cfg.SEQ, cfg.D), np.float32)
            for b in range(B):
                for tq in range(ntq):
                    idx = tq_index(cfg, ntq, tq, True) - cfg.CTX
                    out[b, idx, :] = res.results[b * ntq + tq]["xout"].T
        else:
            for b in range(B):
                for tq in range(ntq):
                    idx = tq_index(cfg, ntq, tq, False)
                    xT[b][:, idx] = res.results[b * ntq + tq]["xout"]
        del maps, res
    return out, xT


def mix_perm(cfg):
    NQ = cfg.NQ
    RW, SW = 256 * NQ, 512 * NQ
    idx = []
    for q in range(NQ):
        idx += list(range(q * 256, (q + 1) * 256))
        idx += list(range(RW + q * 512, RW + (q + 1) * 512))
        idx += list(range(RW + SW + q * 256, RW + SW + (q + 1) * 256))
    return np.array(idx)


def _splits(NF, FS):
    out = []
    f = 0
    while f < NF:
        out.append(min(FS, NF - f)); f += out[-1]
    return out


def build_fused(cfg, stop=99):
    D, KD, T, NQ, CTX, SEQ, MIX = cfg.D, cfg.KD, cfg.T, cfg.NQ, cfg.CTX, cfg.SEQ, cfg.MIX
    KM = MIX // 128
    TLc, TLl = CTX // NQ, SEQ // NQ
    TL0 = TLc + TLl
    NTJ = TLl // 512
    NE = cfg.NEXP
    groups = [[b * NQ + q for q in range(NQ)] for b in range(cfg.B)]
    nc = bass.Bass("TRN2", target_bir_lowering=False)
    dt_in = lambda n, sh, dt=F32: nc.dram_tensor(n, sh, dt, kind="ExternalInput").ap()
    dt_sc = lambda n, sh, dt=F32: nc.dram_tensor(n, sh, dt, kind="Internal").ap()
    xTb = dt_in("xTb", [D, T]); xTl = dt_in("xTl", [D, TL0])
    sh = {"ccol": dt_in("ccol", [128, KD, 2]), "consts": dt_in("consts", [128, len(CNAMES), 128]), "selc": dt_in("selc", [8, 8, 128])}
    for n_ in ("cosd", "sind", "cosr", "sinr"):
        sh[n_] = dt_in(n_, [128, T])
    L = []
    for l in range(2):
        d = {"w_in": dt_in(f"w_in{l}", [D, cfg.PC]), "ada_w": dt_in(f"ada_w{l}", [D, 6 * D]), "ada_b": dt_in(f"ada_b{l}", [128, 6 * KD]),
             "nmw": dt_in(f"nmw{l}", [128, KD]), "nfw": dt_in(f"nfw{l}", [128, KD]), "w_out": dt_in(f"w_out{l}", [MIX, D]),
             "midx": dt_in(f"midx{l}", [128, KM, NTJ + 1], mybir.dt.int32)}
        for n_, shp in K1_SMALL:
            d[n_] = dt_in(f"{n_}{l}", shp)
        L.append(d)
    NFd = cfg.DFF // 128; NFe = cfg.DFE // 128
    L[0]["wg"] = dt_in("wg0", [D, NFd * 128]); L[0]["wu"] = dt_in("wu0", [D, NFd * 128]); L[0]["wd"] = dt_in("wd0", [NFd * 128, D])
    L[1]["wg"] = dt_in("wg1", [NE, D, NFe * 128]); L[1]["wu"] = dt_in("wu1", [NE, D, NFe * 128]); L[1]["wd"] = dt_in("wd1", [NE, NFe * 128, D])
    L[1]["rw"] = dt_in("rw1", [128, KD, 8]); L[1]["fnw"] = dt_in("fnw", [128, KD])
    xout = nc.dram_tensor("xout", [D, TLl], F32, kind="ExternalOutput").ap()
    hT = dt_sc("hT", [D, T], BF16); projT = dt_sc("projT", [cfg.PC, T]); convT = dt_sc("convT", [1024, T])
    mixl = [dt_sc(f"mixl{l}", [NQ * NTJ * 1024, 512], BF16) for l in range(2)]
    mixlg = [dt_sc(f"mixlg{l}", [NQ * NQ * NTJ * 1024, 512], BF16) for l in range(2)]
    mixc = dt_sc("mixc", [NQ * 1024, TLc], BF16); mixcg = dt_sc("mixcg", [NQ * NQ * 1024, TLc], BF16)
    x1loc = dt_sc("x1loc", [D, TL0]); xg = dt_sc("xg", [NQ * D, TL0])
    d_xg = Buf(None, "d_xg"); d_x1 = Buf(None, "d_x1")
    nblk0 = (KD + 1) // 2 + sum(2 * ((nf + 1) // 2) + (KD + 1) // 2 for nf in _splits(NFd, 22))
    nblk1 = (KD + 1) // 2 + NE * sum(2 * ((nf + 1) // 2) + (KD + 1) // 2 for nf in _splits(NFe, 16))
    CB = 120
    L[0]["wcache"] = [dt_sc(f"wcache0_{i}", [min(CB, nblk0 - i * CB), 128, 32 * 256], BF16) for i in range((nblk0 + CB - 1) // CB)]
    L[1]["wcache"] = [dt_sc(f"wcache1_{i}", [min(CB, nblk1 - i * CB), 128, 32 * 256], BF16) for i in range((nblk1 + CB - 1) // CB)]
    xg5 = xg.rearrange("(k h q r) t -> h q r k t", h=2, q=NQ, r=64)

    with contextlib.ExitStack() as es:
        S = Sched(nc)
        ps = [Buf(es.enter_context(nc.psum_tensor(f"ps{i}", [128, 512], F32)), f"ps{i}") for i in range(8)]
        midx_sb = [Buf(es.enter_context(nc.sbuf_tensor(f"midx_sb{l}", [128, KM, NTJ + 1], mybir.dt.int32)), f"midx{l}") for l in range(2)]
        for l in range(2):
            S.dma("sp", midx_sb[l].ap[:], L[l]["midx"], writes=[midx_sb[l]])
        for l in range(2):
            last = l == 1
            lam_init = 0.8 - 0.6 * math.exp(-0.3 * l)
            I1 = dict(sh); I1.update(L[l]); I1["ada_w"] = L[l]["ada_w"][:, 0:2 * D]; I1["ada_b"] = L[l]["ada_b"][:, 0:2 * KD]
            I1["hT"], I1["projT"], I1["convT"] = hT, projT, convT
            d_mix = Buf(None, f"d_mix{l}"); d_mixg = Buf(None, f"d_mixg{l}")

            def load_x(S_, xt, t0, n, l=l):
                if l == 0:
                    for kg in range(0, KD, 8):
                        k2 = min(KD, kg + 8)
                        S_.dma("sp", xt.ap[:, kg:k2, :n], xTb[kg * 128:k2 * 128, t0:t0 + n].rearrange("(k p) t -> p k t", p=128), writes=[xt])
                elif t0 < CTX:
                    for tq in range(NQ):
                        for kg in range(0, KD, 8):
                            k2 = min(KD, kg + 8)
                            for h in range(2):
                                S_.dma("sp", xt.ap[h * 64:(h + 1) * 64, kg:k2, tq * TLc:(tq + 1) * TLc], xg5[h, tq, :, kg:k2, 0:TLc], reads=[d_xg], writes=[xt])
                else:
                    lt = t0 - CTX
                    tq = lt // TLl; off = TLc + lt % TLl
                    for kg in range(0, KD, 8):
                        k2 = min(KD, kg + 8)
                        for h in range(2):
                            S_.dma("sp", xt.ap[h * 64:(h + 1) * 64, kg:k2, :n], xg5[h, tq, :, kg:k2, off:off + n], reads=[d_xg], writes=[xt])

            def store_mix(S_, r0, nr, t0, n, src, sbuf, dm, l=l):
                if t0 < CTX:
                    for tq in range(NQ):
                        S_.dma("pool", mixc[tq * 1024 + r0:tq * 1024 + r0 + nr, :], src[:, tq * TLc:(tq + 1) * TLc], reads=[sbuf], writes=[dm])
                else:
                    lt = t0 - CTX
                    tq = lt // TLl; j = (lt % TLl) // 512
                    base = (tq * NTJ + j) * 1024 + r0
                    S_.dma("pool", mixl[l][base:base + nr, :], src, reads=[sbuf], writes=[dm])

            emit_k1(nc, S, ps, cfg, last, lam_init, I1, f"a{l}_", load_x, store_mix, d_mix)
            if stop <= 4 * l + 0:
                break
            for c in range(NQ * NTJ):
                S.cc_allgather(mixl[l][c * 1024:(c + 1) * 1024, :], mixlg[l][c * NQ * 1024:(c + 1) * NQ * 1024, :], groups, reads=[d_mix], writes=[d_mixg])
            if not last:
                S.cc_allgather(mixc, mixcg, groups, reads=[d_mix], writes=[d_mixg])
            S.barrier()
            if stop <= 4 * l + 1:
                break
            I2 = dict(sh); I2.update(L[l]); I2["ada_w"] = L[l]["ada_w"][:, 2 * D:6 * D]; I2["ada_b"] = L[l]["ada_b"][:, 2 * KD:6 * KD]
            tl_c = 0 if last else TLc

            def load_mix(S_, mh, t0, n, kind, l=l, tl_c=tl_c):
                if kind == 1:
                    src = mixcg; col = NTJ
                else:
                    src = mixlg[l]; col = (t0 - tl_c) // 512
                for km in range(KM):
                    S_.idma(mh.ap[:, km, :n], src, midx_sb[l].ap[:, km, col:col + 1], reads=[d_mixg, midx_sb[l]], writes=[mh])

            if not last:
                emit_k2(nc, S, ps, cfg, NQ, False, False, I2, f"b{l}_", xTl, load_mix, x1loc, d_x1)
                if stop <= 4 * l + 2:
                    break
                for c in range(D // 64):
                    S.cc_allgather(x1loc[c * 64:(c + 1) * 64, :], xg[c * NQ * 64:(c + 1) * NQ * 64, :], groups, reads=[d_x1], writes=[d_xg])
                S.barrier()
            else:
                d_out = Buf(None, "d_out")
                emit_k2(nc, S, ps, cfg, NQ, True, True, I2, f"b{l}_", x1loc[:, TLc:TL0], load_mix, xout, d_out)
    return nc


def fused_inputs(cfg, inp, b, q, shared):
    D, KD, NQ, CTX, SEQ, MIX = cfg.D, cfg.KD, cfg.NQ, cfg.CTX, cfg.SEQ, cfg.MIX
    KM = MIX // 128
    TLc, TLl = CTX // NQ, SEQ // NQ
    NTJ = TLl // 512
    xTb = shared["xT"][b]
    m = {"xTb": xTb, "xTl": np.ascontiguousarray(xTb[:, tq_index(cfg, NQ, q, False)])}
    m["ccol"] = np.ascontiguousarray(np.stack([col_layout(inp["c"][b]), col_layout(inp["c_ctx"])], axis=-1))
    m["consts"] = CPACK; m["selc"] = SELC
    m["cosd"], m["sind"], m["cosr"], m["sinr"] = shared["tabs"]
    for l in range(2):
        k1 = k1_inputs(cfg, inp, l, b, q, xTb, shared["tabs"])
        m[f"w_in{l}"] = k1["w_in"]
        for n_, _ in K1_SMALL:
            m[f"{n_}{l}"] = k1[n_]
        m[f"ada_w{l}"] = inp["ada_w"][l]
        m[f"ada_b{l}"] = col_layout(inp["ada_b"][l])
        m[f"nmw{l}"] = col_layout(inp["norm_mix_w"][l]); m[f"nfw{l}"] = col_layout(inp["norm_ffn_w"][l])
        m[f"w_out{l}"] = shared["w_out"][l]
        ntj1 = NTJ + 1
        idx = np.zeros((128, KM, ntj1), np.int64)
        p = np.arange(128)
        for km in range(KM):
            qq = km // 8; rr = (km % 8) * 128 + p
            for j in range(NTJ):
                idx[:, km, j] = (q * NTJ + j) * (NQ * 1024) + qq * 1024 + rr
            idx[:, km, NTJ] = qq * (NQ * 1024) + q * 1024 + rr
        m[f"midx{l}"] = idx.astype(np.int32)
    m["wg0"] = inp["dense_w_gate"][0]; m["wu0"] = inp["dense_w_up"][0]; m["wd0"] = inp["dense_w_down"][0]
    m["wg1"] = inp["moe_w_gate"][0]; m["wu1"] = inp["moe_w_up"][0]; m["wd1"] = inp["moe_w_down"][0]
    m["rw1"] = np.ascontiguousarray(inp["moe_router"][0].reshape(KD, 128, 8).transpose(1, 0, 2))
    m["fnw"] = col_layout(inp["final_norm_w"])
    out = {}
    for k, v in m.items():
        out[k] = np.ascontiguousarray(v, dtype=(np.int32 if k.startswith("midx") else np.float32))
    return out


def run_fused(cfg, inp, stop=99):
    B, NQ = cfg.B, cfg.NQ
    TLl = cfg.SEQ // NQ
    perm = mix_perm(cfg)
    shared = {"tabs": rope_tables(cfg, 64) + rope_tables(cfg, 128),
              "xT": [np.ascontiguousarray(np.concatenate([inp["ctx"][b], inp["x"][b]], axis=0).T) for b in range(B)],
              "w_out": [np.ascontiguousarray(inp["w_out"][l][perm]) for l in range(2)]}
    nc = build_fused(cfg, stop)
    maps = [fused_inputs(cfg, inp, b, q, shared) for b in range(B) for q in range(NQ)]
    res = run_bass_kernel_spmd(nc, maps, core_ids=list(range(len(maps))))
    out = np.empty((B, cfg.SEQ, cfg.D), np.float32)
    for b in range(B):
        for q in range(NQ):
            out[b, q * TLl:(q + 1) * TLl, :] = res.results[b * NQ + q]["xout"].T
    return out


def kernel(**inputs):
    inp = {k: np.asarray(v) for k, v in inputs.items()}
    return run_fused(FULL, inp)
```
